# Optimizing a Trainium2 kernel written in Bass

```python
import jax
import jax.numpy as jnp
from jax import lax
import numpy as np

D_MODEL = 1024
BATCH = 8
SEQ = 4096
DEPTH = 2

GRID_W = 64
CTX_LEN = 256
EPS = 1e-6

HGRN_HEADS = 4
HGRN_DK = 128
HGRN_DV = 128
HGRN_W = HGRN_HEADS * HGRN_DK
HGRN_VW = HGRN_HEADS * HGRN_DV
HGRN_CHUNK = 64
SGU_HEADS = 4
SGU_HEAD_DIM = 128
SGU_W = SGU_HEADS * SGU_HEAD_DIM
SGU_CHUNK = 128
ROWS_PER_CHUNK = SGU_CHUNK // GRID_W
MIX_W = HGRN_VW + SGU_W
IN_W = 3 * HGRN_W + 2 * HGRN_VW + 2 * SGU_W
SPLIT_POINTS = (HGRN_W, 2 * HGRN_W, 3 * HGRN_W, 3 * HGRN_W + HGRN_VW,
                3 * HGRN_W + 2 * HGRN_VW, 3 * HGRN_W + 2 * HGRN_VW + SGU_W)
N_GROUPS = 4
EXPERTS_PER_GROUP = 8
N_EXPERTS = N_GROUPS * EXPERTS_PER_GROUP
TOP_K = 2
D_EXPERT = 512
MOE_BLOCK = 128

kernel_name = 'hybrid_hgrn2_sgu_hmoe_dit'


def rmsnorm(x, gain):
    xf = x.astype(jnp.float32)
    y = xf * lax.rsqrt(jnp.mean(xf * xf, axis=-1, keepdims=True) + EPS)
    return (y * gain.astype(jnp.float32)).astype(x.dtype)


def modulate(h, shift, scale):
    return h * (1 + scale) + shift


def flip(t):
    return jnp.flip(t, axis=1)


def _heads(t, n_heads):
    return t.astype(jnp.float32).reshape(t.shape[0], t.shape[1], n_heads, -1)


def forget_gate(z, lb):
    zf = z.astype(jnp.float32)
    lb = lb.astype(jnp.float32)
    log_f = jnp.logaddexp(jnp.log(lb), jnp.log1p(-lb) + jax.nn.log_sigmoid(zf))
    k = (1.0 - lb) * jax.nn.sigmoid(-zf)
    return log_f, k


def split_mixer_inputs(p, lb):
    q, z_fwd, z_bwd, i, g, u, v = jnp.split(p, SPLIT_POINTS, axis=-1)
    lf_fwd, k_fwd = forget_gate(z_fwd, lb[0])
    lf_bwd, k_bwd = forget_gate(z_bwd, lb[1])
    h = HGRN_HEADS
    return (_heads(q, h), _heads(i, h), _heads(lf_fwd, h), _heads(k_fwd, h),
            _heads(lf_bwd, h), _heads(k_bwd, h), g, u, v)


def hgrn_chunk(q, k, v, log_f, s0):
    b, l = q.shape[0], q.shape[1]
    n = l // HGRN_CHUNK

    def blocks(t):
        return t.reshape(b, n, HGRN_CHUNK, t.shape[2], t.shape[3])

    q, k, v, log_f = blocks(q), blocks(k), blocks(v), blocks(log_f)
    cum = jnp.cumsum(log_f, axis=2)
    ref = cum[:, :, HGRN_CHUNK // 2 - 1][:, :, None]
    tot = cum[:, :, -1]
    scores = jnp.einsum('bnihd,bnjhd->bnhij', q * jnp.exp(cum - ref), k * jnp.exp(ref - cum))
    lower_tri = jnp.tril(jnp.ones((HGRN_CHUNK, HGRN_CHUNK), dtype=bool))
    scores = jnp.where(lower_tri, scores, 0.0)
    o_intra = jnp.einsum('bnhij,bnjhe->bnihe', scores, v)
    u = jnp.einsum('bnjhd,bnjhe->nbhde', k * jnp.exp(tot[:, :, None] - cum), v)
    decay = jnp.exp(tot).transpose(1, 0, 2, 3)

    def step(s, inp):
        d, un = inp
        return d[..., None] * s + un, s

    s_final, s_start = lax.scan(step, s0, (decay, u))
    o_inter = jnp.einsum('bnihd,nbhde->bnihe', q * jnp.exp(cum), s_start)
    return (o_intra + o_inter).reshape(b, l, q.shape[3], v.shape[4]), s_final


def hgrn_final_state(k, v, log_f):
    cum = jnp.cumsum(log_f, axis=1)
    return jnp.einsum('blhd,blhe->bhde', k * jnp.exp(cum[:, -1:] - cum), v)


def hgrn_bidirectional(q, i, lf_f, k_f, lf_b, k_b, s0_f, s0_b):
    o_f, s_f = hgrn_chunk(q, k_f, i, lf_f, s0_f)
    o_b, s_b = hgrn_chunk(flip(q), flip(k_b), flip(i), flip(lf_b), s0_b)
    return o_f + flip(o_b), s_f, s_b


def head_rmsnorm(o, gain):
    o = o * lax.rsqrt(jnp.mean(o * o, axis=-1, keepdims=True) + EPS)
    return o.reshape(o.shape[0], o.shape[1], -1) * gain.astype(jnp.float32)


def spatial_gating(u, v, gain, w_s, b_s, n_chunks):
    b = u.shape[0]
    v = rmsnorm(v, gain).reshape(b, n_chunks, SGU_CHUNK, SGU_HEADS, SGU_HEAD_DIM)
    mixed = jnp.einsum('hij,bnjhd->bnihd', w_s, v) + b_s.T[None, None, :, :, None]
    return u * mixed.reshape(u.shape)


def mixer_output(o, g, u, v, n_chunks, hgrn_gain, sgu_gain, w_s, b_s, w_o):
    hg = (head_rmsnorm(o, hgrn_gain) * jax.nn.silu(g.astype(jnp.float32))).astype(g.dtype)
    sg = spatial_gating(jax.nn.gelu(u), jax.nn.gelu(v), sgu_gain, w_s, b_s, n_chunks)
    return jnp.concatenate([hg, sg], axis=-1) @ w_o


def hier_moe(h, w_group, b_group, w_router, b_router, w_gate, w_up, w_down):
    t, d = h.shape
    logits_g = (h @ w_group).astype(jnp.float32) + b_group.astype(jnp.float32)
    p_group = jax.nn.softmax(logits_g, axis=-1)
    g_sel = jnp.argmax(logits_g, axis=-1)
    p_sel = jnp.take_along_axis(p_group, g_sel[:, None], axis=-1)
    logits_e = ((h @ w_router).astype(jnp.float32) + b_router.astype(jnp.float32))
    logits_e = logits_e.reshape(t, N_GROUPS, EXPERTS_PER_GROUP)
    logits_in = jnp.take_along_axis(logits_e, g_sel[:, None, None], axis=1)[:, 0]
    top_logits, top_idx = lax.top_k(logits_in, TOP_K)
    combine = p_sel * jax.nn.softmax(top_logits, axis=-1)
    expert = (g_sel[:, None] * EXPERTS_PER_GROUP + top_idx).reshape(-1)
    n_slots = t * TOP_K
    order = jnp.argsort(expert)
    e_sorted = expert[order]
    tok_sorted = order // TOP_K
    counts = jax.ops.segment_sum(jnp.ones((n_slots,), jnp.int32), expert, num_segments=N_EXPERTS)
    padded = (counts + MOE_BLOCK - 1) // MOE_BLOCK * MOE_BLOCK
    pad_end = jnp.cumsum(padded)
    pad_start = pad_end - padded
    start = jnp.cumsum(counts) - counts
    dest = pad_start[e_sorted] + jnp.arange(n_slots) - start[e_sorted]
    n_blocks = -(-n_slots // MOE_BLOCK) + N_EXPERTS
    buf = jnp.zeros((n_blocks * MOE_BLOCK, d), h.dtype).at[dest].set(h[tok_sorted])
    block_expert = jnp.minimum(
        jnp.searchsorted(pad_end, jnp.arange(n_blocks) * MOE_BLOCK, side='right'), N_EXPERTS - 1)

    def expert_block(args):
        xb, e = args
        return (jax.nn.silu(xb @ w_gate[e]) * (xb @ w_up[e])) @ w_down[e]

    out = lax.map(expert_block, (buf.reshape(n_blocks, MOE_BLOCK, d), block_expert))
    y_sorted = out.reshape(-1, d)[dest] * combine.reshape(-1)[order][:, None].astype(h.dtype)
    return jax.ops.segment_sum(y_sorted, tok_sorted, num_segments=t)


def setup_inputs(seed: int = 0) -> dict:
    key = jax.random.key(seed)
    ks = jax.random.split(key, 24)
    d = D_MODEL

    def nrm(k, shape, scale):
        return jax.random.normal(k, shape, jnp.float32) * scale

    return {
        'x': nrm(ks[0], (BATCH, SEQ, d), 1.0),
        'c': nrm(ks[1], (BATCH, d), 1.0),
        'ctx': nrm(ks[2], (BATCH, CTX_LEN, d), 1.0),
        'c_ctx': nrm(ks[3], (d,), 1.0),
        'norm1': 1.0 + nrm(ks[4], (DEPTH, d), 0.02),
        'norm2': 1.0 + nrm(ks[5], (DEPTH, d), 0.02),
        'w_mod': nrm(ks[6], (DEPTH, d, 6 * d), 0.5 * d ** -0.5),
        'b_mod': nrm(ks[7], (DEPTH, 6 * d), 0.02),
        'w_in': nrm(ks[8], (DEPTH, d, IN_W), d ** -0.5),
        'lb_logits': nrm(ks[9], (DEPTH, 2, HGRN_W), 0.5),
        'hgrn_norm': 1.0 + nrm(ks[10], (DEPTH, HGRN_VW), 0.02),
        'sgu_norm': 1.0 + nrm(ks[11], (DEPTH, SGU_W), 0.02),
        'sgu_w': nrm(ks[12], (DEPTH, SGU_HEADS, SGU_CHUNK, SGU_CHUNK), SGU_CHUNK ** -0.5),
        'sgu_b': 1.0 + nrm(ks[13], (DEPTH, SGU_HEADS, SGU_CHUNK), 0.02),
        'w_out': nrm(ks[14], (DEPTH, MIX_W, d), MIX_W ** -0.5),
        'w_group': nrm(ks[15], (DEPTH, d, N_GROUPS), d ** -0.5),
        'b_group': nrm(ks[16], (DEPTH, N_GROUPS), 0.01),
        'w_router': nrm(ks[17], (DEPTH, d, N_EXPERTS), d ** -0.5),
        'b_router': nrm(ks[18], (DEPTH, N_EXPERTS), 0.01),
        'w_gate': nrm(ks[19], (DEPTH, N_EXPERTS, d, D_EXPERT), d ** -0.5),
        'w_up': nrm(ks[20], (DEPTH, N_EXPERTS, d, D_EXPERT), d ** -0.5),
        'w_down': nrm(ks[21], (DEPTH, N_EXPERTS, D_EXPERT, d), D_EXPERT ** -0.5),
        'final_norm': 1.0 + nrm(ks[22], (d,), 0.02),
    }


def reference(x, c, ctx, c_ctx, norm1, norm2, w_mod, b_mod, w_in, lb_logits, hgrn_norm,
              sgu_norm, sgu_w, sgu_b, w_out, w_group, b_group, w_router, b_router,
              w_gate, w_up, w_down, final_norm):
    b, l, d = x.shape
    rows = l // GRID_W
    n_lat_chunks = rows // ROWS_PER_CHUNK
    n_ctx_chunks = ctx.shape[1] // SGU_CHUNK
    lb_cum = jnp.cumsum(jax.nn.softmax(lb_logits.astype(jnp.float32), axis=0), axis=0)
    lower_bound = jnp.maximum(lb_cum - lb_cum[0:1], 0.0)
    s_zero = jnp.zeros((b, HGRN_HEADS, HGRN_DK, HGRN_DV), jnp.float32)
    y = ctx
    for layer in range(DEPTH):
        last = layer == DEPTH - 1
        mod_lat = jax.nn.silu(c) @ w_mod[layer] + b_mod[layer]
        mod_ctx = jax.nn.silu(c_ctx) @ w_mod[layer] + b_mod[layer]
        sh1, sc1, g1, sh2, sc2, g2 = jnp.split(mod_lat[:, None, :], 6, axis=-1)
        csh1, csc1, cg1, csh2, csc2, cg2 = jnp.split(mod_ctx[None, None, :], 6, axis=-1)

        p_lat = modulate(rmsnorm(x, norm1[layer]), sh1, sc1) @ w_in[layer]
        p_ctx = modulate(rmsnorm(y, norm1[layer]), csh1, csc1) @ w_in[layer]
        q, i, lf_f, k_f, lf_b, k_b, g, u, v = split_mixer_inputs(p_lat, lower_bound[layer])
        cq, ci, clf_f, ck_f, clf_b, ck_b, cg, cu, cv = split_mixer_inputs(p_ctx, lower_bound[layer])
        if last:
            s_f = hgrn_final_state(ck_f, ci, clf_f)
            s_b = hgrn_final_state(flip(ck_b), flip(ci), flip(clf_b))
        else:
            co, s_f, s_b = hgrn_bidirectional(cq, ci, clf_f, ck_f, clf_b, ck_b, s_zero, s_zero)
            out_ctx = mixer_output(co, cg, cu, cv, n_ctx_chunks, hgrn_norm[layer], sgu_norm[layer],
                                   sgu_w[layer], sgu_b[layer], w_out[layer])
            y = y + cg1 * out_ctx
        lo, _, _ = hgrn_bidirectional(q, i, lf_f, k_f, lf_b, k_b, s_f, s_b)
        out_lat = mixer_output(lo, g, u, v, n_lat_chunks, hgrn_norm[layer], sgu_norm[layer],
                               sgu_w[layer], sgu_b[layer], w_out[layer])
        x = x + g1 * out_lat

        moe_w = (w_group[layer], b_group[layer], w_router[layer], b_router[layer],
                 w_gate[layer], w_up[layer], w_down[layer])
        h2_lat = modulate(rmsnorm(x, norm2[layer]), sh2, sc2).reshape(-1, d)
        if last:
            f_lat = hier_moe(h2_lat, *moe_w).reshape(b, l, d)
        else:
            h2_ctx = modulate(rmsnorm(y, norm2[layer]), csh2, csc2).reshape(-1, d)
            f_all = hier_moe(jnp.concatenate([h2_lat, h2_ctx], axis=0), *moe_w)
            f_lat = f_all[: b * l].reshape(b, l, d)
            y = y + cg2 * f_all[b * l:].reshape(y.shape)
        x = x + g2 * f_lat
    return rmsnorm(x, final_norm)
```

```python
import numpy as np
from contextlib import ExitStack
import concourse.bass as bass
import concourse.mybir as mybir
from concourse.bass_utils import run_bass_kernel_spmd

F32 = mybir.dt.float32
BF16 = mybir.dt.bfloat16
I32 = mybir.dt.int32
U8 = mybir.dt.uint8
AF = mybir.ActivationFunctionType
ALU = mybir.AluOpType
AX = mybir.AxisListType

D = 1024
TL = 4096
TC = 256
TOK = TL + TC
NT = 256
NTILES = TOK // NT
NSUB = TOK // 128
NCH = TOK // 64
INW = 3584
NE = 32
BLK = 256
NB = (NSUB * 2 * 128 + BLK - 1) // BLK + NE
NSB = NB * (BLK // 128)
EPS = 1e-6
CW = 1024


class _Rec:
    def __init__(self):
        self.calls = []

    def __getattr__(self, name):
        def f(*a, **k):
            self.calls.append((name, a, k))
            return self
        return f


def _bind(fn):
    rec = _Rec()
    fn(rec)
    assert len(rec.calls) == 1, rec.calls
    name, a, k = rec.calls[0]
    return lambda e: getattr(e, name)(*a, **k)


class Prog:
    ENG = ['pe', 'dve', 'act', 'pool', 'sp']
    NDS = 8

    def __init__(self, nc, stack):
        self.nc = nc
        self.q = {e: [] for e in self.ENG}
        self.cnt = {e: 0 for e in self.ENG}
        self.sems = {}
        for e in self.ENG:
            self.sems['s_' + e] = stack.enter_context(nc.semaphore('s_' + e))
        for e in ('sp', 'pool', 'act'):
            for i in range(self.NDS):
                self.sems['d_%s%d' % (e, i)] = stack.enter_context(nc.semaphore('d_%s%d' % (e, i)))
        self.dcnt = {e: 0 for e in ('sp', 'pool', 'act')}
        self.waited = {e: {} for e in self.ENG}
        self.lastw = {}
        self.readers = {}
        self.out_toks = []

    def _deps(self, reads, writes):
        deps = []
        for b in reads:
            if b in self.lastw:
                deps.append(self.lastw[b])
        for b in writes:
            if b in self.lastw:
                deps.append(self.lastw[b])
            deps.extend(self.readers.get(b, []))
        return deps

    def _wait(self, eng, tok):
        key, val, src = tok
        if src == 'pe' and eng == 'pe' and key == 's_pe':
            return
        if self.waited[eng].get(key, 0) >= val:
            return
        self.waited[eng][key] = val
        sem = self.sems[key]
        self.q[eng].append(lambda e, sem=sem, val=val: e.wait_ge(sem, val))

    def _record(self, tok, reads, writes):
        for b in reads:
            self.readers.setdefault(b, []).append(tok)
        for b in writes:
            self.lastw[b] = tok
            self.readers[b] = []

    def op(self, eng, fn, reads=(), writes=()):
        fn = _bind(fn)
        for tok in self._deps(reads, writes):
            self._wait(eng, tok)
        self.cnt[eng] += 1
        seq = self.cnt[eng]
        sem = self.sems['s_' + eng]
        self.q[eng].append(lambda e, fn=fn, sem=sem: fn(e).then_inc(sem, 1))
        tok = ('s_' + eng, seq, eng)
        self._record(tok, reads, writes)
        return tok

    def dma(self, eng, fn, reads=(), writes=(), is_out=False):
        fn = _bind(fn)
        for tok in self._deps(reads, writes):
            self._wait(eng, tok)
        k = self.dcnt[eng]
        self.dcnt[eng] += 1
        slot = k % self.NDS
        val = 16 * (k // self.NDS + 1)
        key = 'd_%s%d' % (eng, slot)
        if k >= self.NDS:
            self._wait(eng, (key, val - 16, eng))
        sem = self.sems[key]
        self.q[eng].append(lambda e, fn=fn, sem=sem: fn(e).then_inc(sem, 16))
        tok = (key, val, eng)
        self._record(tok, reads, writes)
        if is_out:
            self.out_toks.append(tok)
        return tok

    def barrier_all(self):
        toks = []
        for e in self.ENG:
            if self.cnt[e] > 0:
                toks.append(('s_' + e, self.cnt[e], e))
        for e in ('sp', 'pool', 'act'):
            k = self.dcnt[e]
            for slot in range(self.NDS):
                n = (k - slot + self.NDS - 1) // self.NDS if k > slot else 0
                if n > 0:
                    toks.append(('d_%s%d' % (e, slot), 16 * n, e))
        for e in self.ENG:
            for t in toks:
                if t[2] == 'pe' and e == 'pe' and t[0] == 's_pe':
                    pass
                key, val, src = t
                if self.waited[e].get(key, 0) >= val:
                    continue
                self.waited[e][key] = val
                sem = self.sems[key]
                self.q[e].append(lambda en, sem=sem, val=val: en.wait_ge(sem, val))
        self.lastw = {}
        self.readers = {}

    def finish(self):
        for tok in self.out_toks:
            self._wait('sp', tok)

    def replay(self, block):
        q = self.q

        @block.tensor
        def _(e):
            for f in q['pe']:
                f(e)

        @block.vector
        def _(e):
            for f in q['dve']:
                f(e)

        @block.scalar
        def _(e):
            for f in q['act']:
                f(e)

        @block.gpsimd
        def _(e):
            for f in q['pool']:
                f(e)

        @block.sync
        def _(e):
            for f in q['sp']:
                f(e)


class _Stop(Exception):
    pass


def build_nc(n_layers=2, dbg=None):
    nc = bass.Bass("TRN2", target_bir_lowering=False)

    def din(name, shape, dt=F32):
        return nc.dram_tensor(name, shape, dt, kind="ExternalInput").ap()

    def dint(name, shape, dt=F32):
        return nc.dram_tensor(name, shape, dt, kind=("ExternalOutput" if dbg else "Internal")).ap()

    xs = din("xs", [TOK, D])
    rows_in = din("rows_in", [72, 128])
    w_mod = din("w_mod", [2, D, 6 * D])
    b_mod = din("b_mod", [2, 6 * D])
    w_in = din("w_in", [2, D, INW])
    n2row = din("n2row", [2, D])
    sgn = din("sgn", [2, 512])
    sgw = din("sgw", [2, 4, 128, 128])
    sgb = din("sgb", [2, 512])
    w_out = din("w_out", [2, D, D])
    wrt = din("wrt", [2, D, 36])
    brt = din("brt", [2, 36])
    esh = [8, 8] if dbg in ("s0", "l0", "p1a", "p1", "p2", "p3", "ta", "tb", "tc", "td", "te", "tf", "tg") else [8192, 2048]
    wg2 = [din("wg2_%d" % i, esh) for i in range(2)]
    wu2 = [din("wu2_%d" % i, esh) for i in range(2)]
    wd2 = [din("wd2_%d" % i, esh) for i in range(2)]
    fnorm = din("fnorm", [1, D])
    cst = din("cst", [128, CW])
    cst8 = din("cst8", [128, 256], U8)
    out = nc.dram_tensor("out", [TL, D], F32, kind="ExternalOutput").ap()

    MODROW = dint("MODROW", [2, 2, 6 * D])
    OBF = dint("OBF", [512, TOK])
    QF = dint("QF", [512, TOK], BF16)
    GS = dint("GS", [512, TOK], BF16)
    SGD = dint("SGD", [512, TOK], BF16)
    KFT = dint("KFT", [TOK, 512], BF16)
    VT = dint("VT", [TOK, 512], BF16)
    XM = dint("XM", [TOK, D])
    H2 = dint("H2", [TOK, D], BF16)
    XB = dint("XB", [NSB * 128, D], BF16)
    YB = dint("YB", [NSB * 128, D])
    XS1 = dint("XS1", [TOK, D])

    with ExitStack() as top:
        E = top.enter_context
        P = Prog(nc, top)

        uid = [0]

        def sb(name, shape, dt=F32, st=top):
            uid[0] += 1
            return st.enter_context(nc.sbuf_tensor("%s_u%d" % (name, uid[0]), shape, dt))

        def V(fn, r=(), w=()):
            return P.op('dve', fn, r, w)

        def A(fn, r=(), w=()):
            return P.op('act', fn, r, w)

        def G(fn, r=(), w=()):
            return P.op('pool', fn, r, w)

        def T(fn, r=(), w=()):
            return P.op('pe', fn, r, w)

        def LD(fn, r=(), w=(), is_out=False):
            return P.dma('sp', fn, r, w, is_out)

        def GD(fn, r=(), w=()):
            return P.dma('pool', fn, r, w)

        cs = sb("cs", [128, CW])
        m8 = sb("m8", [128, 256], U8)
        identb = sb("identb", [128, 128], BF16)
        trib = sb("trib", [128, 128], BF16)
        onesb = sb("onesb", [128, 128], BF16)
        colsA = sb("colsA", [128, 72])
        colsM = sb("colsM", [128, 192])
        scv = sb("scv", [128, 16])
        lbc = sb("lbc", [128, 2, 8])
        oml = sb("oml", [128, 2, 8])
        lbm1 = sb("lbm1", [128, 2, 8])
        a1c = sb("a1c", [128, 2, 2, 8])
        ERF = sb("ERF", [128, 4, NCH])
        ETRF = sb("ETRF", [128, 4, NCH])
        DECF = sb("DECF", [128, 4, NCH])
        M1all = sb("M1all", [128, NSUB, NE], BF16)
        M2all = sb("M2all", [128, NSUB, NE], BF16)
        W12 = sb("W12", [128, NSUB, 2])
        fn_bc = sb("fn_bc", [128, D])
        Sst = sb("Sst", [128, 4, 128])
        Sbf = sb("Sbf", [128, 4, 4, 128], BF16)
        scb = [sb("scb%d" % i, [128, 128], BF16) for i in range(2)]
        scf = [sb("scf%d" % i, [128, 128], BF16) for i in range(2)]

        ps = [E(nc.psum_tensor("ps%d" % i, [128, 512], F32)) for i in range(7)]

        ident = cs[:, 0:128]
        tri32 = cs[:, 128:256]
        mskrow = cs[:, 256:512]
        ones32 = cs[:, 512:640]
        nblk = cs[:, 640:640 + NB]
        pcol = cs[:, 760:761]
        maskF = m8[:, 0:128]
        maskB = m8[:, 128:256]

        open_stacks = []

        def newstack():
            stx = ExitStack()
            open_stacks.append(stx)
            return stx

        def stop(tag):
            if dbg == tag:
                P.barrier_all()
                raise _Stop()

        try:
            LD(lambda e: e.dma_start(out=cs[:], in_=cst), w=["cs"])
            LD(lambda e: e.dma_start(out=m8[:], in_=cst8), w=["m8"])
            V(lambda e: e.tensor_copy(identb[:], ident), r=["cs"], w=["identb"])
            V(lambda e: e.tensor_copy(trib[:], tri32), r=["cs"], w=["trib"])
            V(lambda e: e.tensor_copy(onesb[:], ones32), r=["cs"], w=["onesb"])
            LD(lambda e: e.dma_start(out=fn_bc[:], in_=fnorm.to_broadcast([128, D])), w=["fn_bc"])
            for i in range(2):
                V(lambda e, i=i: e.memset(scb[i][:], 0.0), w=["scb%d" % i])
                V(lambda e, i=i: e.memset(scf[i][:], 0.0), w=["scf%d" % i])

            s0 = newstack()
            rowsA = sb("rowsA", [72, 128], st=s0)
            LD(lambda e: e.dma_start(out=rowsA[:], in_=rows_in), w=["rowsA"])
            T(lambda e: e.transpose(out=ps[0][:, 0:72], in_=rowsA[:], identity=cs[0:72, 0:72]), r=["rowsA", "cs"], w=["ps0"])
            V(lambda e: e.tensor_copy(colsA[:], ps[0][:, 0:72]), r=["ps0"], w=["colsA"])
            A(lambda e: e.activation(out=scv[:], in_=colsA[:, 56:72], func=AF.Silu), r=["colsA"], w=["scv"])
            V(lambda e: e.memset(lbc[:], 0.0), w=["lbc"])
            V(lambda e: e.tensor_tensor(out=lbc[:, 1, :], in0=colsA[:, 40:48], in1=colsA[:, 32:40], op=ALU.subtract), r=["colsA"], w=["lbc"])
            A(lambda e: e.activation(out=lbc[:, 1, :], in_=lbc[:, 1, :], func=AF.Sigmoid), r=["lbc"], w=["lbc"])
            V(lambda e: e.tensor_scalar(out=oml[:], in0=lbc[:], scalar1=-1.0, scalar2=1.0, op0=ALU.mult, op1=ALU.add), r=["lbc"], w=["oml"])
            V(lambda e: e.tensor_scalar(out=lbm1[:], in0=lbc[:], scalar1=-1.0, scalar2=None, op0=ALU.add), r=["lbc"], w=["lbm1"])

            wmb = [sb("wmb%d" % i, [128, 8, 512], st=s0) for i in range(2)]
            bmod_sb = sb("bmod_sb", [2, 6 * D], st=s0)
            modsb = sb("modsb", [2, 6 * D], st=s0)
            it = 0
            for l in range(2):
                LD(lambda e, l=l: e.dma_start(out=bmod_sb[:], in_=b_mod[l:l + 1, :].to_broadcast([2, 6 * D])), w=["bmod_sb"])
                for n in range(12):
                    wb = wmb[it % 2]
                    wn = "wmb%d" % (it % 2)
                    LD(lambda e, l=l, n=n, wb=wb: e.dma_start(
                        out=wb[:], in_=w_mod[l, :, n * 512:(n + 1) * 512].rearrange("(kc p) f -> p kc f", p=128)), w=[wn])
                    pb = ps[it % 2]
                    pn = "ps%d" % (it % 2)
                    for kc in range(8):
                        T(lambda e, kc=kc, wb=wb, pb=pb: e.matmul(pb[0:2, :], lhsT=scv[:, kc:16:8], rhs=wb[:, kc, :],
                                                                 start=(kc == 0), stop=(kc == 7)), r=[wn, "scv"], w=[pn])
                    V(lambda e, n=n, pb=pb: e.tensor_tensor(out=modsb[:, n * 512:(n + 1) * 512], in0=pb[0:2, :],
                                                            in1=bmod_sb[:, n * 512:(n + 1) * 512], op=ALU.add),
                      r=[pn, "bmod_sb"], w=["modsb"])
                    it += 1
                LD(lambda e, l=l: e.dma_start(out=MODROW[l], in_=modsb[:]), r=["modsb"], w=["MODROW"])
            rowsM = sb("rowsM", [96, 2, 128], st=s0)
            MR = MODROW.rearrange("l s (r q) -> l (s r) q", q=128)
            for l in range(2):
                LD(lambda e, l=l: e.dma_start(out=rowsM[:, l, :], in_=MR[l]), r=["MODROW"], w=["rowsM"])
            for l in range(2):
                T(lambda e, l=l: e.transpose(out=ps[2][:, l * 96:(l + 1) * 96], in_=rowsM[:, l, :], identity=cs[0:96, 0:96]),
                  r=["rowsM", "cs"], w=["ps2"])
            V(lambda e: e.tensor_copy(colsM[:], ps[2][:, 0:192]), r=["ps2"], w=["colsM"])

            def cm(l, s, k):
                o = l * 96 + s * 48 + k * 8
                return colsM[:, o:o + 8]

            for l in range(2):
                for s in range(2):
                    V(lambda e, l=l, s=s: e.scalar_tensor_tensor(out=a1c[:, l, s, :], in0=cm(l, s, 1), scalar=1.0,
                                                                 in1=colsA[:, l * 16:l * 16 + 8], op0=ALU.add, op1=ALU.mult),
                      r=["colsM", "colsA"], w=["a1c"])
            P.barrier_all()
            s0.close(); open_stacks.remove(s0)
            stop("s0")

            for l in range(n_layers):
                last = (l == 1)
                XIN = xs if l == 0 else XS1
                sm = newstack()
                winb = sb("winb", [128, 8, INW], BF16, st=sm)
                woutb = sb("woutb", [128, 8, D], BF16, st=sm)
                wr32 = sb("wr32", [128, 8, 36], st=sm)
                brow = sb("brow", [1, 36], st=sm)
                wsT = sb("wsT", [128, 4, 128], BF16, st=sm)
                bsrow = sb("bsrow", [1, 512], BF16, st=sm)
                sgn_bc = sb("sgn_bc", [128, 512], st=sm)
                g1_bc = sb("g1_bc", [128, 2, D], st=sm)
                a2_bc = sb("a2_bc", [128, 2, D], st=sm)
                b2_bc = sb("b2_bc", [128, 2, D], st=sm)
                for kc in range(8):
                    for hf in range(2):
                        GD(lambda e, kc=kc, hf=hf: e.dma_start(out=winb[:, kc, hf * 1792:(hf + 1) * 1792],
                                                               in_=w_in[l, kc * 128:(kc + 1) * 128, hf * 1792:(hf + 1) * 1792]),
                           w=["winb"])
                    GD(lambda e, kc=kc: e.dma_start(out=woutb[:, kc, :], in_=w_out[l, kc * 128:(kc + 1) * 128, :]), w=["woutb"])
                LD(lambda e: e.dma_start(out=wr32[:], in_=wrt[l].rearrange("(kc p) f -> p kc f", p=128)), w=["wr32"])
                LD(lambda e: e.dma_start(out=brow[:], in_=brt[l:l + 1, :]), w=["brow"])
                GD(lambda e: e.dma_start(out=bsrow[:], in_=sgb[l:l + 1, :]), w=["bsrow"])
                LD(lambda e: e.dma_start(out=sgn_bc[:], in_=sgn[l:l + 1, :].to_broadcast([128, 512])), w=["sgn_bc"])
                sl = newstack()
                wsn = sb("wsn", [128, 4, 128], st=sl)
                LD(lambda e: e.dma_start(out=wsn[:], in_=sgw[l].rearrange("h i j -> i h j")), w=["wsn"])
                for h in range(4):
                    T(lambda e, h=h: e.transpose(out=ps[0][:, h * 128:(h + 1) * 128], in_=wsn[:, h, :], identity=ident),
                      r=["wsn", "cs"], w=["ps0"])
                V(lambda e: e.tensor_copy(wsT[:].rearrange("p h i -> p (h i)"), ps[0][:]), r=["ps0"], w=["wsT"])
                tmpb = sb("tmpb", [128, D], st=sl)
                for s in range(2):
                    LD(lambda e, s=s: e.dma_start(out=g1_bc[:, s, :], in_=MODROW[l, s:s + 1, 2 * D:3 * D].to_broadcast([128, D])), w=["g1_bc"])
                    LD(lambda e, s=s: e.dma_start(out=b2_bc[:, s, :], in_=MODROW[l, s:s + 1, 3 * D:4 * D].to_broadcast([128, D])), w=["b2_bc"])
                    LD(lambda e, s=s: e.dma_start(out=a2_bc[:, s, :], in_=MODROW[l, s:s + 1, 4 * D:5 * D].to_broadcast([128, D])), w=["a2_bc"])
                LD(lambda e: e.dma_start(out=tmpb[:], in_=n2row[l:l + 1, :].to_broadcast([128, D])), w=["tmpb"])
                for s in range(2):
                    V(lambda e, s=s: e.scalar_tensor_tensor(out=a2_bc[:, s, :], in0=a2_bc[:, s, :], scalar=1.0, in1=tmpb[:],
                                                            op0=ALU.add, op1=ALU.mult), r=["a2_bc", "tmpb"], w=["a2_bc"])
                P.barrier_all()
                sl.close(); open_stacks.remove(sl)
                stop("l0")

                xt = [sb("xt%d" % i, [128, 2, D], st=sm) for i in range(2)]
                hT = sb("hT", [128, 8, NT], BF16, st=sm)
                q32 = sb("q32", [128, 4, NT], st=sm)
                sgt = [sb("sgt%d" % i, [128, NT], st=sm) for i in range(2)]
                lft = [sb("lft%d" % i, [128, NT], st=sm) for i in range(2)]
                kkt = [sb("kkt%d" % i, [128, NT], st=sm) for i in range(2)]
                Att = [sb("Att%d" % i, [128, NT], st=sm) for i in range(2)]
                e1t = [sb("e1t%d" % i, [128, NT], st=sm) for i in range(2)]
                qtf = sb("qtf", [128, 4, NT], BF16, st=sm)
                ktf = sb("ktf", [128, 4, NT], BF16, st=sm)
                qtb = sb("qtb", [128, 4, NT], BF16, st=sm)
                ktb = sb("ktb", [128, 4, NT], BF16, st=sm)
                gsb = sb("gsb", [128, 4, NT], BF16, st=sm)
                ug = sb("ug", [128, 4, NT], BF16, st=sm)
                mixT = sb("mixT", [128, 8, NT], BF16, st=sm)
                vtok = sb("vtok", [128, 2, 512], BF16, st=sm)
                vg = sb("vg", [128, 512], st=sm)
                vntok = sb("vntok", [128, 2, 512], BF16, st=sm)
                kbtok = sb("kbtok", [128, 2, 512], BF16, st=sm)
                kftok = sb("kftok", [128, 2, 512], BF16, st=sm)
                obf = sb("obf", [128, 4, NT], st=sm)
                ofull = obf
                sqt = sb("sqt", [128, NT], st=sm)
                rt = sb("rt", [128, NT], st=sm)
                mt = sb("mt", [128, NT], st=sm)
                stat = sb("stat", [128, 8], st=sm)
                erb = sb("erb", [128, 4, 4], st=sm)
                etrb = sb("etrb", [128, 4, 4], st=sm)
                decb = sb("decb", [128, 4, 4], st=sm)
                dtmp = sb("dtmp", [128, 4, 4], st=sm)
                utmp = sb("utmp", [128, 4, 128], st=sm)
                tmp2 = sb("tmp2", [128, 512], st=sm)
                h2t = sb("h2t", [128, D], st=sm)
                junk = h2t
                h2T = sb("h2T", [128, 8, 128], st=sm)
                Ls = sb("Ls", [128, 36], st=sm)
                rsm = sb("rsm", [128, 64], st=sm)

                OBFv = OBF.rearrange("(h p) t -> p h t", p=128)
                QFv = QF.rearrange("(h p) t -> p h t", p=128)
                GSv = GS.rearrange("(h p) t -> p h t", p=128)
                SGv = SGD.rearrange("(h p) t -> p h t", p=128)
                KFTv = KFT.rearrange("(n p) f -> p n f", p=128)
                VTv = VT.rearrange("(n p) f -> p n f", p=128)
                XINv = XIN.rearrange("(n p) f -> p n f", p=128)
                XMv = XM.rearrange("(n p) f -> p n f", p=128)
                H2v = H2.rearrange("(n p) f -> p n f", p=128)

                def rstd_from_ss(ssap, rap, n, inv):
                    V(lambda e: e.tensor_scalar(out=rap, in0=ssap, scalar1=inv, scalar2=EPS, op0=ALU.mult, op1=ALU.add),
                      r=["stat"], w=["stat"])
                    A(lambda e: e.activation(out=rap, in_=rap, func=AF.Sqrt), r=["stat"], w=["stat"])
                    V(lambda e: e.reciprocal(out=rap, in_=rap), r=["stat"], w=["stat"])

                def load_x(ti, bi):
                    LD(lambda e: e.dma_start(out=xt[bi][:], in_=XINv[:, 2 * ti:2 * ti + 2, :]), r=["XIN%d" % ti], w=["xt%d" % bi])

                def norm_to_hT(ti, bi, s):
                    x = xt[bi]
                    xn = "xt%d" % bi
                    for j in range(2):
                        A(lambda e, j=j: e.activation(out=junk[:], in_=x[:, j, :], func=AF.Square, accum_out=stat[:, j:j + 1]),
                          r=[xn], w=["h2t", "stat"])
                    rstd_from_ss(stat[:, 0:2], stat[:, 2:4], 2, 1.0 / D)
                    for j in range(2):
                        V(lambda e, j=j: e.tensor_scalar(out=x[:, j, :], in0=x[:, j, :], scalar1=stat[:, 2 + j:3 + j], scalar2=None,
                                                         op0=ALU.mult), r=[xn, "stat"], w=[xn])
                    for c in range(8):
                        pb = ps[2 + (c % 2)]
                        pn = "ps%d" % (2 + (c % 2))
                        for j in range(2):
                            T(lambda e, c=c, j=j, pb=pb: e.transpose(out=pb[:, j * 128:(j + 1) * 128], in_=x[:, j, c * 128:(c + 1) * 128],
                                                                     identity=ident), r=[xn, "cs"], w=[pn])
                        if c % 2 == 0:
                            V(lambda e, c=c, pb=pb: e.tensor_scalar(out=hT[:, c, :], in0=pb[:, 0:NT], scalar1=a1c[:, l, s, c:c + 1],
                                                                    scalar2=cm(l, s, 0)[:, c:c + 1], op0=ALU.mult, op1=ALU.add),
                              r=[pn, "a1c", "colsM"], w=["hT"])
                        else:
                            A(lambda e, c=c, pb=pb: e.activation(out=hT[:, c, :], in_=pb[:, 0:NT], func=AF.Identity,
                                                                 bias=cm(l, s, 0)[:, c:c + 1], scale=a1c[:, l, s, c:c + 1]),
                              r=[pn, "a1c", "colsM"], w=["hT"])

                pj = [0]

                def proj_fm(m):
                    k = pj[0] % 2
                    pj[0] += 1
                    pb = ps[k]
                    for c in range(8):
                        T(lambda e, c=c, pb=pb: e.matmul(pb[:, 0:NT], lhsT=winb[:, c, m * 128:(m + 1) * 128], rhs=hT[:, c, :],
                                                         start=(c == 0), stop=(c == 7)), r=["winb", "hT"], w=["ps%d" % k])
                    return pb, "ps%d" % k

                def proj_tm(j, col0):
                    k = pj[0] % 2
                    pj[0] += 1
                    pb = ps[k]
                    for c in range(8):
                        T(lambda e, c=c, pb=pb: e.matmul(pb[:, :], lhsT=hT[:, c, j * 128:(j + 1) * 128], rhs=winb[:, c, col0:col0 + 512],
                                                         start=(c == 0), stop=(c == 7)), r=["winb", "hT"], w=["ps%d" % k])
                    return pb, "ps%d" % k

                gi = [0]

                def gates(h, dr, ti):
                    k = gi[0] % 2
                    gi[0] += 1
                    pb, pn = proj_fm(4 + 4 * dr + h)
                    sg_, lf_, kk_, I_, A_, e1_, e2_ = sgt[k], lft[k], kkt[k], sgt[k], Att[k], e1t[k], Att[k]
                    nm = ["sgt%d" % k, "lft%d" % k, "kkt%d" % k, "sgt%d" % k, "Att%d" % k, "e1t%d" % k, "Att%d" % k]
                    ci = dr * 4 + h
                    A(lambda e: e.activation(out=sg_[:], in_=pb[:, 0:NT], func=AF.Sigmoid), r=[pn], w=[nm[0]])
                    A(lambda e: e.activation(out=lf_[:], in_=sg_[:], func=AF.Ln, bias=lbc[:, l, ci:ci + 1], scale=oml[:, l, ci:ci + 1]),
                      r=[nm[0], "lbc", "oml"], w=[nm[1]])
                    V(lambda e: e.tensor_scalar(out=kk_[:], in0=sg_[:], scalar1=-1.0, scalar2=lbm1[:, l, ci:ci + 1], op0=ALU.add, op1=ALU.mult),
                      r=[nm[0], "lbm1"], w=[nm[2]])
                    V(lambda e: e.tensor_tensor_scan(out=I_[:], data0=mskrow, data1=lf_[:], initial=0.0, op0=ALU.mult, op1=ALU.add),
                      r=[nm[1], "cs"], w=[nm[3]])
                    I3 = I_[:].rearrange("p (c t) -> p c t", t=64)
                    A3 = A_[:].rearrange("p (c t) -> p c t", t=64)
                    if dr == 0:
                        V(lambda e: e.tensor_tensor(out=A3, in0=I3, in1=I3[:, :, 31:32].to_broadcast([128, 4, 64]), op=ALU.subtract),
                          r=[nm[3]], w=[nm[4]])
                        A(lambda e: e.activation(out=e1_[:], in_=A_[:], func=AF.Exp), r=[nm[4]], w=[nm[5]])
                        A(lambda e: e.activation(out=e2_[:], in_=A_[:], func=AF.Exp, scale=-1.0), r=[nm[4]], w=[nm[6]])
                        G(lambda e: e.tensor_tensor(out=qtf[:, h, :], in0=q32[:, h, :], in1=e1_[:], op=ALU.mult), r=["q32", nm[5]], w=["qtf"])
                        G(lambda e: e.tensor_tensor(out=ktf[:, h, :], in0=kk_[:], in1=e2_[:], op=ALU.mult), r=[nm[2], nm[6]], w=["ktf"])
                        gc = ti * 4
                        A(lambda e: e.activation(out=ERF[:, h, gc:gc + 4], in_=I3[:, :, 31], func=AF.Exp), r=[nm[3]], w=["ERF"])
                        A(lambda e: e.activation(out=DECF[:, h, gc:gc + 4], in_=I3[:, :, 63], func=AF.Exp), r=[nm[3]], w=["DECF"])
                        V(lambda e: e.tensor_tensor(out=dtmp[:, h, :], in0=I3[:, :, 63], in1=I3[:, :, 31], op=ALU.subtract), r=[nm[3]], w=["dtmp"])
                        A(lambda e: e.activation(out=ETRF[:, h, gc:gc + 4], in_=dtmp[:, h, :], func=AF.Exp), r=["dtmp"], w=["ETRF"])
                    else:
                        V(lambda e: e.tensor_tensor(out=lf_[:], in0=I_[:], in1=lf_[:], op=ALU.subtract), r=[nm[3], nm[1]], w=[nm[1]])
                        E3 = lf_[:].rearrange("p (c t) -> p c t", t=64)
                        V(lambda e: e.tensor_tensor(out=A3, in0=E3, in1=E3[:, :, 32:33].to_broadcast([128, 4, 64]), op=ALU.subtract),
                          r=[nm[1]], w=[nm[4]])
                        A(lambda e: e.activation(out=e1_[:], in_=A_[:], func=AF.Exp, scale=-1.0), r=[nm[4]], w=[nm[5]])
                        A(lambda e: e.activation(out=e2_[:], in_=A_[:], func=AF.Exp), r=[nm[4]], w=[nm[6]])
                        G(lambda e: e.tensor_tensor(out=qtb[:, h, :], in0=q32[:, h, :], in1=e1_[:], op=ALU.mult), r=["q32", nm[5]], w=["qtb"])
                        G(lambda e: e.tensor_tensor(out=ktb[:, h, :], in0=kk_[:], in1=e2_[:], op=ALU.mult), r=[nm[2], nm[6]], w=["ktb"])
                        A(lambda e: e.activation(out=etrb[:, h, :], in_=E3[:, :, 32], func=AF.Exp), r=[nm[1]], w=["etrb"])
                        A(lambda e: e.activation(out=decb[:, h, :], in_=I3[:, :, 63], func=AF.Exp), r=[nm[3]], w=["decb"])
                        V(lambda e: e.tensor_tensor(out=dtmp[:, h, :], in0=I3[:, :, 63], in1=E3[:, :, 32], op=ALU.subtract), r=[nm[3], nm[1]], w=["dtmp"])
                        A(lambda e: e.activation(out=erb[:, h, :], in_=dtmp[:, h, :], func=AF.Exp), r=["dtmp"], w=["erb"])

                def state_step(c, j, cc, ktok, ktn, er_ap, etr_ap, dec_ap, kU, scn):
                    pb = ps[4 + kU % 2]
                    pn = "ps%d" % (4 + kU % 2)
                    for h in range(4):
                        T(lambda e, h=h, pb=pb: e.matmul(pb[:, h * 128:(h + 1) * 128],
                                                         lhsT=ktok[cc * 64:(cc + 1) * 64, j, h * 128:(h + 1) * 128],
                                                         rhs=vtok[cc * 64:(cc + 1) * 64, j, h * 128:(h + 1) * 128], start=True, stop=True),
                          r=[ktn, "vtok"], w=[pn])
                    V(lambda e: e.tensor_tensor(out=Sbf[:, c, :, :], in0=Sst[:], in1=er_ap.to_broadcast([128, 4, 128]), op=ALU.mult),
                      r=["Sst"] + scn, w=["Sbf%d" % c])
                    V(lambda e, pb=pb: e.tensor_tensor(out=utmp[:], in0=pb[:].rearrange("p (h e) -> p h e", e=128),
                                                       in1=etr_ap.to_broadcast([128, 4, 128]), op=ALU.mult), r=[pn] + scn, w=["utmp"])
                    V(lambda e: e.tensor_tensor(out=Sst[:], in0=Sst[:], in1=dec_ap.to_broadcast([128, 4, 128]), op=ALU.mult),
                      r=["Sst"] + scn, w=["Sst"])
                    V(lambda e: e.tensor_tensor(out=Sst[:], in0=Sst[:], in1=utmp[:], op=ALU.add), r=["Sst", "utmp"], w=["Sst"])

                ku = [0]

                V(lambda e: e.memset(Sst[:], 0.0), w=["Sst"])
                order1 = [0] + list(range(NTILES - 1, 0, -1))
                load_x(order1[0], 0)
                for oi, ti in enumerate(order1):
                    bi = oi % 2
                    s = 1 if ti == 0 else 0
                    if oi + 1 < len(order1):
                        load_x(order1[oi + 1], 1 - bi)
                    norm_to_hT(ti, bi, s)
                    stop("ta")
                    for j in range(2):
                        pb, pn = proj_tm(j, 1536)
                        A(lambda e, j=j, pb=pb: e.activation(out=vtok[:, j, :], in_=pb[:, :], func=AF.Copy), r=[pn], w=["vtok"])
                        pb, pn = proj_tm(j, 3072)
                        A(lambda e, pb=pb: e.activation(out=vg[:], in_=pb[:, :], func=AF.Gelu_apprx_tanh), r=[pn], w=["vg"])
                        A(lambda e, j=j: e.activation(out=junk[:, 0:512], in_=vg[:], func=AF.Square, accum_out=stat[:, 4 + j:5 + j]),
                          r=["vg"], w=["h2t", "stat"])
                        rstd_from_ss(stat[:, 4 + j:5 + j], stat[:, 6 + j:7 + j], 1, 1.0 / 512)
                        V(lambda e, j=j: e.scalar_tensor_tensor(out=vntok[:, j, :], in0=vg[:], scalar=stat[:, 6 + j:7 + j], in1=sgn_bc[:],
                                                                op0=ALU.mult, op1=ALU.mult), r=["vg", "stat", "sgn_bc"], w=["vntok"])
                    stop("tb")
                    for h in range(4):
                        pb, pn = proj_fm(h)
                        A(lambda e, h=h, pb=pb: e.activation(out=q32[:, h, :], in_=pb[:, 0:NT], func=AF.Copy), r=[pn], w=["q32"])
                    for h in range(4):
                        gates(h, 0, ti)
                        gates(h, 1, ti)
                    for h in range(4):
                        pb, pn = proj_fm(16 + h)
                        A(lambda e, h=h, pb=pb: e.activation(out=gsb[:, h, :], in_=pb[:, 0:NT], func=AF.Silu), r=[pn], w=["gsb"])
                        pb, pn = proj_fm(20 + h)
                        A(lambda e, h=h, pb=pb: e.activation(out=ug[:, h, :], in_=pb[:, 0:NT], func=AF.Gelu_apprx_tanh), r=[pn], w=["ug"])
                    stop("tc")
                    for j in range(2):
                        pb = ps[2 + j]
                        pn = "ps%d" % (2 + j)
                        for h in range(4):
                            T(lambda e, h=h, j=j, pb=pb: e.matmul(pb[:, h * 128:(h + 1) * 128], lhsT=vntok[:, j, h * 128:(h + 1) * 128],
                                                                  rhs=wsT[:, h, :], start=True, stop=False), r=["vntok", "wsT"], w=[pn])
                            T(lambda e, h=h, pb=pb: e.matmul(pb[:, h * 128:(h + 1) * 128], lhsT=onesb[0:1, :],
                                                             rhs=bsrow[0:1, h * 128:(h + 1) * 128], start=False, stop=True),
                              r=["onesb", "bsrow"], w=[pn])
                        V(lambda e, j=j, pb=pb: e.tensor_tensor(out=mixT[:, 4:8, j * 128:(j + 1) * 128],
                                                                in0=pb[:].rearrange("p (h i) -> p h i", i=128),
                                                                in1=ug[:, :, j * 128:(j + 1) * 128], op=ALU.mult), r=[pn, "ug"], w=["mixT"])
                    stop("td")
                    for j in range(2):
                        for h in range(4):
                            T(lambda e, h=h, j=j: e.matmul(ps[2][:, h * 128:(h + 1) * 128], lhsT=ktb[:, h, j * 128:(j + 1) * 128],
                                                           rhs=identb[:], start=True, stop=True), r=["ktb", "identb"], w=["ps2"])
                            T(lambda e, h=h, j=j: e.matmul(ps[3][:, h * 128:(h + 1) * 128], lhsT=ktf[:, h, j * 128:(j + 1) * 128],
                                                           rhs=identb[:], start=True, stop=True), r=["ktf", "identb"], w=["ps3"])
                        A(lambda e, j=j: e.activation(out=kbtok[:, j, :], in_=ps[2][:, :], func=AF.Copy), r=["ps2"], w=["kbtok"])
                        V(lambda e, j=j: e.tensor_copy(kftok[:, j, :], ps[3][:, :]), r=["ps3"], w=["kftok"])
                    stop("te")
                    for c in (3, 2, 1, 0):
                        j, cc = c // 2, c % 2
                        state_step(c, j, cc, kbtok, "kbtok", erb[:, :, c:c + 1], etrb[:, :, c:c + 1], decb[:, :, c:c + 1], ku[0], ["erb", "etrb", "decb"])
                        ku[0] += 1
                    stop("tf")
                    for j in (1, 0):
                        for h in range(4):
                            k = h % 2
                            off = k * 256
                            jc = slice(j * 128, (j + 1) * 128)
                            T(lambda e, h=h, jc=jc, off=off: e.matmul(ps[6][:, off:off + 128], lhsT=ktb[:, h, jc], rhs=qtb[:, h, jc],
                                                                      start=True, stop=True), r=["ktb", "qtb"], w=["ps6_%d" % k])
                            T(lambda e, h=h, jc=jc, off=off: e.matmul(ps[6][:, off + 128:off + 256], lhsT=ktf[:, h, jc], rhs=qtf[:, h, jc],
                                                                      start=True, stop=True), r=["ktf", "qtf"], w=["ps6_%d" % k])
                            V(lambda e, k=k, off=off: e.copy_predicated(out=scb[k][:], mask=maskB, data=ps[6][:, off:off + 128]),
                              r=["ps6_%d" % k, "m8"], w=["scb%d" % k])
                            V(lambda e, k=k, off=off: e.copy_predicated(out=scf[k][:], mask=maskF, data=ps[6][:, off + 128:off + 256]),
                              r=["ps6_%d" % k, "m8"], w=["scf%d" % k])
                            oo = ps[5][:, h * 128:(h + 1) * 128]
                            T(lambda e, h=h, j=j, k=k, oo=oo: e.matmul(oo, lhsT=vtok[:, j, h * 128:(h + 1) * 128], rhs=scb[k][:],
                                                                       start=True, stop=False), r=["vtok", "scb%d" % k], w=["ps5"])
                            T(lambda e, h=h, j=j, k=k, oo=oo: e.matmul(oo, lhsT=vtok[:, j, h * 128:(h + 1) * 128], rhs=scf[k][:],
                                                                       start=False, stop=False), r=["vtok", "scf%d" % k], w=["ps5"])
                            for cc in (1, 0):
                                c = 2 * j + cc
                                T(lambda e, h=h, c=c, cc=cc, j=j: e.matmul(ps[5][:, h * 128 + cc * 64:h * 128 + (cc + 1) * 64],
                                                                           lhsT=Sbf[:, c, h, :],
                                                                           rhs=qtb[:, h, j * 128 + cc * 64:j * 128 + (cc + 1) * 64],
                                                                           start=False, stop=(cc == 0)), r=["Sbf%d" % c, "qtb"], w=["ps5"])
                        A(lambda e, j=j: e.activation(out=obf[:, :, j * 128:(j + 1) * 128], in_=ps[5][:].rearrange("p (h i) -> p h i", i=128),
                                                      func=AF.Copy), r=["ps5"], w=["obf"])
                    stop("tg")
                    t0 = ti * NT
                    LD(lambda e, t0=t0: e.dma_start(out=OBFv[:, :, t0:t0 + NT], in_=obf[:]), r=["obf"], w=["OBF%d" % ti])
                    LD(lambda e, t0=t0: e.dma_start(out=QFv[:, :, t0:t0 + NT], in_=qtf[:]), r=["qtf"], w=["QF%d" % ti])
                    LD(lambda e, t0=t0: e.dma_start(out=GSv[:, :, t0:t0 + NT], in_=gsb[:]), r=["gsb"], w=["GS%d" % ti])
                    LD(lambda e, t0=t0: e.dma_start(out=SGv[:, :, t0:t0 + NT], in_=mixT[:, 4:8, :]), r=["mixT"], w=["SG%d" % ti])
                    LD(lambda e, ti=ti: e.dma_start(out=KFTv[:, 2 * ti:2 * ti + 2, :], in_=kftok[:]), r=["kftok"], w=["KFT%d" % ti])
                    LD(lambda e, ti=ti: e.dma_start(out=VTv[:, 2 * ti:2 * ti + 2, :], in_=vtok[:]), r=["vtok"], w=["VT%d" % ti])
                    stop("p1a")

                stop("p1")
                V(lambda e: e.memset(Sst[:], 0.0), w=["Sst"])

                def load2(ti, bi):
                    t0 = ti * NT
                    load_x(ti, bi)

                load_x(0, 0)
                for ti in range(NTILES):
                    bi = ti % 2
                    s = 1 if ti == 0 else 0
                    t0 = ti * NT
                    x = xt[bi]
                    xn = "xt%d" % bi
                    LD(lambda e, t0=t0: e.dma_start(out=obf[:], in_=OBFv[:, :, t0:t0 + NT]), r=["OBF%d" % ti], w=["obf"])
                    LD(lambda e, t0=t0: e.dma_start(out=qtf[:], in_=QFv[:, :, t0:t0 + NT]), r=["QF%d" % ti], w=["qtf"])
                    LD(lambda e, t0=t0: e.dma_start(out=gsb[:], in_=GSv[:, :, t0:t0 + NT]), r=["GS%d" % ti], w=["gsb"])
                    LD(lambda e, t0=t0: e.dma_start(out=mixT[:, 4:8, :], in_=SGv[:, :, t0:t0 + NT]), r=["SG%d" % ti], w=["mixT"])
                    LD(lambda e, ti=ti: e.dma_start(out=kftok[:], in_=KFTv[:, 2 * ti:2 * ti + 2, :]), r=["KFT%d" % ti], w=["kftok"])
                    LD(lambda e, ti=ti: e.dma_start(out=vtok[:], in_=VTv[:, 2 * ti:2 * ti + 2, :]), r=["VT%d" % ti], w=["vtok"])
                    if ti + 1 < NTILES:
                        load_x(ti + 1, 1 - bi)
                    gc = ti * 4
                    for c in range(4):
                        j, cc = c // 2, c % 2
                        state_step(c, j, cc, kftok, "kftok", ERF[:, :, gc + c:gc + c + 1], ETRF[:, :, gc + c:gc + c + 1],
                                   DECF[:, :, gc + c:gc + c + 1], ku[0], ["ERF", "ETRF", "DECF"])
                        ku[0] += 1
                    for j in range(2):
                        for h in range(4):
                            for cc in range(2):
                                c = 2 * j + cc
                                T(lambda e, h=h, c=c, cc=cc, j=j: e.matmul(ps[5][:, h * 128 + cc * 64:h * 128 + (cc + 1) * 64],
                                                                           lhsT=Sbf[:, c, h, :],
                                                                           rhs=qtf[:, h, j * 128 + cc * 64:j * 128 + (cc + 1) * 64],
                                                                           start=True, stop=True), r=["Sbf%d" % c, "qtf"], w=["ps5"])
                        V(lambda e, j=j: e.tensor_tensor(out=ofull[:, :, j * 128:(j + 1) * 128], in0=ps[5][:].rearrange("p (h i) -> p h i", i=128),
                                                         in1=obf[:, :, j * 128:(j + 1) * 128], op=ALU.add), r=["ps5", "obf"], w=["obf"])
                    for h in range(4):
                        A(lambda e, h=h: e.activation(out=sqt[:], in_=ofull[:, h, :], func=AF.Square), r=["obf"], w=["sqt"])
                        T(lambda e: e.matmul(ps[6][:, 0:NT], lhsT=ones32, rhs=sqt[:], start=True, stop=True), r=["sqt", "cs"], w=["ps6_0", "ps6_1"])
                        V(lambda e: e.tensor_scalar(out=rt[:], in0=ps[6][:, 0:NT], scalar1=1.0 / 128, scalar2=EPS, op0=ALU.mult, op1=ALU.add),
                          r=["ps6_0", "ps6_1"], w=["rt"])
                        A(lambda e: e.activation(out=rt[:], in_=rt[:], func=AF.Sqrt), r=["rt"], w=["rt"])
                        V(lambda e: e.reciprocal(out=rt[:], in_=rt[:]), r=["rt"], w=["rt"])
                        V(lambda e, h=h: e.scalar_tensor_tensor(out=mt[:], in0=ofull[:, h, :], scalar=colsA[:, 48 + l * 4 + h:49 + l * 4 + h],
                                                                in1=rt[:], op0=ALU.mult, op1=ALU.mult), r=["obf", "rt", "colsA"], w=["mt"])
                        G(lambda e, h=h: e.tensor_tensor(out=mixT[:, h, :], in0=mt[:], in1=gsb[:, h, :], op=ALU.mult), r=["mt", "gsb"], w=["mixT"])
                    for j in range(2):
                        for hf in range(2):
                            k = pj[0] % 2
                            pj[0] += 1
                            pb = ps[k]
                            pn = "ps%d" % k
                            for c in range(8):
                                T(lambda e, c=c, j=j, hf=hf, pb=pb: e.matmul(pb[:, :], lhsT=mixT[:, c, j * 128:(j + 1) * 128],
                                                                             rhs=woutb[:, c, hf * 512:(hf + 1) * 512],
                                                                             start=(c == 0), stop=(c == 7)), r=["mixT", "woutb"], w=[pn])
                            V(lambda e, hf=hf, pb=pb: e.tensor_tensor(out=tmp2[:], in0=pb[:, :], in1=g1_bc[:, s, hf * 512:(hf + 1) * 512],
                                                                      op=ALU.mult), r=[pn, "g1_bc"], w=["tmp2"])
                            V(lambda e, j=j, hf=hf: e.tensor_tensor(out=x[:, j, hf * 512:(hf + 1) * 512], in0=tmp2[:],
                                                                    in1=x[:, j, hf * 512:(hf + 1) * 512], op=ALU.add), r=["tmp2", xn], w=[xn])
                    LD(lambda e, ti=ti: e.dma_start(out=XMv[:, 2 * ti:2 * ti + 2, :], in_=x[:]), r=[xn], w=["XM%d" % ti])
                    for j in range(2):
                        A(lambda e, j=j: e.activation(out=junk[:], in_=x[:, j, :], func=AF.Square, accum_out=stat[:, j:j + 1]),
                          r=[xn], w=["h2t", "stat"])
                    rstd_from_ss(stat[:, 0:2], stat[:, 2:4], 2, 1.0 / D)
                    for j in range(2):
                        sub = 2 * ti + j
                        V(lambda e, j=j: e.scalar_tensor_tensor(out=h2t[:], in0=x[:, j, :], scalar=stat[:, 2 + j:3 + j], in1=a2_bc[:, s, :],
                                                                op0=ALU.mult, op1=ALU.mult), r=[xn, "stat", "a2_bc"], w=["h2t"])
                        G(lambda e: e.tensor_tensor(out=h2t[:], in0=h2t[:], in1=b2_bc[:, s, :], op=ALU.add), r=["h2t", "b2_bc"], w=["h2t"])
                        GD(lambda e, sub=sub: e.dma_start(out=H2v[:, sub, :], in_=h2t[:]), r=["h2t"], w=["H2_%d" % sub])
                        for c in range(8):
                            pb = ps[2 + c // 4]
                            pn = "ps%d" % (2 + c // 4)
                            T(lambda e, c=c, pb=pb: e.transpose(out=pb[:, (c % 4) * 128:(c % 4 + 1) * 128], in_=h2t[:, c * 128:(c + 1) * 128],
                                                                identity=ident), r=["h2t", "cs"], w=[pn])
                        A(lambda e: e.activation(out=h2T[:, 0:4, :], in_=ps[2][:].rearrange("p (c t) -> p c t", t=128), func=AF.Copy),
                          r=["ps2"], w=["h2T"])
                        A(lambda e: e.activation(out=h2T[:, 4:8, :], in_=ps[3][:].rearrange("p (c t) -> p c t", t=128), func=AF.Copy),
                          r=["ps3"], w=["h2T"])
                        for c in range(8):
                            T(lambda e, c=c: e.matmul(ps[4][:, 0:36], lhsT=h2T[:, c, :], rhs=wr32[:, c, :], start=(c == 0), stop=False),
                              r=["h2T", "wr32"], w=["ps4"])
                        T(lambda e: e.matmul(ps[4][:, 0:36], lhsT=ones32[0:1, :], rhs=brow[0:1, :], start=False, stop=True),
                          r=["cs", "brow"], w=["ps4"])
                        V(lambda e: e.tensor_copy(Ls[:], ps[4][:, 0:36]), r=["ps4"], w=["Ls"])
                        R_ = ["rsm"]
                        V(lambda e: e.tensor_reduce(out=rsm[:, 0:1], in_=Ls[:, 0:4], axis=AX.X, op=ALU.max), r=["Ls"], w=R_)
                        V(lambda e: e.tensor_scalar(out=rsm[:, 1:2], in0=rsm[:, 0:1], scalar1=-1.0, scalar2=None, op0=ALU.mult), r=R_, w=R_)
                        A(lambda e: e.activation(out=rsm[:, 48:52], in_=Ls[:, 0:4], func=AF.Exp, bias=rsm[:, 1:2], accum_out=rsm[:, 2:3]),
                          r=["Ls"] + R_, w=R_)
                        V(lambda e: e.reciprocal(out=rsm[:, 3:4], in_=rsm[:, 2:3]), r=R_, w=R_)
                        V(lambda e: e.tensor_scalar(out=rsm[:, 4:8], in0=Ls[:, 0:4], scalar1=rsm[:, 0:1], scalar2=None, op0=ALU.is_equal),
                          r=["Ls"] + R_, w=R_)
                        V(lambda e: e.tensor_scalar(out=rsm[:, 8:16], in0=Ls[:, 4:12], scalar1=rsm[:, 4:5], scalar2=None, op0=ALU.mult),
                          r=["Ls"] + R_, w=R_)
                        for g in range(1, 4):
                            V(lambda e, g=g: e.scalar_tensor_tensor(out=rsm[:, 8:16], in0=Ls[:, 4 + 8 * g:12 + 8 * g], scalar=rsm[:, 4 + g:5 + g],
                                                                    in1=rsm[:, 8:16], op0=ALU.mult, op1=ALU.add), r=["Ls"] + R_, w=R_)
                        V(lambda e: e.max(out=rsm[:, 16:24], in_=rsm[:, 8:16]), r=R_, w=R_)
                        V(lambda e: e.tensor_scalar(out=rsm[:, 24:32], in0=rsm[:, 8:16], scalar1=rsm[:, 16:17], scalar2=None, op0=ALU.is_equal), r=R_, w=R_)
                        V(lambda e: e.tensor_scalar(out=rsm[:, 32:40], in0=rsm[:, 8:16], scalar1=rsm[:, 17:18], scalar2=None, op0=ALU.is_equal), r=R_, w=R_)
                        V(lambda e: e.tensor_tensor(out=rsm[:, 40:41], in0=rsm[:, 16:17], in1=rsm[:, 17:18], op=ALU.subtract), r=R_, w=R_)
                        A(lambda e: e.activation(out=rsm[:, 41:42], in_=rsm[:, 40:41], func=AF.Sigmoid), r=R_, w=R_)
                        V(lambda e, sub=sub: e.tensor_tensor(out=W12[:, sub, 0:1], in0=rsm[:, 41:42], in1=rsm[:, 3:4], op=ALU.mult), r=R_, w=["W12"])
                        V(lambda e, sub=sub: e.tensor_tensor(out=W12[:, sub, 1:2], in0=rsm[:, 3:4], in1=W12[:, sub, 0:1], op=ALU.subtract),
                          r=R_ + ["W12"], w=["W12"])
                        for g in range(4):
                            V(lambda e, g=g, sub=sub: e.tensor_scalar(out=M1all[:, sub, g * 8:(g + 1) * 8], in0=rsm[:, 24:32],
                                                                      scalar1=rsm[:, 4 + g:5 + g], scalar2=None, op0=ALU.mult), r=R_, w=["M1all"])
                            V(lambda e, g=g, sub=sub: e.tensor_scalar(out=M2all[:, sub, g * 8:(g + 1) * 8], in0=rsm[:, 32:40],
                                                                      scalar1=rsm[:, 4 + g:5 + g], scalar2=None, op0=ALU.mult), r=R_, w=["M2all"])
                P.barrier_all()
                sm.close(); open_stacks.remove(sm)

                stop("p2")
                se = newstack()
                pre = sb("pre", [128, NE], st=se)
                cnt_i = sb("cnt_i", [128, NE], I32, st=se)
                padf = sb("padf", [128, NE], st=se)
                incl = sb("incl", [128, NE], st=se)
                offs = sb("offs", [128, NE], st=se)
                cmp3 = sb("cmp3", [128, NB, NE], st=se)
                bef = sb("bef", [128, NB], st=se)
                tmpr = sb("tmpr", [128, NE], st=se)
                tmpr2 = sb("tmpr2", [128, NE], st=se)
                hrow = [sb("hrow%d" % i, [128, D], BF16, st=se) for i in range(2)]
                wgb = [sb("wgb%d" % i, [128, 4096], BF16, st=se) for i in range(2)]
                wub = [sb("wub%d" % i, [128, 4096], BF16, st=se) for i in range(2)]
                wdb = [sb("wdb%d" % i, [128, 4096], BF16, st=se) for i in range(2)]
                xbT = sb("xbT", [128, 8, 128], BF16, st=se)
                sgate = sb("sgate", [128, 512], st=se)
                actb = sb("actb", [128, 512], BF16, st=se)
                actT = sb("actT", [128, 4, 128], BF16, st=se)
                ybuf = [sb("ybuf%d" % i, [128, D], st=se) for i in range(2)]
                y1 = [sb("y1_%d" % i, [128, D], st=se) for i in range(2)]
                y2 = [sb("y2_%d" % i, [128, D], st=se) for i in range(2)]
                xm = [sb("xm%d" % i, [128, D], st=se) for i in range(2)]
                junk2 = sb("junk2", [128, D], st=se)
                st2 = sb("st2", [128, 4], st=se)
                g2_bc = sb("g2_bc", [128, 2, D], st=se)
                Rall = sb("Rall", [128, NSUB, NE], st=se)
                Mall = sb("Mall", [128, NSUB, NE], BF16, st=se)
                dest_f = sb("dest_f", [128, NSUB, 2], st=se)
                dest_i = sb("dest_i", [128, NSUB, 2], I32, st=se)
                widx = sb("widx", [128, NB, 2], I32, st=se)
                for s in range(2):
                    LD(lambda e, s=s: e.dma_start(out=g2_bc[:, s, :], in_=MODROW[l, s:s + 1, 5 * D:6 * D].to_broadcast([128, D])), w=["g2_bc"])

                zrow = sb("zrow", [128, D], BF16, st=se)
                V(lambda e: e.memset(zrow[:], 0.0), w=["zrow"])
                XBz = XB.rearrange("(n p) f -> p n f", p=128)
                for n0 in range(0, NSB, 12):
                    LD(lambda e, n0=n0: e.dma_start(out=XBz[:, n0:n0 + 12, :], in_=zrow[:].unsqueeze(1).to_broadcast([128, 12, D])),
                       r=["zrow"], w=["XBz"])
                V(lambda e: e.tensor_tensor(out=Mall[:], in0=M1all[:], in1=M2all[:], op=ALU.add), r=["M1all", "M2all"], w=["Mall"])
                V(lambda e: e.memset(pre[:], 0.0), w=["pre"])
                for i in range(NSUB):
                    T(lambda e, i=i: e.matmul(ps[0][:, 0:NE], lhsT=trib[:], rhs=Mall[:, i, :], start=True, stop=True), r=["Mall", "trib"], w=["ps0"])
                    T(lambda e, i=i: e.matmul(ps[1][:, 0:NE], lhsT=onesb[:], rhs=Mall[:, i, :], start=True, stop=True), r=["Mall", "onesb"], w=["ps1"])
                    V(lambda e, i=i: e.tensor_tensor(out=Rall[:, i, :], in0=ps[0][:, 0:NE], in1=pre[:], op=ALU.add), r=["ps0", "pre"], w=["Rall"])
                    V(lambda e: e.tensor_tensor(out=pre[:], in0=ps[1][:, 0:NE], in1=pre[:], op=ALU.add), r=["ps1", "pre"], w=["pre"])
                cmpT = cmp3[:].rearrange("p n e -> p (n e)").rearrange("p (e n) -> p e n", n=NB)
                V(lambda e: e.tensor_tensor(out=cmpT, in0=pre[:].unsqueeze(2).to_broadcast([128, NE, NB]),
                                            in1=nblk.unsqueeze(1).to_broadcast([128, NE, NB]), op=ALU.is_gt), r=["pre", "cs"], w=["cmp3"])
                V(lambda e: e.tensor_reduce(out=padf[:], in_=cmpT, axis=AX.X, op=ALU.add), r=["cmp3"], w=["padf"])
                V(lambda e: e.tensor_scalar(out=padf[:], in0=padf[:], scalar1=float(BLK), scalar2=None, op0=ALU.mult), r=["padf"], w=["padf"])
                V(lambda e: e.tensor_tensor_scan(out=incl[:], data0=ones32[:, 0:NE], data1=padf[:], initial=0.0, op0=ALU.mult, op1=ALU.add),
                  r=["padf", "cs"], w=["incl"])
                V(lambda e: e.tensor_tensor(out=offs[:], in0=incl[:], in1=padf[:], op=ALU.subtract), r=["incl", "padf"], w=["offs"])
                V(lambda e: e.tensor_tensor(out=cmp3[:], in0=nblk.unsqueeze(2).to_broadcast([128, NB, NE]),
                                            in1=incl[:].unsqueeze(1).to_broadcast([128, NB, NE]), op=ALU.is_ge), r=["incl", "cs"], w=["cmp3"])
                V(lambda e: e.tensor_reduce(out=bef[:], in_=cmp3[:], axis=AX.X, op=ALU.add), r=["cmp3"], w=["bef"])
                V(lambda e: e.tensor_scalar(out=bef[:], in0=bef[:], scalar1=31.0, scalar2=128.0, op0=ALU.min, op1=ALU.mult), r=["bef"], w=["bef"])
                V(lambda e: e.tensor_scalar(out=bef[:], in0=bef[:], scalar1=pcol, scalar2=None, op0=ALU.add), r=["bef", "cs"], w=["bef"])
                V(lambda e: e.tensor_copy(widx[:, :, 0], bef[:]), r=["bef"], w=["widx"])
                V(lambda e: e.tensor_scalar(out=bef[:], in0=bef[:], scalar1=4096.0, scalar2=None, op0=ALU.add), r=["bef"], w=["bef"])
                V(lambda e: e.tensor_copy(widx[:, :, 1], bef[:]), r=["bef"], w=["widx"])
                for i in range(NSUB):
                    V(lambda e, i=i: e.tensor_tensor(out=tmpr[:], in0=Rall[:, i, :], in1=offs[:], op=ALU.add), r=["Rall", "offs"], w=["tmpr"])
                    V(lambda e, i=i: e.scalar_tensor_tensor(out=tmpr2[:], in0=tmpr[:], scalar=1.0, in1=M1all[:, i, :], op0=ALU.mult, op1=ALU.mult,
                                                            accum_out=dest_f[:, i, 0:1]), r=["tmpr", "M1all"], w=["tmpr2", "dest_f"])
                    V(lambda e, i=i: e.scalar_tensor_tensor(out=tmpr2[:], in0=tmpr[:], scalar=1.0, in1=M2all[:, i, :], op0=ALU.mult, op1=ALU.mult,
                                                            accum_out=dest_f[:, i, 1:2]), r=["tmpr", "M2all"], w=["tmpr2", "dest_f"])
                V(lambda e: e.tensor_copy(dest_i[:], dest_f[:]), r=["dest_f"], w=["dest_i"])
                for i in range(NSUB):
                    hb = hrow[i % 2]
                    hn = "hrow%d" % (i % 2)
                    LD(lambda e, i=i, hb=hb: e.dma_start(out=hb[:], in_=H2v[:, i, :]), r=["H2_%d" % i], w=[hn])
                    for k in range(2):
                        GD(lambda e, i=i, k=k, hb=hb: e.indirect_dma_start(
                            out=XB, out_offset=bass.IndirectOffsetOnAxis(ap=dest_i[:, i, k:k + 1], axis=0), in_=hb[:], in_offset=None),
                           r=[hn, "dest_i", "XBz"], w=["XBs%d_%d" % (i, k)])
                P.barrier_all()

                stop("p3")
                XBv = XB.rearrange("(n p) f -> p n f", p=128)
                YBv = YB.rearrange("(n p) f -> p n f", p=128)

                def wload(n):
                    k = n % 2
                    for hf in range(2):
                        GD(lambda e, n=n, hf=hf, k=k: e.indirect_dma_start(
                            out=wgb[k][:, hf * 2048:(hf + 1) * 2048], out_offset=None, in_=wg2[l],
                            in_offset=bass.IndirectOffsetOnAxis(ap=widx[:, n, hf:hf + 1], axis=0)), r=["widx"], w=["wgb%d" % k])
                        GD(lambda e, n=n, hf=hf, k=k: e.indirect_dma_start(
                            out=wub[k][:, hf * 2048:(hf + 1) * 2048], out_offset=None, in_=wu2[l],
                            in_offset=bass.IndirectOffsetOnAxis(ap=widx[:, n, hf:hf + 1], axis=0)), r=["widx"], w=["wub%d" % k])
                        GD(lambda e, n=n, hf=hf, k=k: e.indirect_dma_start(
                            out=wdb[k][:, hf * 2048:(hf + 1) * 2048], out_offset=None, in_=wd2[l],
                            in_offset=bass.IndirectOffsetOnAxis(ap=widx[:, n, hf:hf + 1], axis=0)), r=["widx"], w=["wdb%d" % k])

                wload(0)
                for n in range(NB):
                    k = n % 2
                    if n + 1 < NB:
                        wload(n + 1)
                    for sub in range(BLK // 128):
                        sbi = n * (BLK // 128) + sub
                        kk2 = sbi % 2
                        hb = hrow[kk2]
                        hn = "hrow%d" % kk2
                        LD(lambda e, sbi=sbi, hb=hb: e.dma_start(out=hb[:], in_=XBv[:, sbi, :]), r=[], w=[hn])
                        for c in range(8):
                            T(lambda e, c=c, hb=hb: e.matmul(ps[4 + c // 4][:, (c % 4) * 128:(c % 4 + 1) * 128], lhsT=hb[:, c * 128:(c + 1) * 128],
                                                             rhs=identb[:], start=True, stop=True), r=[hn, "identb"], w=["ps%d" % (4 + c // 4)])
                        A(lambda e: e.activation(out=xbT[:, 0:4, :].rearrange("p c t -> p (c t)"), in_=ps[4][:, :], func=AF.Copy), r=["ps4"], w=["xbT"])
                        V(lambda e: e.tensor_copy(xbT[:, 4:8, :].rearrange("p c t -> p (c t)"), ps[5][:, :]), r=["ps5"], w=["xbT"])
                        wg3 = wgb[k][:].rearrange("p (c f) -> p c f", f=512)
                        wu3 = wub[k][:].rearrange("p (c f) -> p c f", f=512)
                        wd3 = wdb[k][:].rearrange("p (c f) -> p c f", f=1024)
                        for c in range(8):
                            T(lambda e, c=c, wg3=wg3: e.matmul(ps[0][:, :], lhsT=xbT[:, c, :], rhs=wg3[:, c, :], start=(c == 0), stop=(c == 7)),
                              r=["xbT", "wgb%d" % k], w=["ps0"])
                        for c in range(8):
                            T(lambda e, c=c, wu3=wu3: e.matmul(ps[1][:, :], lhsT=xbT[:, c, :], rhs=wu3[:, c, :], start=(c == 0), stop=(c == 7)),
                              r=["xbT", "wub%d" % k], w=["ps1"])
                        A(lambda e: e.activation(out=sgate[:], in_=ps[0][:, :], func=AF.Silu), r=["ps0"], w=["sgate"])
                        V(lambda e: e.tensor_tensor(out=actb[:], in0=ps[1][:, :], in1=sgate[:], op=ALU.mult), r=["ps1", "sgate"], w=["actb"])
                        for c in range(4):
                            T(lambda e, c=c: e.matmul(ps[6][:, c * 128:(c + 1) * 128], lhsT=actb[:, c * 128:(c + 1) * 128], rhs=identb[:],
                                                      start=True, stop=True), r=["actb", "identb"], w=["ps6"])
                        V(lambda e: e.tensor_copy(actT[:].rearrange("p c t -> p (c t)"), ps[6][:, :]), r=["ps6"], w=["actT"])
                        for hf in range(2):
                            for c in range(4):
                                T(lambda e, c=c, hf=hf, wd3=wd3: e.matmul(ps[2 + hf][:, :], lhsT=actT[:, c, :], rhs=wd3[:, c, hf * 512:(hf + 1) * 512],
                                                                         start=(c == 0), stop=(c == 3)), r=["actT", "wdb%d" % k], w=["ps%d" % (2 + hf)])
                        yb = ybuf[kk2]
                        yn = "ybuf%d" % kk2
                        A(lambda e, yb=yb: e.activation(out=yb[:, 0:512], in_=ps[2][:, :], func=AF.Copy), r=["ps2"], w=[yn])
                        V(lambda e, yb=yb: e.tensor_copy(yb[:, 512:1024], ps[3][:, :]), r=["ps3"], w=[yn])
                        LD(lambda e, sbi=sbi, yb=yb: e.dma_start(out=YBv[:, sbi, :], in_=yb[:]), r=[yn], w=["YB%d" % sbi])
                P.barrier_all()

                stop("p4")
                XOv = XS1.rearrange("(n p) f -> p n f", p=128)
                OUTv = out.rearrange("(n p) f -> p n f", p=128)
                for i in range(NSUB):
                    if last and i < 2:
                        continue
                    k = i % 2
                    s = 1 if i < 2 else 0
                    GD(lambda e, i=i, k=k: e.indirect_dma_start(out=y1[k][:], out_offset=None, in_=YB,
                                                                in_offset=bass.IndirectOffsetOnAxis(ap=dest_i[:, i, 0:1], axis=0)),
                       r=["dest_i"], w=["y1_%d" % k])
                    GD(lambda e, i=i, k=k: e.indirect_dma_start(out=y2[k][:], out_offset=None, in_=YB,
                                                                in_offset=bass.IndirectOffsetOnAxis(ap=dest_i[:, i, 1:2], axis=0)),
                       r=["dest_i"], w=["y2_%d" % k])
                    LD(lambda e, i=i, k=k: e.dma_start(out=xm[k][:], in_=XMv[:, i, :]), r=["XM%d" % (i // 2)], w=["xm%d" % k])
                    V(lambda e, i=i, k=k: e.tensor_scalar(out=y1[k][:], in0=y1[k][:], scalar1=W12[:, i, 0:1], scalar2=None, op0=ALU.mult),
                      r=["y1_%d" % k, "W12"], w=["y1_%d" % k])
                    V(lambda e, i=i, k=k: e.scalar_tensor_tensor(out=y1[k][:], in0=y2[k][:], scalar=W12[:, i, 1:2], in1=y1[k][:],
                                                                 op0=ALU.mult, op1=ALU.add), r=["y1_%d" % k, "y2_%d" % k, "W12"], w=["y1_%d" % k])
                    G(lambda e, k=k, s=s: e.tensor_tensor(out=y1[k][:], in0=y1[k][:], in1=g2_bc[:, s, :], op=ALU.mult),
                      r=["y1_%d" % k, "g2_bc"], w=["y1_%d" % k])
                    V(lambda e, k=k: e.tensor_tensor(out=xm[k][:], in0=xm[k][:], in1=y1[k][:], op=ALU.add), r=["xm%d" % k, "y1_%d" % k], w=["xm%d" % k])
                    if not last:
                        LD(lambda e, i=i, k=k: e.dma_start(out=XOv[:, i, :], in_=xm[k][:]), r=["xm%d" % k], w=["XIN%d" % (i // 2)])
                    else:
                        if dbg:
                            LD(lambda e, i=i, k=k: e.dma_start(out=XOv[:, i, :], in_=xm[k][:]), r=["xm%d" % k], w=["XIN%d" % (i // 2)])
                        A(lambda e, k=k: e.activation(out=junk2[:], in_=xm[k][:], func=AF.Square, accum_out=st2[:, 0:1]), r=["xm%d" % k], w=["junk2", "st2"])
                        V(lambda e: e.tensor_scalar(out=st2[:, 1:2], in0=st2[:, 0:1], scalar1=1.0 / D, scalar2=EPS, op0=ALU.mult, op1=ALU.add),
                          r=["st2"], w=["st2"])
                        A(lambda e: e.activation(out=st2[:, 1:2], in_=st2[:, 1:2], func=AF.Sqrt), r=["st2"], w=["st2"])
                        V(lambda e: e.reciprocal(out=st2[:, 2:3], in_=st2[:, 1:2]), r=["st2"], w=["st2"])
                        V(lambda e, k=k: e.scalar_tensor_tensor(out=xm[k][:], in0=xm[k][:], scalar=st2[:, 2:3], in1=fn_bc[:], op0=ALU.mult, op1=ALU.mult),
                          r=["xm%d" % k, "st2", "fn_bc"], w=["xm%d" % k])
                        LD(lambda e, i=i, k=k: e.dma_start(out=OUTv[:, i - 2, :], in_=xm[k][:]), r=["xm%d" % k], w=["OUT"], is_out=True)
                if dbg and last:
                    LD(lambda e: e.dma_start(out=YBv[:, NSB - 1, :], in_=fn_bc[:]), r=["fn_bc"], w=["YBdbg"])
                    LD(lambda e: e.dma_start(out=YBv[:, NSB - 2, 0:4], in_=st2[:]), r=["st2"], w=["YBdbg2"])
                P.barrier_all()
                se.close(); open_stacks.remove(se)
        except _Stop:
            for stx in reversed(open_stacks):
                stx.close()
        P.finish()
        block = E(nc.Block())
        P.replay(block)
    return nc


def _consts():
    c = np.zeros((128, CW), np.float32)
    c[:, 0:128] = np.eye(128, dtype=np.float32)
    tp = np.arange(128)[:, None]
    tt = np.arange(128)[None, :]
    c[:, 128:256] = (tp < tt).astype(np.float32)
    m = np.ones(256, np.float32)
    m[::64] = 0.0
    c[:, 256:512] = m[None, :]
    c[:, 512:640] = 1.0
    c[:, 640:640 + NB] = (np.arange(NB) * float(BLK))[None, :]
    c[:, 760] = np.arange(128)
    same = (tp // 64) == (tt // 64)
    m8 = np.zeros((128, 256), np.uint8)
    m8[:, 0:128] = (same & (tt >= tp)).astype(np.uint8)
    m8[:, 128:256] = (same & (tt <= tp)).astype(np.uint8)
    return c, m8


def _relay_gate(w):
    a = w.reshape(NE, 8, 128, 512).transpose(0, 2, 1, 3).reshape(NE, 128, 2, 2048)
    return np.ascontiguousarray(a.transpose(2, 0, 1, 3).reshape(2 * NE * 128, 2048))


def _relay_down(w):
    a = w.reshape(NE, 4, 128, 1024).transpose(0, 2, 1, 3).reshape(NE, 128, 2, 2048)
    return np.ascontiguousarray(a.transpose(2, 0, 1, 3).reshape(2 * NE * 128, 2048))


def make_in_maps(inputs, cores, small=False):
    f = lambda a: np.ascontiguousarray(np.asarray(a, dtype=np.float32))
    x, c, ctx, c_ctx = f(inputs['x']), f(inputs['c']), f(inputs['ctx']), f(inputs['c_ctx'])
    norm1, norm2 = f(inputs['norm1']), f(inputs['norm2'])
    cst, cst8 = _consts()
    shared = dict(
        w_mod=f(inputs['w_mod']), b_mod=f(inputs['b_mod']), w_in=f(inputs['w_in']), n2row=norm2,
        sgn=f(inputs['sgu_norm']), sgw=f(inputs['sgu_w']), sgb=f(inputs['sgu_b']).reshape(2, 512),
        w_out=f(inputs['w_out']),
        wrt=np.ascontiguousarray(np.concatenate([f(inputs['w_group']), f(inputs['w_router'])], axis=2)),
        brt=np.ascontiguousarray(np.concatenate([f(inputs['b_group']), f(inputs['b_router'])], axis=1)),
        wg2_0=_relay_gate(f(inputs['w_gate'][0])), wg2_1=_relay_gate(f(inputs['w_gate'][1])),
        wu2_0=_relay_gate(f(inputs['w_up'][0])), wu2_1=_relay_gate(f(inputs['w_up'][1])),
        wd2_0=_relay_down(f(inputs['w_down'][0])), wd2_1=_relay_down(f(inputs['w_down'][1])),
        fnorm=f(inputs['final_norm']).reshape(1, D), cst=cst, cst8=cst8,
    )
    if small:
        for k in list(shared):
            if k[:3] in ("wg2", "wu2", "wd2"):
                shared[k] = np.zeros((8, 8), np.float32)
    maps = []
    for b in cores:
        rows = np.zeros((72, 128), np.float32)
        for l in range(2):
            rows[l * 16:l * 16 + 8] = norm1[l].reshape(8, 128)
            rows[l * 16 + 8:l * 16 + 16] = norm2[l].reshape(8, 128)
        rows[32:48] = f(inputs['lb_logits']).reshape(16, 128)
        rows[48:56] = f(inputs['hgrn_norm']).reshape(8, 128)
        rows[56:64] = c[b].reshape(8, 128)
        rows[64:72] = c_ctx.reshape(8, 128)
        m = dict(shared)
        m['xs'] = np.ascontiguousarray(np.concatenate([ctx[b], x[b]], axis=0))
        m['rows_in'] = rows
        maps.append(m)
    return maps


def kernel(**inputs):
    n = 8
    nc = build_nc()
    in_maps = make_in_maps(inputs, list(range(n)))
    res = run_bass_kernel_spmd(nc, in_maps, core_ids=list(range(n)))
    return np.stack([np.asarray(r["out"], dtype=np.float32) for r in res.results], axis=0)
```

```python
import numpy as np
from contextlib import ExitStack
import concourse.bass as bass
import concourse.mybir as mybir
from concourse.bass_utils import run_bass_kernel_spmd

F32 = mybir.dt.float32
BF16 = mybir.dt.bfloat16
I32 = mybir.dt.int32
U8 = mybir.dt.uint8
AF = mybir.ActivationFunctionType
ALU = mybir.AluOpType
AX = mybir.AxisListType

D = 1024
TL = 4096
TC = 256
TOK = TL + TC
NT = 256
NTILES = TOK // NT
NSUB = TOK // 128
NCH = TOK // 64
INW = 3584
NE = 32
BLK = 256
NB = (NSUB * 2 * 128 + BLK - 1) // BLK + NE
NSB = NB * (BLK // 128)
EPS = 1e-6
CW = 1024


class _Rec:
    def __init__(self):
        self.calls = []

    def __getattr__(self, name):
        def f(*a, **k):
            self.calls.append((name, a, k))
            return self
        return f


def _bind(fn):
    rec = _Rec()
    fn(rec)
    assert len(rec.calls) == 1, rec.calls
    name, a, k = rec.calls[0]
    return lambda e: getattr(e, name)(*a, **k)


class Prog:
    ENG = ['pe', 'dve', 'act', 'pool', 'sp']
    NDS = 16

    def __init__(self, nc, stack):
        self.nc = nc
        self.q = {e: [] for e in self.ENG}
        self.cnt = {e: 0 for e in self.ENG}
        self.sems = {}
        for e in self.ENG:
            self.sems['s_' + e] = stack.enter_context(nc.semaphore('s_' + e))
        for e in ('sp', 'pool', 'act'):
            for i in range(self.NDS):
                self.sems['d_%s%d' % (e, i)] = stack.enter_context(nc.semaphore('d_%s%d' % (e, i)))
        self.dcnt = {e: 0 for e in ('sp', 'pool', 'act')}
        self.waited = {e: {} for e in self.ENG}
        self.lastw = {}
        self.readers = {}
        self.out_toks = []

    def _deps(self, reads, writes):
        deps = []
        for b in reads:
            if b in self.lastw:
                deps.append(self.lastw[b])
        for b in writes:
            if b in self.lastw:
                deps.append(self.lastw[b])
            deps.extend(self.readers.get(b, []))
        return deps

    def _wait(self, eng, tok):
        key, val, src = tok
        if src == 'pe' and eng == 'pe' and key == 's_pe':
            return
        if self.waited[eng].get(key, 0) >= val:
            return
        self.waited[eng][key] = val
        sem = self.sems[key]
        self.q[eng].append(lambda e, sem=sem, val=val: e.wait_ge(sem, val))

    def _record(self, tok, reads, writes):
        for b in reads:
            self.readers.setdefault(b, []).append(tok)
        for b in writes:
            self.lastw[b] = tok
            self.readers[b] = []

    def op(self, eng, fn, reads=(), writes=()):
        fn = _bind(fn)
        for tok in self._deps(reads, writes):
            self._wait(eng, tok)
        self.cnt[eng] += 1
        seq = self.cnt[eng]
        sem = self.sems['s_' + eng]
        self.q[eng].append(lambda e, fn=fn, sem=sem: fn(e).then_inc(sem, 1))
        tok = ('s_' + eng, seq, eng)
        self._record(tok, reads, writes)
        return tok

    def dma(self, eng, fn, reads=(), writes=(), is_out=False):
        fn = _bind(fn)
        for tok in self._deps(reads, writes):
            self._wait(eng, tok)
        k = self.dcnt[eng]
        self.dcnt[eng] += 1
        slot = k % self.NDS
        val = 16 * (k // self.NDS + 1)
        key = 'd_%s%d' % (eng, slot)
        if k >= self.NDS:
            self._wait(eng, (key, val - 16, eng))
        sem = self.sems[key]
        self.q[eng].append(lambda e, fn=fn, sem=sem: fn(e).then_inc(sem, 16))
        tok = (key, val, eng)
        self._record(tok, reads, writes)
        if is_out:
            self.out_toks.append(tok)
        return tok

    def barrier_all(self):
        toks = []
        for e in self.ENG:
            if self.cnt[e] > 0:
                toks.append(('s_' + e, self.cnt[e], e))
        for e in ('sp', 'pool', 'act'):
            k = self.dcnt[e]
            for slot in range(self.NDS):
                n = (k - slot + self.NDS - 1) // self.NDS if k > slot else 0
                if n > 0:
                    toks.append(('d_%s%d' % (e, slot), 16 * n, e))
        for e in self.ENG:
            for t in toks:
                if t[2] == 'pe' and e == 'pe' and t[0] == 's_pe':
                    pass
                key, val, src = t
                if self.waited[e].get(key, 0) >= val:
                    continue
                self.waited[e][key] = val
                sem = self.sems[key]
                self.q[e].append(lambda en, sem=sem, val=val: en.wait_ge(sem, val))
        self.lastw = {}
        self.readers = {}

    def finish(self):
        for tok in self.out_toks:
            self._wait('sp', tok)

    def replay(self, block):
        q = self.q

        @block.tensor
        def _(e):
            for f in q['pe']:
                f(e)

        @block.vector
        def _(e):
            for f in q['dve']:
                f(e)

        @block.scalar
        def _(e):
            for f in q['act']:
                f(e)

        @block.gpsimd
        def _(e):
            for f in q['pool']:
                f(e)

        @block.sync
        def _(e):
            for f in q['sp']:
                f(e)


class _Stop(Exception):
    pass


def build_nc(n_layers=2, dbg=None):
    nc = bass.Bass("TRN2", target_bir_lowering=False)

    def din(name, shape, dt=F32):
        return nc.dram_tensor(name, shape, dt, kind="ExternalInput").ap()

    def dint(name, shape, dt=F32):
        return nc.dram_tensor(name, shape, dt, kind=("ExternalOutput" if dbg else "Internal")).ap()

    xs = din("xs", [TOK, D])
    rows_in = din("rows_in", [72, 128])
    w_mod = din("w_mod", [2, D, 6 * D])
    b_mod = din("b_mod", [2, 6 * D])
    w_in = din("w_in", [2, D, INW])
    n2row = din("n2row", [2, D])
    sgn = din("sgn", [2, 512])
    sgw = din("sgw", [2, 4, 128, 128])
    sgb = din("sgb", [2, 512])
    w_out = din("w_out", [2, D, D])
    wrt = din("wrt", [2, D, 36])
    brt = din("brt", [2, 36])
    esh = [8, 8] if dbg in ("s0", "l0", "p1a", "p1", "p2", "p3", "ta", "tb", "tc", "td", "te", "tf", "tg") else [8192, 2048]
    wg2 = [din("wg2_%d" % i, esh) for i in range(2)]
    wu2 = [din("wu2_%d" % i, esh) for i in range(2)]
    wd2 = [din("wd2_%d" % i, esh) for i in range(2)]
    fnorm = din("fnorm", [1, D])
    cst = din("cst", [128, CW])
    cst8 = din("cst8", [128, 256], U8)
    out = nc.dram_tensor("out", [TL, D], F32, kind="ExternalOutput").ap()

    MODROW = dint("MODROW", [2, 2, 6 * D])
    OBF = dint("OBF", [512, TOK])
    QF = dint("QF", [512, TOK], BF16)
    GS = dint("GS", [512, TOK], BF16)
    SGD = dint("SGD", [512, TOK], BF16)
    KFT = dint("KFT", [TOK, 512], BF16)
    VT = dint("VT", [TOK, 512], BF16)
    XM = dint("XM", [TOK, D])
    H2 = dint("H2", [TOK, D], BF16)
    XB = dint("XB", [NSB * 128, D], BF16)
    YB = dint("YB", [NSB * 128, D])
    XS1 = dint("XS1", [TOK, D])

    with ExitStack() as top:
        E = top.enter_context
        P = Prog(nc, top)

        uid = [0]

        def sb(name, shape, dt=F32, st=top):
            uid[0] += 1
            return st.enter_context(nc.sbuf_tensor("%s_u%d" % (name, uid[0]), shape, dt))

        def V(fn, r=(), w=()):
            return P.op('dve', fn, r, w)

        def A(fn, r=(), w=()):
            return P.op('act', fn, r, w)

        def G(fn, r=(), w=()):
            return P.op('pool', fn, r, w)

        def T(fn, r=(), w=()):
            return P.op('pe', fn, r, w)

        def LD(fn, r=(), w=(), is_out=False):
            return P.dma('sp', fn, r, w, is_out)

        def GD(fn, r=(), w=()):
            return P.dma('pool', fn, r, w)

        cs = sb("cs", [128, CW])
        m8 = sb("m8", [128, 256], U8)
        identb = sb("identb", [128, 128], BF16)
        trib = sb("trib", [128, 128], BF16)
        onesb = sb("onesb", [128, 128], BF16)
        colsA = sb("colsA", [128, 72])
        colsM = sb("colsM", [128, 192])
        scv = sb("scv", [128, 16])
        lbc = sb("lbc", [128, 2, 8])
        oml = sb("oml", [128, 2, 8])
        lbm1 = sb("lbm1", [128, 2, 8])
        a1c = sb("a1c", [128, 2, 2, 8])
        ERF = sb("ERF", [128, 4, NCH])
        ETRF = sb("ETRF", [128, 4, NCH])
        DECF = sb("DECF", [128, 4, NCH])
        M1all = sb("M1all", [128, NSUB, NE], BF16)
        M2all = sb("M2all", [128, NSUB, NE], BF16)
        W12 = sb("W12", [128, NSUB, 2])
        fn_bc = sb("fn_bc", [128, D])
        Sst = sb("Sst", [128, 4, 128])
        Sbf = sb("Sbf", [128, 4, 4, 128], BF16)
        scb = [sb("scb%d" % i, [128, 128], BF16) for i in range(2)]
        scf = [sb("scf%d" % i, [128, 128], BF16) for i in range(2)]

        ps = [E(nc.psum_tensor("ps%d" % i, [128, 512], F32)) for i in range(8)]

        ident = cs[:, 0:128]
        tri32 = cs[:, 128:256]
        mskrow = cs[:, 256:512]
        ones32 = cs[:, 512:640]
        nblk = cs[:, 640:640 + NB]
        pcol = cs[:, 760:761]
        maskF = m8[:, 0:128]
        maskB = m8[:, 128:256]

        open_stacks = []

        def newstack():
            stx = ExitStack()
            open_stacks.append(stx)
            return stx

        def stop(tag):
            if dbg == tag:
                P.barrier_all()
                raise _Stop()

        try:
            LD(lambda e: e.dma_start(out=cs[:], in_=cst), w=["cs"])
            LD(lambda e: e.dma_start(out=m8[:], in_=cst8), w=["m8"])
            V(lambda e: e.tensor_copy(identb[:], ident), r=["cs"], w=["identb"])
            V(lambda e: e.tensor_copy(trib[:], tri32), r=["cs"], w=["trib"])
            V(lambda e: e.tensor_copy(onesb[:], ones32), r=["cs"], w=["onesb"])
            LD(lambda e: e.dma_start(out=fn_bc[:], in_=fnorm.to_broadcast([128, D])), w=["fn_bc"])
            for i in range(2):
                V(lambda e, i=i: e.memset(scb[i][:], 0.0), w=["scb%d" % i])
                V(lambda e, i=i: e.memset(scf[i][:], 0.0), w=["scf%d" % i])

            s0 = newstack()
            rowsA = sb("rowsA", [72, 128], st=s0)
            LD(lambda e: e.dma_start(out=rowsA[:], in_=rows_in), w=["rowsA"])
            T(lambda e: e.transpose(out=ps[0][:, 0:72], in_=rowsA[:], identity=cs[0:72, 0:72]), r=["rowsA", "cs"], w=["ps0"])
            V(lambda e: e.tensor_copy(colsA[:], ps[0][:, 0:72]), r=["ps0"], w=["colsA"])
            A(lambda e: e.activation(out=scv[:], in_=colsA[:, 56:72], func=AF.Silu), r=["colsA"], w=["scv"])
            V(lambda e: e.memset(lbc[:], 0.0), w=["lbc"])
            V(lambda e: e.tensor_tensor(out=lbc[:, 1, :], in0=colsA[:, 40:48], in1=colsA[:, 32:40], op=ALU.subtract), r=["colsA"], w=["lbc"])
            A(lambda e: e.activation(out=lbc[:, 1, :], in_=lbc[:, 1, :], func=AF.Sigmoid), r=["lbc"], w=["lbc"])
            V(lambda e: e.tensor_scalar(out=oml[:], in0=lbc[:], scalar1=-1.0, scalar2=1.0, op0=ALU.mult, op1=ALU.add), r=["lbc"], w=["oml"])
            V(lambda e: e.tensor_scalar(out=lbm1[:], in0=lbc[:], scalar1=-1.0, scalar2=None, op0=ALU.add), r=["lbc"], w=["lbm1"])

            wmb = [sb("wmb%d" % i, [128, 8, 512], st=s0) for i in range(2)]
            bmod_sb = sb("bmod_sb", [2, 6 * D], st=s0)
            modsb = sb("modsb", [2, 6 * D], st=s0)
            it = 0
            for l in range(2):
                LD(lambda e, l=l: e.dma_start(out=bmod_sb[:], in_=b_mod[l:l + 1, :].to_broadcast([2, 6 * D])), w=["bmod_sb"])
                for n in range(12):
                    wb = wmb[it % 2]
                    wn = "wmb%d" % (it % 2)
                    LD(lambda e, l=l, n=n, wb=wb: e.dma_start(
                        out=wb[:], in_=w_mod[l, :, n * 512:(n + 1) * 512].rearrange("(kc p) f -> p kc f", p=128)), w=[wn])
                    pb = ps[it % 2]
                    pn = "ps%d" % (it % 2)
                    for kc in range(8):
                        T(lambda e, kc=kc, wb=wb, pb=pb: e.matmul(pb[0:2, :], lhsT=scv[:, kc:16:8], rhs=wb[:, kc, :],
                                                                 start=(kc == 0), stop=(kc == 7)), r=[wn, "scv"], w=[pn])
                    V(lambda e, n=n, pb=pb: e.tensor_tensor(out=modsb[:, n * 512:(n + 1) * 512], in0=pb[0:2, :],
                                                            in1=bmod_sb[:, n * 512:(n + 1) * 512], op=ALU.add),
                      r=[pn, "bmod_sb"], w=["modsb"])
                    it += 1
                LD(lambda e, l=l: e.dma_start(out=MODROW[l], in_=modsb[:]), r=["modsb"], w=["MODROW"])
            rowsM = sb("rowsM", [96, 2, 128], st=s0)
            MR = MODROW.rearrange("l s (r q) -> l (s r) q", q=128)
            for l in range(2):
                LD(lambda e, l=l: e.dma_start(out=rowsM[:, l, :], in_=MR[l]), r=["MODROW"], w=["rowsM"])
            for l in range(2):
                T(lambda e, l=l: e.transpose(out=ps[2][:, l * 96:(l + 1) * 96], in_=rowsM[:, l, :], identity=cs[0:96, 0:96]),
                  r=["rowsM", "cs"], w=["ps2"])
            V(lambda e: e.tensor_copy(colsM[:], ps[2][:, 0:192]), r=["ps2"], w=["colsM"])

            def cm(l, s, k):
                o = l * 96 + s * 48 + k * 8
                return colsM[:, o:o + 8]

            for l in range(2):
                for s in range(2):
                    V(lambda e, l=l, s=s: e.scalar_tensor_tensor(out=a1c[:, l, s, :], in0=cm(l, s, 1), scalar=1.0,
                                                                 in1=colsA[:, l * 16:l * 16 + 8], op0=ALU.add, op1=ALU.mult),
                      r=["colsM", "colsA"], w=["a1c"])
            P.barrier_all()
            s0.close(); open_stacks.remove(s0)
            stop("s0")

            for l in range(n_layers):
                last = (l == 1)
                XIN = xs if l == 0 else XS1
                sm = newstack()
                winb = sb("winb", [128, 8, INW], BF16, st=sm)
                woutb = sb("woutb", [128, 8, D], BF16, st=sm)
                wr32 = sb("wr32", [128, 8, 36], st=sm)
                brow = sb("brow", [1, 36], st=sm)
                wsT = sb("wsT", [128, 4, 128], BF16, st=sm)
                bsrow = sb("bsrow", [1, 512], BF16, st=sm)
                sgn_bc = sb("sgn_bc", [128, 512], st=sm)
                g1_bc = sb("g1_bc", [128, 2, D], st=sm)
                a2_bc = sb("a2_bc", [128, 2, D], st=sm)
                b2_bc = sb("b2_bc", [128, 2, D], st=sm)
                for kc in range(8):
                    for hf in range(2):
                        GD(lambda e, kc=kc, hf=hf: e.dma_start(out=winb[:, kc, hf * 1792:(hf + 1) * 1792],
                                                               in_=w_in[l, kc * 128:(kc + 1) * 128, hf * 1792:(hf + 1) * 1792]),
                           w=["winb"])
                    GD(lambda e, kc=kc: e.dma_start(out=woutb[:, kc, :], in_=w_out[l, kc * 128:(kc + 1) * 128, :]), w=["woutb"])
                LD(lambda e: e.dma_start(out=wr32[:], in_=wrt[l].rearrange("(kc p) f -> p kc f", p=128)), w=["wr32"])
                LD(lambda e: e.dma_start(out=brow[:], in_=brt[l:l + 1, :]), w=["brow"])
                GD(lambda e: e.dma_start(out=bsrow[:], in_=sgb[l:l + 1, :]), w=["bsrow"])
                LD(lambda e: e.dma_start(out=sgn_bc[:], in_=sgn[l:l + 1, :].to_broadcast([128, 512])), w=["sgn_bc"])
                sl = newstack()
                wsn = sb("wsn", [128, 4, 128], st=sl)
                LD(lambda e: e.dma_start(out=wsn[:], in_=sgw[l].rearrange("h i j -> i h j")), w=["wsn"])
                for h in range(4):
                    T(lambda e, h=h: e.transpose(out=ps[0][:, h * 128:(h + 1) * 128], in_=wsn[:, h, :], identity=ident),
                      r=["wsn", "cs"], w=["ps0"])
                V(lambda e: e.tensor_copy(wsT[:].rearrange("p h i -> p (h i)"), ps[0][:]), r=["ps0"], w=["wsT"])
                tmpb = sb("tmpb", [128, D], st=sl)
                for s in range(2):
                    LD(lambda e, s=s: e.dma_start(out=g1_bc[:, s, :], in_=MODROW[l, s:s + 1, 2 * D:3 * D].to_broadcast([128, D])), w=["g1_bc"])
                    LD(lambda e, s=s: e.dma_start(out=b2_bc[:, s, :], in_=MODROW[l, s:s + 1, 3 * D:4 * D].to_broadcast([128, D])), w=["b2_bc"])
                    LD(lambda e, s=s: e.dma_start(out=a2_bc[:, s, :], in_=MODROW[l, s:s + 1, 4 * D:5 * D].to_broadcast([128, D])), w=["a2_bc"])
                LD(lambda e: e.dma_start(out=tmpb[:], in_=n2row[l:l + 1, :].to_broadcast([128, D])), w=["tmpb"])
                for s in range(2):
                    V(lambda e, s=s: e.scalar_tensor_tensor(out=a2_bc[:, s, :], in0=a2_bc[:, s, :], scalar=1.0, in1=tmpb[:],
                                                            op0=ALU.add, op1=ALU.mult), r=["a2_bc", "tmpb"], w=["a2_bc"])
                P.barrier_all()
                sl.close(); open_stacks.remove(sl)
                stop("l0")

                xt = [sb("xt%d" % i, [128, 2, D], st=sm) for i in range(2)]
                hT = sb("hT", [128, 8, NT], BF16, st=sm)
                q32 = sb("q32", [128, 4, NT], st=sm)
                sgt = [sb("sgt%d" % i, [128, NT], st=sm) for i in range(2)]
                lft = [sb("lft%d" % i, [128, NT], st=sm) for i in range(2)]
                kkt = [sb("kkt%d" % i, [128, NT], st=sm) for i in range(2)]
                Att = [sb("Att%d" % i, [128, NT], st=sm) for i in range(2)]
                e1t = [sb("e1t%d" % i, [128, NT], st=sm) for i in range(2)]
                qtf = sb("qtf", [128, 4, NT], BF16, st=sm)
                ktf = sb("ktf", [128, 4, NT], BF16, st=sm)
                qtb = sb("qtb", [128, 4, NT], BF16, st=sm)
                ktb = sb("ktb", [128, 4, NT], BF16, st=sm)
                gsb = sb("gsb", [128, 4, NT], BF16, st=sm)
                ug = sb("ug", [128, 4, NT], BF16, st=sm)
                mixT = sb("mixT", [128, 8, NT], BF16, st=sm)
                vtok = sb("vtok", [128, 2, 512], BF16, st=sm)
                vg = sb("vg", [128, 512], st=sm)
                vntok = sb("vntok", [128, 2, 512], BF16, st=sm)
                kbtok = sb("kbtok", [128, 2, 512], BF16, st=sm)
                kftok = sb("kftok", [128, 2, 512], BF16, st=sm)
                obf = sb("obf", [128, 4, NT], st=sm)
                ofull = obf
                sqt = sb("sqt", [128, NT], st=sm)
                rt = sb("rt", [128, NT], st=sm)
                mt = sb("mt", [128, NT], st=sm)
                stat = sb("stat", [128, 8], st=sm)
                erb = sb("erb", [128, 4, 4], st=sm)
                etrb = sb("etrb", [128, 4, 4], st=sm)
                decb = sb("decb", [128, 4, 4], st=sm)
                dtmp = sb("dtmp", [128, 4, 4], st=sm)
                utmp = sb("utmp", [128, 4, 128], st=sm)
                tmp2 = sb("tmp2", [128, 512], st=sm)
                h2t = sb("h2t", [128, D], st=sm)
                junk = h2t
                h2T = sb("h2T", [128, 8, 128], st=sm)
                Ls = sb("Ls", [128, 36], st=sm)
                rsm = sb("rsm", [128, 64], st=sm)

                OBFv = OBF.rearrange("(h p) t -> p h t", p=128)
                QFv = QF.rearrange("(h p) t -> p h t", p=128)
                GSv = GS.rearrange("(h p) t -> p h t", p=128)
                SGv = SGD.rearrange("(h p) t -> p h t", p=128)
                KFTv = KFT.rearrange("(n p) f -> p n f", p=128)
                VTv = VT.rearrange("(n p) f -> p n f", p=128)
                XINv = XIN.rearrange("(n p) f -> p n f", p=128)
                XMv = XM.rearrange("(n p) f -> p n f", p=128)
                H2v = H2.rearrange("(n p) f -> p n f", p=128)

                def rstd_from_ss(ssap, rap, n, inv):
                    V(lambda e: e.tensor_scalar(out=rap, in0=ssap, scalar1=inv, scalar2=EPS, op0=ALU.mult, op1=ALU.add),
                      r=["stat"], w=["stat"])
                    A(lambda e: e.activation(out=rap, in_=rap, func=AF.Sqrt), r=["stat"], w=["stat"])
                    V(lambda e: e.reciprocal(out=rap, in_=rap), r=["stat"], w=["stat"])

                def load_x(ti, bi):
                    LD(lambda e: e.dma_start(out=xt[bi][:], in_=XINv[:, 2 * ti:2 * ti + 2, :]), r=["XIN%d" % ti], w=["xt%d" % bi])

                def norm_to_hT(ti, bi, s):
                    x = xt[bi]
                    xn = "xt%d" % bi
                    for j in range(2):
                        A(lambda e, j=j: e.activation(out=junk[:], in_=x[:, j, :], func=AF.Square, accum_out=stat[:, j:j + 1]),
                          r=[xn], w=["h2t", "stat"])
                    rstd_from_ss(stat[:, 0:2], stat[:, 2:4], 2, 1.0 / D)
                    for j in range(2):
                        V(lambda e, j=j: e.tensor_scalar(out=x[:, j, :], in0=x[:, j, :], scalar1=stat[:, 2 + j:3 + j], scalar2=None,
                                                         op0=ALU.mult), r=[xn, "stat"], w=[xn])
                    for c in range(8):
                        pb = ps[2 + (c % 2)]
                        pn = "ps%d" % (2 + (c % 2))
                        for j in range(2):
                            T(lambda e, c=c, j=j, pb=pb: e.transpose(out=pb[:, j * 128:(j + 1) * 128], in_=x[:, j, c * 128:(c + 1) * 128],
                                                                     identity=ident), r=[xn, "cs"], w=[pn])
                        if c % 2 == 0:
                            V(lambda e, c=c, pb=pb: e.tensor_scalar(out=hT[:, c, :], in0=pb[:, 0:NT], scalar1=a1c[:, l, s, c:c + 1],
                                                                    scalar2=cm(l, s, 0)[:, c:c + 1], op0=ALU.mult, op1=ALU.add),
                              r=[pn, "a1c", "colsM"], w=["hT"])
                        else:
                            A(lambda e, c=c, pb=pb: e.activation(out=hT[:, c, :], in_=pb[:, 0:NT], func=AF.Identity,
                                                                 bias=cm(l, s, 0)[:, c:c + 1], scale=a1c[:, l, s, c:c + 1]),
                              r=[pn, "a1c", "colsM"], w=["hT"])

                pj = [0]

                def proj_fm(m):
                    k = pj[0] % 2
                    pj[0] += 1
                    pb = ps[k]
                    for c in range(8):
                        T(lambda e, c=c, pb=pb: e.matmul(pb[:, 0:NT], lhsT=winb[:, c, m * 128:(m + 1) * 128], rhs=hT[:, c, :],
                                                         start=(c == 0), stop=(c == 7)), r=["winb", "hT"], w=["ps%d" % k])
                    return pb, "ps%d" % k

                def proj_tm(j, col0):
                    k = pj[0] % 2
                    pj[0] += 1
                    pb = ps[k]
                    for c in range(8):
                        T(lambda e, c=c, pb=pb: e.matmul(pb[:, :], lhsT=hT[:, c, j * 128:(j + 1) * 128], rhs=winb[:, c, col0:col0 + 512],
                                                         start=(c == 0), stop=(c == 7)), r=["winb", "hT"], w=["ps%d" % k])
                    return pb, "ps%d" % k

                gi = [0]

                def gates(h, dr, ti):
                    k = gi[0] % 2
                    gi[0] += 1
                    pb, pn = proj_fm(4 + 4 * dr + h)
                    sg_, lf_, kk_, I_, A_, e1_, e2_ = sgt[k], lft[k], kkt[k], sgt[k], Att[k], e1t[k], Att[k]
                    nm = ["sgt%d" % k, "lft%d" % k, "kkt%d" % k, "sgt%d" % k, "Att%d" % k, "e1t%d" % k, "Att%d" % k]
                    ci = dr * 4 + h
                    A(lambda e: e.activation(out=sg_[:], in_=pb[:, 0:NT], func=AF.Sigmoid), r=[pn], w=[nm[0]])
                    A(lambda e: e.activation(out=lf_[:], in_=sg_[:], func=AF.Ln, bias=lbc[:, l, ci:ci + 1], scale=oml[:, l, ci:ci + 1]),
                      r=[nm[0], "lbc", "oml"], w=[nm[1]])
                    V(lambda e: e.tensor_scalar(out=kk_[:], in0=sg_[:], scalar1=-1.0, scalar2=lbm1[:, l, ci:ci + 1], op0=ALU.add, op1=ALU.mult),
                      r=[nm[0], "lbm1"], w=[nm[2]])
                    V(lambda e: e.tensor_tensor_scan(out=I_[:], data0=mskrow, data1=lf_[:], initial=0.0, op0=ALU.mult, op1=ALU.add),
                      r=[nm[1], "cs"], w=[nm[3]])
                    I3 = I_[:].rearrange("p (c t) -> p c t", t=64)
                    A3 = A_[:].rearrange("p (c t) -> p c t", t=64)
                    if dr == 0:
                        V(lambda e: e.tensor_tensor(out=A3, in0=I3, in1=I3[:, :, 31:32].to_broadcast([128, 4, 64]), op=ALU.subtract),
                          r=[nm[3]], w=[nm[4]])
                        A(lambda e: e.activation(out=e1_[:], in_=A_[:], func=AF.Exp), r=[nm[4]], w=[nm[5]])
                        A(lambda e: e.activation(out=e2_[:], in_=A_[:], func=AF.Exp, scale=-1.0), r=[nm[4]], w=[nm[6]])
                        G(lambda e: e.tensor_tensor(out=qtf[:, h, :], in0=q32[:, h, :], in1=e1_[:], op=ALU.mult), r=["q32", nm[5]], w=["qtf"])
                        G(lambda e: e.tensor_tensor(out=ktf[:, h, :], in0=kk_[:], in1=e2_[:], op=ALU.mult), r=[nm[2], nm[6]], w=["ktf"])
                        gc = ti * 4
                        A(lambda e: e.activation(out=ERF[:, h, gc:gc + 4], in_=I3[:, :, 31], func=AF.Exp), r=[nm[3]], w=["ERF"])
                        A(lambda e: e.activation(out=DECF[:, h, gc:gc + 4], in_=I3[:, :, 63], func=AF.Exp), r=[nm[3]], w=["DECF"])
                        V(lambda e: e.tensor_tensor(out=dtmp[:, h, :], in0=I3[:, :, 63], in1=I3[:, :, 31], op=ALU.subtract), r=[nm[3]], w=["dtmp"])
                        A(lambda e: e.activation(out=ETRF[:, h, gc:gc + 4], in_=dtmp[:, h, :], func=AF.Exp), r=["dtmp"], w=["ETRF"])
                    else:
                        V(lambda e: e.tensor_tensor(out=lf_[:], in0=I_[:], in1=lf_[:], op=ALU.subtract), r=[nm[3], nm[1]], w=[nm[1]])
                        E3 = lf_[:].rearrange("p (c t) -> p c t", t=64)
                        V(lambda e: e.tensor_tensor(out=A3, in0=E3, in1=E3[:, :, 32:33].to_broadcast([128, 4, 64]), op=ALU.subtract),
                          r=[nm[1]], w=[nm[4]])
                        A(lambda e: e.activation(out=e1_[:], in_=A_[:], func=AF.Exp, scale=-1.0), r=[nm[4]], w=[nm[5]])
                        A(lambda e: e.activation(out=e2_[:], in_=A_[:], func=AF.Exp), r=[nm[4]], w=[nm[6]])
                        G(lambda e: e.tensor_tensor(out=qtb[:, h, :], in0=q32[:, h, :], in1=e1_[:], op=ALU.mult), r=["q32", nm[5]], w=["qtb"])
                        G(lambda e: e.tensor_tensor(out=ktb[:, h, :], in0=kk_[:], in1=e2_[:], op=ALU.mult), r=[nm[2], nm[6]], w=["ktb"])
                        A(lambda e: e.activation(out=etrb[:, h, :], in_=E3[:, :, 32], func=AF.Exp), r=[nm[1]], w=["etrb"])
                        A(lambda e: e.activation(out=decb[:, h, :], in_=I3[:, :, 63], func=AF.Exp), r=[nm[3]], w=["decb"])
                        V(lambda e: e.tensor_tensor(out=dtmp[:, h, :], in0=I3[:, :, 63], in1=E3[:, :, 32], op=ALU.subtract), r=[nm[3], nm[1]], w=["dtmp"])
                        A(lambda e: e.activation(out=erb[:, h, :], in_=dtmp[:, h, :], func=AF.Exp), r=["dtmp"], w=["erb"])

                def state_step(c, j, cc, ktok, ktn, er_ap, etr_ap, dec_ap, kU, scn):
                    pb = ps[4 + kU % 2]
                    pn = "ps%d" % (4 + kU % 2)
                    for h in range(4):
                        T(lambda e, h=h, pb=pb: e.matmul(pb[:, h * 128:(h + 1) * 128],
                                                         lhsT=ktok[cc * 64:(cc + 1) * 64, j, h * 128:(h + 1) * 128],
                                                         rhs=vtok[cc * 64:(cc + 1) * 64, j, h * 128:(h + 1) * 128], start=True, stop=True),
                          r=[ktn, "vtok"], w=[pn])
                    V(lambda e: e.tensor_tensor(out=Sbf[:, c, :, :], in0=Sst[:], in1=er_ap.to_broadcast([128, 4, 128]), op=ALU.mult),
                      r=["Sst"] + scn, w=["Sbf%d" % c])
                    V(lambda e, pb=pb: e.tensor_tensor(out=utmp[:], in0=pb[:].rearrange("p (h e) -> p h e", e=128),
                                                       in1=etr_ap.to_broadcast([128, 4, 128]), op=ALU.mult), r=[pn] + scn, w=["utmp"])
                    V(lambda e: e.tensor_tensor(out=Sst[:], in0=Sst[:], in1=dec_ap.to_broadcast([128, 4, 128]), op=ALU.mult),
                      r=["Sst"] + scn, w=["Sst"])
                    V(lambda e: e.tensor_tensor(out=Sst[:], in0=Sst[:], in1=utmp[:], op=ALU.add), r=["Sst", "utmp"], w=["Sst"])

                ku = [0]

                V(lambda e: e.memset(Sst[:], 0.0), w=["Sst"])
                order1 = [0] + list(range(NTILES - 1, 0, -1))
                load_x(order1[0], 0)
                for oi, ti in enumerate(order1):
                    bi = oi % 2
                    s = 1 if ti == 0 else 0
                    if oi + 1 < len(order1):
                        load_x(order1[oi + 1], 1 - bi)
                    norm_to_hT(ti, bi, s)
                    stop("ta")
                    for j in range(2):
                        pb, pn = proj_tm(j, 1536)
                        A(lambda e, j=j, pb=pb: e.activation(out=vtok[:, j, :], in_=pb[:, :], func=AF.Copy), r=[pn], w=["vtok"])
                        pb, pn = proj_tm(j, 3072)
                        A(lambda e, pb=pb: e.activation(out=vg[:], in_=pb[:, :], func=AF.Gelu_apprx_tanh), r=[pn], w=["vg"])
                        A(lambda e, j=j: e.activation(out=junk[:, 0:512], in_=vg[:], func=AF.Square, accum_out=stat[:, 4 + j:5 + j]),
                          r=["vg"], w=["h2t", "stat"])
                        rstd_from_ss(stat[:, 4 + j:5 + j], stat[:, 6 + j:7 + j], 1, 1.0 / 512)
                        V(lambda e, j=j: e.scalar_tensor_tensor(out=vntok[:, j, :], in0=vg[:], scalar=stat[:, 6 + j:7 + j], in1=sgn_bc[:],
                                                                op0=ALU.mult, op1=ALU.mult), r=["vg", "stat", "sgn_bc"], w=["vntok"])
                    stop("tb")
                    for h in range(4):
                        pb, pn = proj_fm(h)
                        A(lambda e, h=h, pb=pb: e.activation(out=q32[:, h, :], in_=pb[:, 0:NT], func=AF.Copy), r=[pn], w=["q32"])
                    for h in range(4):
                        gates(h, 0, ti)
                        gates(h, 1, ti)
                    for h in range(4):
                        pb, pn = proj_fm(16 + h)
                        A(lambda e, h=h, pb=pb: e.activation(out=gsb[:, h, :], in_=pb[:, 0:NT], func=AF.Silu), r=[pn], w=["gsb"])
                        pb, pn = proj_fm(20 + h)
                        A(lambda e, h=h, pb=pb: e.activation(out=ug[:, h, :], in_=pb[:, 0:NT], func=AF.Gelu_apprx_tanh), r=[pn], w=["ug"])
                    stop("tc")
                    for j in range(2):
                        pb = ps[2 + j]
                        pn = "ps%d" % (2 + j)
                        for h in range(4):
                            T(lambda e, h=h, j=j, pb=pb: e.matmul(pb[:, h * 128:(h + 1) * 128], lhsT=vntok[:, j, h * 128:(h + 1) * 128],
                                                                  rhs=wsT[:, h, :], start=True, stop=False), r=["vntok", "wsT"], w=[pn])
                            T(lambda e, h=h, pb=pb: e.matmul(pb[:, h * 128:(h + 1) * 128], lhsT=onesb[0:1, :],
                                                             rhs=bsrow[0:1, h * 128:(h + 1) * 128], start=False, stop=True),
                              r=["onesb", "bsrow"], w=[pn])
                        V(lambda e, j=j, pb=pb: e.tensor_tensor(out=mixT[:, 4:8, j * 128:(j + 1) * 128],
                                                                in0=pb[:].rearrange("p (h i) -> p h i", i=128),
                                                                in1=ug[:, :, j * 128:(j + 1) * 128], op=ALU.mult), r=[pn, "ug"], w=["mixT"])
                    stop("td")
                    for j in range(2):
                        for h in range(4):
                            T(lambda e, h=h, j=j: e.matmul(ps[2][:, h * 128:(h + 1) * 128], lhsT=ktb[:, h, j * 128:(j + 1) * 128],
                                                           rhs=identb[:], start=True, stop=True), r=["ktb", "identb"], w=["ps2"])
                            T(lambda e, h=h, j=j: e.matmul(ps[3][:, h * 128:(h + 1) * 128], lhsT=ktf[:, h, j * 128:(j + 1) * 128],
                                                           rhs=identb[:], start=True, stop=True), r=["ktf", "identb"], w=["ps3"])
                        A(lambda e, j=j: e.activation(out=kbtok[:, j, :], in_=ps[2][:, :], func=AF.Copy), r=["ps2"], w=["kbtok"])
                        V(lambda e, j=j: e.tensor_copy(kftok[:, j, :], ps[3][:, :]), r=["ps3"], w=["kftok"])
                    stop("te")
                    for c in (3, 2, 1, 0):
                        j, cc = c // 2, c % 2
                        state_step(c, j, cc, kbtok, "kbtok", erb[:, :, c:c + 1], etrb[:, :, c:c + 1], decb[:, :, c:c + 1], ku[0], ["erb", "etrb", "decb"])
                        ku[0] += 1
                    stop("tf")
                    for j in (1, 0):
                        for h in range(4):
                            k = h % 2
                            off = k * 256
                            jc = slice(j * 128, (j + 1) * 128)
                            T(lambda e, h=h, jc=jc, off=off: e.matmul(ps[6][:, off:off + 128], lhsT=ktb[:, h, jc], rhs=qtb[:, h, jc],
                                                                      start=True, stop=True), r=["ktb", "qtb"], w=["ps6_%d" % k])
                            T(lambda e, h=h, jc=jc, off=off: e.matmul(ps[6][:, off + 128:off + 256], lhsT=ktf[:, h, jc], rhs=qtf[:, h, jc],
                                                                      start=True, stop=True), r=["ktf", "qtf"], w=["ps6_%d" % k])
                            V(lambda e, k=k, off=off: e.copy_predicated(out=scb[k][:], mask=maskB, data=ps[6][:, off:off + 128]),
                              r=["ps6_%d" % k, "m8"], w=["scb%d" % k])
                            V(lambda e, k=k, off=off: e.copy_predicated(out=scf[k][:], mask=maskF, data=ps[6][:, off + 128:off + 256]),
                              r=["ps6_%d" % k, "m8"], w=["scf%d" % k])
                            oo = ps[5][:, h * 128:(h + 1) * 128]
                            T(lambda e, h=h, j=j, k=k, oo=oo: e.matmul(oo, lhsT=vtok[:, j, h * 128:(h + 1) * 128], rhs=scb[k][:],
                                                                       start=True, stop=False), r=["vtok", "scb%d" % k], w=["ps5"])
                            T(lambda e, h=h, j=j, k=k, oo=oo: e.matmul(oo, lhsT=vtok[:, j, h * 128:(h + 1) * 128], rhs=scf[k][:],
                                                                       start=False, stop=False), r=["vtok", "scf%d" % k], w=["ps5"])
                            for cc in (1, 0):
                                c = 2 * j + cc
                                T(lambda e, h=h, c=c, cc=cc, j=j: e.matmul(ps[5][:, h * 128 + cc * 64:h * 128 + (cc + 1) * 64],
                                                                           lhsT=Sbf[:, c, h, :],
                                                                           rhs=qtb[:, h, j * 128 + cc * 64:j * 128 + (cc + 1) * 64],
                                                                           start=False, stop=(cc == 0)), r=["Sbf%d" % c, "qtb"], w=["ps5"])
                        A(lambda e, j=j: e.activation(out=obf[:, :, j * 128:(j + 1) * 128], in_=ps[5][:].rearrange("p (h i) -> p h i", i=128),
                                                      func=AF.Copy), r=["ps5"], w=["obf"])
                    stop("tg")
                    t0 = ti * NT
                    LD(lambda e, t0=t0: e.dma_start(out=OBFv[:, :, t0:t0 + NT], in_=obf[:]), r=["obf"], w=["OBF%d" % ti])
                    LD(lambda e, t0=t0: e.dma_start(out=QFv[:, :, t0:t0 + NT], in_=qtf[:]), r=["qtf"], w=["QF%d" % ti])
                    LD(lambda e, t0=t0: e.dma_start(out=GSv[:, :, t0:t0 + NT], in_=gsb[:]), r=["gsb"], w=["GS%d" % ti])
                    LD(lambda e, t0=t0: e.dma_start(out=SGv[:, :, t0:t0 + NT], in_=mixT[:, 4:8, :]), r=["mixT"], w=["SG%d" % ti])
                    LD(lambda e, ti=ti: e.dma_start(out=KFTv[:, 2 * ti:2 * ti + 2, :], in_=kftok[:]), r=["kftok"], w=["KFT%d" % ti])
                    LD(lambda e, ti=ti: e.dma_start(out=VTv[:, 2 * ti:2 * ti + 2, :], in_=vtok[:]), r=["vtok"], w=["VT%d" % ti])
                    stop("p1a")

                stop("p1")
                V(lambda e: e.memset(Sst[:], 0.0), w=["Sst"])

                def load2(ti, bi):
                    t0 = ti * NT
                    load_x(ti, bi)

                load_x(0, 0)
                for ti in range(NTILES):
                    bi = ti % 2
                    s = 1 if ti == 0 else 0
                    t0 = ti * NT
                    x = xt[bi]
                    xn = "xt%d" % bi
                    LD(lambda e, t0=t0: e.dma_start(out=obf[:], in_=OBFv[:, :, t0:t0 + NT]), r=["OBF%d" % ti], w=["obf"])
                    LD(lambda e, t0=t0: e.dma_start(out=qtf[:], in_=QFv[:, :, t0:t0 + NT]), r=["QF%d" % ti], w=["qtf"])
                    LD(lambda e, t0=t0: e.dma_start(out=gsb[:], in_=GSv[:, :, t0:t0 + NT]), r=["GS%d" % ti], w=["gsb"])
                    LD(lambda e, t0=t0: e.dma_start(out=mixT[:, 4:8, :], in_=SGv[:, :, t0:t0 + NT]), r=["SG%d" % ti], w=["mixT"])
                    LD(lambda e, ti=ti: e.dma_start(out=kftok[:], in_=KFTv[:, 2 * ti:2 * ti + 2, :]), r=["KFT%d" % ti], w=["kftok"])
                    LD(lambda e, ti=ti: e.dma_start(out=vtok[:], in_=VTv[:, 2 * ti:2 * ti + 2, :]), r=["VT%d" % ti], w=["vtok"])
                    if ti + 1 < NTILES:
                        load_x(ti + 1, 1 - bi)
                    gc = ti * 4
                    for c in range(4):
                        j, cc = c // 2, c % 2
                        state_step(c, j, cc, kftok, "kftok", ERF[:, :, gc + c:gc + c + 1], ETRF[:, :, gc + c:gc + c + 1],
                                   DECF[:, :, gc + c:gc + c + 1], ku[0], ["ERF", "ETRF", "DECF"])
                        ku[0] += 1
                    for j in range(2):
                        for h in range(4):
                            for cc in range(2):
                                c = 2 * j + cc
                                T(lambda e, h=h, c=c, cc=cc, j=j: e.matmul(ps[5][:, h * 128 + cc * 64:h * 128 + (cc + 1) * 64],
                                                                           lhsT=Sbf[:, c, h, :],
                                                                           rhs=qtf[:, h, j * 128 + cc * 64:j * 128 + (cc + 1) * 64],
                                                                           start=True, stop=True), r=["Sbf%d" % c, "qtf"], w=["ps5"])
                        V(lambda e, j=j: e.tensor_tensor(out=ofull[:, :, j * 128:(j + 1) * 128], in0=ps[5][:].rearrange("p (h i) -> p h i", i=128),
                                                         in1=obf[:, :, j * 128:(j + 1) * 128], op=ALU.add), r=["ps5", "obf"], w=["obf"])
                    for h in range(4):
                        A(lambda e, h=h: e.activation(out=sqt[:], in_=ofull[:, h, :], func=AF.Square), r=["obf"], w=["sqt"])
                        T(lambda e: e.matmul(ps[6][:, 0:NT], lhsT=ones32, rhs=sqt[:], start=True, stop=True), r=["sqt", "cs"], w=["ps6_0", "ps6_1"])
                        V(lambda e: e.tensor_scalar(out=rt[:], in0=ps[6][:, 0:NT], scalar1=1.0 / 128, scalar2=EPS, op0=ALU.mult, op1=ALU.add),
                          r=["ps6_0", "ps6_1"], w=["rt"])
                        A(lambda e: e.activation(out=rt[:], in_=rt[:], func=AF.Sqrt), r=["rt"], w=["rt"])
                        V(lambda e: e.reciprocal(out=rt[:], in_=rt[:]), r=["rt"], w=["rt"])
                        V(lambda e, h=h: e.scalar_tensor_tensor(out=mt[:], in0=ofull[:, h, :], scalar=colsA[:, 48 + l * 4 + h:49 + l * 4 + h],
                                                                in1=rt[:], op0=ALU.mult, op1=ALU.mult), r=["obf", "rt", "colsA"], w=["mt"])
                        G(lambda e, h=h: e.tensor_tensor(out=mixT[:, h, :], in0=mt[:], in1=gsb[:, h, :], op=ALU.mult), r=["mt", "gsb"], w=["mixT"])
                    for j in range(2):
                        for hf in range(2):
                            k = pj[0] % 2
                            pj[0] += 1
                            pb = ps[k]
                            pn = "ps%d" % k
                            for c in range(8):
                                T(lambda e, c=c, j=j, hf=hf, pb=pb: e.matmul(pb[:, :], lhsT=mixT[:, c, j * 128:(j + 1) * 128],
                                                                             rhs=woutb[:, c, hf * 512:(hf + 1) * 512],
                                                                             start=(c == 0), stop=(c == 7)), r=["mixT", "woutb"], w=[pn])
                            V(lambda e, hf=hf, pb=pb: e.tensor_tensor(out=tmp2[:], in0=pb[:, :], in1=g1_bc[:, s, hf * 512:(hf + 1) * 512],
                                                                      op=ALU.mult), r=[pn, "g1_bc"], w=["tmp2"])
                            V(lambda e, j=j, hf=hf: e.tensor_tensor(out=x[:, j, hf * 512:(hf + 1) * 512], in0=tmp2[:],
                                                                    in1=x[:, j, hf * 512:(hf + 1) * 512], op=ALU.add), r=["tmp2", xn], w=[xn])
                    LD(lambda e, ti=ti: e.dma_start(out=XMv[:, 2 * ti:2 * ti + 2, :], in_=x[:]), r=[xn], w=["XM%d" % ti])
                    for j in range(2):
                        A(lambda e, j=j: e.activation(out=junk[:], in_=x[:, j, :], func=AF.Square, accum_out=stat[:, j:j + 1]),
                          r=[xn], w=["h2t", "stat"])
                    rstd_from_ss(stat[:, 0:2], stat[:, 2:4], 2, 1.0 / D)
                    for j in range(2):
                        sub = 2 * ti + j
                        V(lambda e, j=j: e.scalar_tensor_tensor(out=h2t[:], in0=x[:, j, :], scalar=stat[:, 2 + j:3 + j], in1=a2_bc[:, s, :],
                                                                op0=ALU.mult, op1=ALU.mult), r=[xn, "stat", "a2_bc"], w=["h2t"])
                        G(lambda e: e.tensor_tensor(out=h2t[:], in0=h2t[:], in1=b2_bc[:, s, :], op=ALU.add), r=["h2t", "b2_bc"], w=["h2t"])
                        GD(lambda e, sub=sub: e.dma_start(out=H2v[:, sub, :], in_=h2t[:]), r=["h2t"], w=["H2_%d" % sub])
                        for c in range(8):
                            pb = ps[2 + c // 4]
                            pn = "ps%d" % (2 + c // 4)
                            T(lambda e, c=c, pb=pb: e.transpose(out=pb[:, (c % 4) * 128:(c % 4 + 1) * 128], in_=h2t[:, c * 128:(c + 1) * 128],
                                                                identity=ident), r=["h2t", "cs"], w=[pn])
                        A(lambda e: e.activation(out=h2T[:, 0:4, :], in_=ps[2][:].rearrange("p (c t) -> p c t", t=128), func=AF.Copy),
                          r=["ps2"], w=["h2T"])
                        A(lambda e: e.activation(out=h2T[:, 4:8, :], in_=ps[3][:].rearrange("p (c t) -> p c t", t=128), func=AF.Copy),
                          r=["ps3"], w=["h2T"])
                        for c in range(8):
                            T(lambda e, c=c: e.matmul(ps[4][:, 0:36], lhsT=h2T[:, c, :], rhs=wr32[:, c, :], start=(c == 0), stop=False),
                              r=["h2T", "wr32"], w=["ps4"])
                        T(lambda e: e.matmul(ps[4][:, 0:36], lhsT=ones32[0:1, :], rhs=brow[0:1, :], start=False, stop=True),
                          r=["cs", "brow"], w=["ps4"])
                        V(lambda e: e.tensor_copy(Ls[:], ps[4][:, 0:36]), r=["ps4"], w=["Ls"])
                        R_ = ["rsm"]
                        V(lambda e: e.tensor_reduce(out=rsm[:, 0:1], in_=Ls[:, 0:4], axis=AX.X, op=ALU.max), r=["Ls"], w=R_)
                        V(lambda e: e.tensor_scalar(out=rsm[:, 1:2], in0=rsm[:, 0:1], scalar1=-1.0, scalar2=None, op0=ALU.mult), r=R_, w=R_)
                        A(lambda e: e.activation(out=rsm[:, 48:52], in_=Ls[:, 0:4], func=AF.Exp, bias=rsm[:, 1:2], accum_out=rsm[:, 2:3]),
                          r=["Ls"] + R_, w=R_)
                        V(lambda e: e.reciprocal(out=rsm[:, 3:4], in_=rsm[:, 2:3]), r=R_, w=R_)
                        V(lambda e: e.tensor_scalar(out=rsm[:, 4:8], in0=Ls[:, 0:4], scalar1=rsm[:, 0:1], scalar2=None, op0=ALU.is_equal),
                          r=["Ls"] + R_, w=R_)
                        V(lambda e: e.tensor_scalar(out=rsm[:, 8:16], in0=Ls[:, 4:12], scalar1=rsm[:, 4:5], scalar2=None, op0=ALU.mult),
                          r=["Ls"] + R_, w=R_)
                        for g in range(1, 4):
                            V(lambda e, g=g: e.scalar_tensor_tensor(out=rsm[:, 8:16], in0=Ls[:, 4 + 8 * g:12 + 8 * g], scalar=rsm[:, 4 + g:5 + g],
                                                                    in1=rsm[:, 8:16], op0=ALU.mult, op1=ALU.add), r=["Ls"] + R_, w=R_)
                        V(lambda e: e.max(out=rsm[:, 16:24], in_=rsm[:, 8:16]), r=R_, w=R_)
                        V(lambda e: e.tensor_scalar(out=rsm[:, 24:32], in0=rsm[:, 8:16], scalar1=rsm[:, 16:17], scalar2=None, op0=ALU.is_equal), r=R_, w=R_)
                        V(lambda e: e.tensor_scalar(out=rsm[:, 32:40], in0=rsm[:, 8:16], scalar1=rsm[:, 17:18], scalar2=None, op0=ALU.is_equal), r=R_, w=R_)
                        V(lambda e: e.tensor_tensor(out=rsm[:, 40:41], in0=rsm[:, 16:17], in1=rsm[:, 17:18], op=ALU.subtract), r=R_, w=R_)
                        A(lambda e: e.activation(out=rsm[:, 41:42], in_=rsm[:, 40:41], func=AF.Sigmoid), r=R_, w=R_)
                        V(lambda e, sub=sub: e.tensor_tensor(out=W12[:, sub, 0:1], in0=rsm[:, 41:42], in1=rsm[:, 3:4], op=ALU.mult), r=R_, w=["W12"])
                        V(lambda e, sub=sub: e.tensor_tensor(out=W12[:, sub, 1:2], in0=rsm[:, 3:4], in1=W12[:, sub, 0:1], op=ALU.subtract),
                          r=R_ + ["W12"], w=["W12"])
                        for g in range(4):
                            V(lambda e, g=g, sub=sub: e.tensor_scalar(out=M1all[:, sub, g * 8:(g + 1) * 8], in0=rsm[:, 24:32],
                                                                      scalar1=rsm[:, 4 + g:5 + g], scalar2=None, op0=ALU.mult), r=R_, w=["M1all"])
                            V(lambda e, g=g, sub=sub: e.tensor_scalar(out=M2all[:, sub, g * 8:(g + 1) * 8], in0=rsm[:, 32:40],
                                                                      scalar1=rsm[:, 4 + g:5 + g], scalar2=None, op0=ALU.mult), r=R_, w=["M2all"])
                P.barrier_all()
                sm.close(); open_stacks.remove(sm)

                stop("p2")
                se = newstack()
                pre = sb("pre", [128, NE], st=se)
                cnt_i = sb("cnt_i", [128, NE], I32, st=se)
                padf = sb("padf", [128, NE], st=se)
                incl = sb("incl", [128, NE], st=se)
                offs = sb("offs", [128, NE], st=se)
                cmp3 = sb("cmp3", [128, NB, NE], st=se)
                bef = sb("bef", [128, NB], st=se)
                tmpr = sb("tmpr", [128, NE], st=se)
                tmpr2 = sb("tmpr2", [128, NE], st=se)
                hrow = [sb("hrow%d" % i, [128, D], BF16, st=se) for i in range(2)]
                NW = 3
                wgb = [sb("wgb%d" % i, [128, 4096], BF16, st=se) for i in range(NW)]
                wub = [sb("wub%d" % i, [128, 4096], BF16, st=se) for i in range(NW)]
                wdb = [sb("wdb%d" % i, [128, 4096], BF16, st=se) for i in range(NW)]
                xbT = [sb("xbT%d" % i, [128, 8, 128], BF16, st=se) for i in range(2)]
                sgate = [sb("sgate%d" % i, [128, 512], st=se) for i in range(2)]
                actb = [sb("actb%d" % i, [128, 512], BF16, st=se) for i in range(2)]
                actT = [sb("actT%d" % i, [128, 4, 128], BF16, st=se) for i in range(2)]
                ybuf = [sb("ybuf%d" % i, [128, D], st=se) for i in range(2)]
                y1 = [sb("y1_%d" % i, [128, D], st=se) for i in range(2)]
                y2 = [sb("y2_%d" % i, [128, D], st=se) for i in range(2)]
                xm = [sb("xm%d" % i, [128, D], st=se) for i in range(2)]
                junk2 = sb("junk2", [128, D], st=se)
                st2 = sb("st2", [128, 4], st=se)
                g2_bc = sb("g2_bc", [128, 2, D], st=se)
                Rall = sb("Rall", [128, NSUB, NE], st=se)
                Mall = sb("Mall", [128, NSUB, NE], BF16, st=se)
                dest_f = sb("dest_f", [128, NSUB, 2], st=se)
                dest_i = sb("dest_i", [128, NSUB, 2], I32, st=se)
                widx = sb("widx", [128, NB, 2], I32, st=se)
                for s in range(2):
                    LD(lambda e, s=s: e.dma_start(out=g2_bc[:, s, :], in_=MODROW[l, s:s + 1, 5 * D:6 * D].to_broadcast([128, D])), w=["g2_bc"])

                zrow = sb("zrow", [128, D], BF16, st=se)
                V(lambda e: e.memset(zrow[:], 0.0), w=["zrow"])
                XBz = XB.rearrange("(n p) f -> p n f", p=128)
                for n0 in range(0, NSB, 12):
                    LD(lambda e, n0=n0: e.dma_start(out=XBz[:, n0:n0 + 12, :], in_=zrow[:].unsqueeze(1).to_broadcast([128, 12, D])),
                       r=["zrow"], w=["XBz"])
                V(lambda e: e.tensor_tensor(out=Mall[:], in0=M1all[:], in1=M2all[:], op=ALU.add), r=["M1all", "M2all"], w=["Mall"])
                V(lambda e: e.memset(pre[:], 0.0), w=["pre"])
                for i in range(NSUB):
                    T(lambda e, i=i: e.matmul(ps[0][:, 0:NE], lhsT=trib[:], rhs=Mall[:, i, :], start=True, stop=True), r=["Mall", "trib"], w=["ps0"])
                    T(lambda e, i=i: e.matmul(ps[1][:, 0:NE], lhsT=onesb[:], rhs=Mall[:, i, :], start=True, stop=True), r=["Mall", "onesb"], w=["ps1"])
                    V(lambda e, i=i: e.tensor_tensor(out=Rall[:, i, :], in0=ps[0][:, 0:NE], in1=pre[:], op=ALU.add), r=["ps0", "pre"], w=["Rall"])
                    V(lambda e: e.tensor_tensor(out=pre[:], in0=ps[1][:, 0:NE], in1=pre[:], op=ALU.add), r=["ps1", "pre"], w=["pre"])
                cmpT = cmp3[:].rearrange("p n e -> p (n e)").rearrange("p (e n) -> p e n", n=NB)
                V(lambda e: e.tensor_tensor(out=cmpT, in0=pre[:].unsqueeze(2).to_broadcast([128, NE, NB]),
                                            in1=nblk.unsqueeze(1).to_broadcast([128, NE, NB]), op=ALU.is_gt), r=["pre", "cs"], w=["cmp3"])
                V(lambda e: e.tensor_reduce(out=padf[:], in_=cmpT, axis=AX.X, op=ALU.add), r=["cmp3"], w=["padf"])
                V(lambda e: e.tensor_scalar(out=padf[:], in0=padf[:], scalar1=float(BLK), scalar2=None, op0=ALU.mult), r=["padf"], w=["padf"])
                V(lambda e: e.tensor_tensor_scan(out=incl[:], data0=ones32[:, 0:NE], data1=padf[:], initial=0.0, op0=ALU.mult, op1=ALU.add),
                  r=["padf", "cs"], w=["incl"])
                V(lambda e: e.tensor_tensor(out=offs[:], in0=incl[:], in1=padf[:], op=ALU.subtract), r=["incl", "padf"], w=["offs"])
                V(lambda e: e.tensor_tensor(out=cmp3[:], in0=nblk.unsqueeze(2).to_broadcast([128, NB, NE]),
                                            in1=incl[:].unsqueeze(1).to_broadcast([128, NB, NE]), op=ALU.is_ge), r=["incl", "cs"], w=["cmp3"])
                V(lambda e: e.tensor_reduce(out=bef[:], in_=cmp3[:], axis=AX.X, op=ALU.add), r=["cmp3"], w=["bef"])
                V(lambda e: e.tensor_scalar(out=bef[:], in0=bef[:], scalar1=31.0, scalar2=128.0, op0=ALU.min, op1=ALU.mult), r=["bef"], w=["bef"])
                V(lambda e: e.tensor_scalar(out=bef[:], in0=bef[:], scalar1=pcol, scalar2=None, op0=ALU.add), r=["bef", "cs"], w=["bef"])
                V(lambda e: e.tensor_copy(widx[:, :, 0], bef[:]), r=["bef"], w=["widx"])
                V(lambda e: e.tensor_scalar(out=bef[:], in0=bef[:], scalar1=4096.0, scalar2=None, op0=ALU.add), r=["bef"], w=["bef"])
                V(lambda e: e.tensor_copy(widx[:, :, 1], bef[:]), r=["bef"], w=["widx"])
                for i in range(NSUB):
                    V(lambda e, i=i: e.tensor_tensor(out=tmpr[:], in0=Rall[:, i, :], in1=offs[:], op=ALU.add), r=["Rall", "offs"], w=["tmpr"])
                    V(lambda e, i=i: e.scalar_tensor_tensor(out=tmpr2[:], in0=tmpr[:], scalar=1.0, in1=M1all[:, i, :], op0=ALU.mult, op1=ALU.mult,
                                                            accum_out=dest_f[:, i, 0:1]), r=["tmpr", "M1all"], w=["tmpr2", "dest_f"])
                    V(lambda e, i=i: e.scalar_tensor_tensor(out=tmpr2[:], in0=tmpr[:], scalar=1.0, in1=M2all[:, i, :], op0=ALU.mult, op1=ALU.mult,
                                                            accum_out=dest_f[:, i, 1:2]), r=["tmpr", "M2all"], w=["tmpr2", "dest_f"])
                V(lambda e: e.tensor_copy(dest_i[:], dest_f[:]), r=["dest_f"], w=["dest_i"])
                for i in range(NSUB):
                    hb = hrow[i % 2]
                    hn = "hrow%d" % (i % 2)
                    LD(lambda e, i=i, hb=hb: e.dma_start(out=hb[:], in_=H2v[:, i, :]), r=["H2_%d" % i], w=[hn])
                    for k in range(2):
                        GD(lambda e, i=i, k=k, hb=hb: e.indirect_dma_start(
                            out=XB, out_offset=bass.IndirectOffsetOnAxis(ap=dest_i[:, i, k:k + 1], axis=0), in_=hb[:], in_offset=None),
                           r=[hn, "dest_i", "XBz"], w=["XBs%d_%d" % (i, k)])
                P.barrier_all()

                stop("p3")
                XBv = XB.rearrange("(n p) f -> p n f", p=128)
                YBv = YB.rearrange("(n p) f -> p n f", p=128)

                def wload(n):
                    k = n % NW
                    for hf in range(2):
                        GD(lambda e, n=n, hf=hf, k=k: e.indirect_dma_start(
                            out=wgb[k][:, hf * 2048:(hf + 1) * 2048], out_offset=None, in_=wg2[l],
                            in_offset=bass.IndirectOffsetOnAxis(ap=widx[:, n, hf:hf + 1], axis=0)), r=["widx"], w=["wgb%d" % k])
                        GD(lambda e, n=n, hf=hf, k=k: e.indirect_dma_start(
                            out=wub[k][:, hf * 2048:(hf + 1) * 2048], out_offset=None, in_=wu2[l],
                            in_offset=bass.IndirectOffsetOnAxis(ap=widx[:, n, hf:hf + 1], axis=0)), r=["widx"], w=["wub%d" % k])
                        GD(lambda e, n=n, hf=hf, k=k: e.indirect_dma_start(
                            out=wdb[k][:, hf * 2048:(hf + 1) * 2048], out_offset=None, in_=wd2[l],
                            in_offset=bass.IndirectOffsetOnAxis(ap=widx[:, n, hf:hf + 1], axis=0)), r=["widx"], w=["wdb%d" % k])

                def stA1(sbi):
                    p = sbi % 2
                    hb, hn = hrow[p], "hrow%d" % p
                    LD(lambda e: e.dma_start(out=hb[:], in_=XBv[:, sbi, :]), r=[], w=[hn])
                    for c in range(8):
                        T(lambda e, c=c: e.matmul(ps[4 + c // 4][:, (c % 4) * 128:(c % 4 + 1) * 128], lhsT=hb[:, c * 128:(c + 1) * 128],
                                                  rhs=identb[:], start=True, stop=True), r=[hn, "identb"], w=["ps%d" % (4 + c // 4)])
                    A(lambda e: e.activation(out=xbT[p][:, 0:4, :].rearrange("p c t -> p (c t)"), in_=ps[4][:, :], func=AF.Copy),
                      r=["ps4"], w=["xbT%d" % p])
                    V(lambda e: e.tensor_copy(xbT[p][:, 4:8, :].rearrange("p c t -> p (c t)"), ps[5][:, :]), r=["ps5"], w=["xbT%d" % p])

                def stA2(sbi):
                    p = sbi % 2
                    kw = (sbi // (BLK // 128)) % NW
                    wg3 = wgb[kw][:].rearrange("p (c f) -> p c f", f=512)
                    wu3 = wub[kw][:].rearrange("p (c f) -> p c f", f=512)
                    for c in range(8):
                        T(lambda e, c=c: e.matmul(ps[2 * p][:, :], lhsT=xbT[p][:, c, :], rhs=wg3[:, c, :], start=(c == 0), stop=(c == 7)),
                          r=["xbT%d" % p, "wgb%d" % kw], w=["ps%d" % (2 * p)])
                    for c in range(8):
                        T(lambda e, c=c: e.matmul(ps[2 * p + 1][:, :], lhsT=xbT[p][:, c, :], rhs=wu3[:, c, :], start=(c == 0), stop=(c == 7)),
                          r=["xbT%d" % p, "wub%d" % kw], w=["ps%d" % (2 * p + 1)])

                def stA3(sbi):
                    p = sbi % 2
                    A(lambda e: e.activation(out=sgate[p][:], in_=ps[2 * p][:, :], func=AF.Silu), r=["ps%d" % (2 * p)], w=["sgate%d" % p])
                    V(lambda e: e.tensor_tensor(out=actb[p][:], in0=ps[2 * p + 1][:, :], in1=sgate[p][:], op=ALU.mult),
                      r=["ps%d" % (2 * p + 1), "sgate%d" % p], w=["actb%d" % p])

                def stB1(sbi):
                    p = sbi % 2
                    for c in range(4):
                        T(lambda e, c=c: e.matmul(ps[6][:, c * 128:(c + 1) * 128], lhsT=actb[p][:, c * 128:(c + 1) * 128], rhs=identb[:],
                                                  start=True, stop=True), r=["actb%d" % p, "identb"], w=["ps6"])
                    V(lambda e: e.tensor_copy(actT[p][:].rearrange("p c t -> p (c t)"), ps[6][:, :]), r=["ps6"], w=["actT%d" % p])

                def stB2(sbi):
                    p = sbi % 2
                    kw = (sbi // (BLK // 128)) % NW
                    wd3 = wdb[kw][:].rearrange("p (c f) -> p c f", f=1024)
                    for hf in range(2):
                        for c in range(4):
                            T(lambda e, c=c, hf=hf: e.matmul(ps[6 + hf][:, :], lhsT=actT[p][:, c, :], rhs=wd3[:, c, hf * 512:(hf + 1) * 512],
                                                             start=(c == 0), stop=(c == 3)), r=["actT%d" % p, "wdb%d" % kw], w=["ps%d" % (6 + hf)])
                    yb, yn = ybuf[p], "ybuf%d" % p
                    A(lambda e: e.activation(out=yb[:, 0:512], in_=ps[6][:, :], func=AF.Copy), r=["ps6"], w=[yn])
                    V(lambda e: e.tensor_copy(yb[:, 512:1024], ps[7][:, :]), r=["ps7"], w=[yn])
                    LD(lambda e: e.dma_start(out=YBv[:, sbi, :], in_=yb[:]), r=[yn], w=["YB%d" % sbi])

                SPB = BLK // 128
                wload(0)
                wload(1)
                stA1(0)
                stA2(0)
                stA3(0)
                for sbi in range(NSB):
                    n = sbi // SPB
                    if sbi % SPB == 0 and n + 2 < NB:
                        wload(n + 2)
                    if sbi + 1 < NSB:
                        stA1(sbi + 1)
                    stB1(sbi)
                    if sbi + 1 < NSB:
                        stA2(sbi + 1)
                        stA3(sbi + 1)
                    stB2(sbi)
                P.barrier_all()

                stop("p4")
                XOv = XS1.rearrange("(n p) f -> p n f", p=128)
                OUTv = out.rearrange("(n p) f -> p n f", p=128)
                for i in range(NSUB):
                    if last and i < 2:
                        continue
                    k = i % 2
                    s = 1 if i < 2 else 0
                    GD(lambda e, i=i, k=k: e.indirect_dma_start(out=y1[k][:], out_offset=None, in_=YB,
                                                                in_offset=bass.IndirectOffsetOnAxis(ap=dest_i[:, i, 0:1], axis=0)),
                       r=["dest_i"], w=["y1_%d" % k])
                    GD(lambda e, i=i, k=k: e.indirect_dma_start(out=y2[k][:], out_offset=None, in_=YB,
                                                                in_offset=bass.IndirectOffsetOnAxis(ap=dest_i[:, i, 1:2], axis=0)),
                       r=["dest_i"], w=["y2_%d" % k])
                    LD(lambda e, i=i, k=k: e.dma_start(out=xm[k][:], in_=XMv[:, i, :]), r=["XM%d" % (i // 2)], w=["xm%d" % k])
                    V(lambda e, i=i, k=k: e.tensor_scalar(out=y1[k][:], in0=y1[k][:], scalar1=W12[:, i, 0:1], scalar2=None, op0=ALU.mult),
                      r=["y1_%d" % k, "W12"], w=["y1_%d" % k])
                    V(lambda e, i=i, k=k: e.scalar_tensor_tensor(out=y1[k][:], in0=y2[k][:], scalar=W12[:, i, 1:2], in1=y1[k][:],
                                                                 op0=ALU.mult, op1=ALU.add), r=["y1_%d" % k, "y2_%d" % k, "W12"], w=["y1_%d" % k])
                    G(lambda e, k=k, s=s: e.tensor_tensor(out=y1[k][:], in0=y1[k][:], in1=g2_bc[:, s, :], op=ALU.mult),
                      r=["y1_%d" % k, "g2_bc"], w=["y1_%d" % k])
                    V(lambda e, k=k: e.tensor_tensor(out=xm[k][:], in0=xm[k][:], in1=y1[k][:], op=ALU.add), r=["xm%d" % k, "y1_%d" % k], w=["xm%d" % k])
                    if not last:
                        LD(lambda e, i=i, k=k: e.dma_start(out=XOv[:, i, :], in_=xm[k][:]), r=["xm%d" % k], w=["XIN%d" % (i // 2)])
                    else:
                        if dbg:
                            LD(lambda e, i=i, k=k: e.dma_start(out=XOv[:, i, :], in_=xm[k][:]), r=["xm%d" % k], w=["XIN%d" % (i // 2)])
                        A(lambda e, k=k: e.activation(out=junk2[:], in_=xm[k][:], func=AF.Square, accum_out=st2[:, 0:1]), r=["xm%d" % k], w=["junk2", "st2"])
                        V(lambda e: e.tensor_scalar(out=st2[:, 1:2], in0=st2[:, 0:1], scalar1=1.0 / D, scalar2=EPS, op0=ALU.mult, op1=ALU.add),
                          r=["st2"], w=["st2"])
                        A(lambda e: e.activation(out=st2[:, 1:2], in_=st2[:, 1:2], func=AF.Sqrt), r=["st2"], w=["st2"])
                        V(lambda e: e.reciprocal(out=st2[:, 2:3], in_=st2[:, 1:2]), r=["st2"], w=["st2"])
                        V(lambda e, k=k: e.scalar_tensor_tensor(out=xm[k][:], in0=xm[k][:], scalar=st2[:, 2:3], in1=fn_bc[:], op0=ALU.mult, op1=ALU.mult),
                          r=["xm%d" % k, "st2", "fn_bc"], w=["xm%d" % k])
                        LD(lambda e, i=i, k=k: e.dma_start(out=OUTv[:, i - 2, :], in_=xm[k][:]), r=["xm%d" % k], w=["OUT"], is_out=True)
                if dbg and last:
                    LD(lambda e: e.dma_start(out=YBv[:, NSB - 1, :], in_=fn_bc[:]), r=["fn_bc"], w=["YBdbg"])
                    LD(lambda e: e.dma_start(out=YBv[:, NSB - 2, 0:4], in_=st2[:]), r=["st2"], w=["YBdbg2"])
                P.barrier_all()
                se.close(); open_stacks.remove(se)
        except _Stop:
            for stx in reversed(open_stacks):
                stx.close()
        P.finish()
        block = E(nc.Block())
        P.replay(block)
    return nc


def _consts():
    c = np.zeros((128, CW), np.float32)
    c[:, 0:128] = np.eye(128, dtype=np.float32)
    tp = np.arange(128)[:, None]
    tt = np.arange(128)[None, :]
    c[:, 128:256] = (tp < tt).astype(np.float32)
    m = np.ones(256, np.float32)
    m[::64] = 0.0
    c[:, 256:512] = m[None, :]
    c[:, 512:640] = 1.0
    c[:, 640:640 + NB] = (np.arange(NB) * float(BLK))[None, :]
    c[:, 760] = np.arange(128)
    same = (tp // 64) == (tt // 64)
    m8 = np.zeros((128, 256), np.uint8)
    m8[:, 0:128] = (same & (tt >= tp)).astype(np.uint8)
    m8[:, 128:256] = (same & (tt <= tp)).astype(np.uint8)
    return c, m8


def _relay_gate(w):
    a = w.reshape(NE, 8, 128, 512).transpose(0, 2, 1, 3).reshape(NE, 128, 2, 2048)
    return np.ascontiguousarray(a.transpose(2, 0, 1, 3).reshape(2 * NE * 128, 2048))


def _relay_down(w):
    a = w.reshape(NE, 4, 128, 1024).transpose(0, 2, 1, 3).reshape(NE, 128, 2, 2048)
    return np.ascontiguousarray(a.transpose(2, 0, 1, 3).reshape(2 * NE * 128, 2048))


def make_in_maps(inputs, cores, small=False):
    f = lambda a: np.ascontiguousarray(np.asarray(a, dtype=np.float32))
    x, c, ctx, c_ctx = f(inputs['x']), f(inputs['c']), f(inputs['ctx']), f(inputs['c_ctx'])
    norm1, norm2 = f(inputs['norm1']), f(inputs['norm2'])
    cst, cst8 = _consts()
    shared = dict(
        w_mod=f(inputs['w_mod']), b_mod=f(inputs['b_mod']), w_in=f(inputs['w_in']), n2row=norm2,
        sgn=f(inputs['sgu_norm']), sgw=f(inputs['sgu_w']), sgb=f(inputs['sgu_b']).reshape(2, 512),
        w_out=f(inputs['w_out']),
        wrt=np.ascontiguousarray(np.concatenate([f(inputs['w_group']), f(inputs['w_router'])], axis=2)),
        brt=np.ascontiguousarray(np.concatenate([f(inputs['b_group']), f(inputs['b_router'])], axis=1)),
        wg2_0=_relay_gate(f(inputs['w_gate'][0])), wg2_1=_relay_gate(f(inputs['w_gate'][1])),
        wu2_0=_relay_gate(f(inputs['w_up'][0])), wu2_1=_relay_gate(f(inputs['w_up'][1])),
        wd2_0=_relay_down(f(inputs['w_down'][0])), wd2_1=_relay_down(f(inputs['w_down'][1])),
        fnorm=f(inputs['final_norm']).reshape(1, D), cst=cst, cst8=cst8,
    )
    if small:
        for k in list(shared):
            if k[:3] in ("wg2", "wu2", "wd2"):
                shared[k] = np.zeros((8, 8), np.float32)
    maps = []
    for b in cores:
        rows = np.zeros((72, 128), np.float32)
        for l in range(2):
            rows[l * 16:l * 16 + 8] = norm1[l].reshape(8, 128)
            rows[l * 16 + 8:l * 16 + 16] = norm2[l].reshape(8, 128)
        rows[32:48] = f(inputs['lb_logits']).reshape(16, 128)
        rows[48:56] = f(inputs['hgrn_norm']).reshape(8, 128)
        rows[56:64] = c[b].reshape(8, 128)
        rows[64:72] = c_ctx.reshape(8, 128)
        m = dict(shared)
        m['xs'] = np.ascontiguousarray(np.concatenate([ctx[b], x[b]], axis=0))
        m['rows_in'] = rows
        maps.append(m)
    return maps


def kernel(**inputs):
    n = 8
    nc = build_nc()
    in_maps = make_in_maps(inputs, list(range(n)))
    res = run_bass_kernel_spmd(nc, in_maps, core_ids=list(range(n)))
    return np.stack([np.asarray(r["out"], dtype=np.float32) for r in res.results], axis=0)
```

```python
import numpy as np
from contextlib import ExitStack
import concourse.bass as bass
import concourse.mybir as mybir
from concourse.bass_utils import run_bass_kernel_spmd

F32 = mybir.dt.float32
BF16 = mybir.dt.bfloat16
I32 = mybir.dt.int32
U8 = mybir.dt.uint8
AF = mybir.ActivationFunctionType
ALU = mybir.AluOpType
AX = mybir.AxisListType

D = 1024
TL = 4096
TC = 256
TOK = TL + TC
NT = 256
NTILES = TOK // NT
NSUB = TOK // 128
NCH = TOK // 64
INW = 3584
NE = 32
BLK = 128
NB = (NSUB * 2 * 128 + BLK - 1) // BLK + NE
NSB = NB * (BLK // 128)
EPS = 1e-6
CW = 1024


class _Rec:
    def __init__(self):
        self.calls = []

    def __getattr__(self, name):
        def f(*a, **k):
            self.calls.append((name, a, k))
            return self
        return f


def _bind(fn):
    rec = _Rec()
    fn(rec)
    assert len(rec.calls) == 1, rec.calls
    name, a, k = rec.calls[0]
    return lambda e: getattr(e, name)(*a, **k)


class Prog:
    ENG = ['pe', 'dve', 'act', 'pool', 'sp']
    NDS = 16

    def __init__(self, nc, stack):
        self.nc = nc
        self.q = {e: [] for e in self.ENG}
        self.cnt = {e: 0 for e in self.ENG}
        self.sems = {}
        for e in self.ENG:
            self.sems['s_' + e] = stack.enter_context(nc.semaphore('s_' + e))
        for e in ('sp', 'pool', 'act'):
            for i in range(self.NDS):
                self.sems['d_%s%d' % (e, i)] = stack.enter_context(nc.semaphore('d_%s%d' % (e, i)))
        self.dcnt = {e: 0 for e in ('sp', 'pool', 'act')}
        self.waited = {e: {} for e in self.ENG}
        self.lastw = {}
        self.readers = {}
        self.out_toks = []

    def _deps(self, reads, writes):
        deps = []
        for b in reads:
            if b in self.lastw:
                deps.append(self.lastw[b])
        for b in writes:
            if b in self.lastw:
                deps.append(self.lastw[b])
            deps.extend(self.readers.get(b, []))
        return deps

    def _wait(self, eng, tok):
        key, val, src = tok
        if src == 'pe' and eng == 'pe' and key == 's_pe':
            return
        if self.waited[eng].get(key, 0) >= val:
            return
        self.waited[eng][key] = val
        sem = self.sems[key]
        self.q[eng].append(lambda e, sem=sem, val=val: e.wait_ge(sem, val))

    def _record(self, tok, reads, writes):
        for b in reads:
            self.readers.setdefault(b, []).append(tok)
        for b in writes:
            self.lastw[b] = tok
            self.readers[b] = []

    def op(self, eng, fn, reads=(), writes=()):
        fn = _bind(fn)
        for tok in self._deps(reads, writes):
            self._wait(eng, tok)
        self.cnt[eng] += 1
        seq = self.cnt[eng]
        sem = self.sems['s_' + eng]
        self.q[eng].append(lambda e, fn=fn, sem=sem: fn(e).then_inc(sem, 1))
        tok = ('s_' + eng, seq, eng)
        self._record(tok, reads, writes)
        return tok

    def dma(self, eng, fn, reads=(), writes=(), is_out=False):
        fn = _bind(fn)
        for tok in self._deps(reads, writes):
            self._wait(eng, tok)
        k = self.dcnt[eng]
        self.dcnt[eng] += 1
        slot = k % self.NDS
        val = 16 * (k // self.NDS + 1)
        key = 'd_%s%d' % (eng, slot)
        if k >= self.NDS:
            self._wait(eng, (key, val - 16, eng))
        sem = self.sems[key]
        self.q[eng].append(lambda e, fn=fn, sem=sem: fn(e).then_inc(sem, 16))
        tok = (key, val, eng)
        self._record(tok, reads, writes)
        if is_out:
            self.out_toks.append(tok)
        return tok

    def barrier_all(self):
        toks = []
        for e in self.ENG:
            if self.cnt[e] > 0:
                toks.append(('s_' + e, self.cnt[e], e))
        for e in ('sp', 'pool', 'act'):
            k = self.dcnt[e]
            for slot in range(self.NDS):
                n = (k - slot + self.NDS - 1) // self.NDS if k > slot else 0
                if n > 0:
                    toks.append(('d_%s%d' % (e, slot), 16 * n, e))
        for e in self.ENG:
            for t in toks:
                if t[2] == 'pe' and e == 'pe' and t[0] == 's_pe':
                    pass
                key, val, src = t
                if self.waited[e].get(key, 0) >= val:
                    continue
                self.waited[e][key] = val
                sem = self.sems[key]
                self.q[e].append(lambda en, sem=sem, val=val: en.wait_ge(sem, val))
        self.lastw = {}
        self.readers = {}

    def finish(self):
        for tok in self.out_toks:
            self._wait('sp', tok)

    def replay(self, block):
        q = self.q

        @block.tensor
        def _(e):
            for f in q['pe']:
                f(e)

        @block.vector
        def _(e):
            for f in q['dve']:
                f(e)

        @block.scalar
        def _(e):
            for f in q['act']:
                f(e)

        @block.gpsimd
        def _(e):
            for f in q['pool']:
                f(e)

        @block.sync
        def _(e):
            for f in q['sp']:
                f(e)


class _Stop(Exception):
    pass


def build_nc(n_layers=2, dbg=None):
    nc = bass.Bass("TRN2", target_bir_lowering=False)

    def din(name, shape, dt=F32):
        return nc.dram_tensor(name, shape, dt, kind="ExternalInput").ap()

    def dint(name, shape, dt=F32):
        return nc.dram_tensor(name, shape, dt, kind=("ExternalOutput" if dbg else "Internal")).ap()

    xs = din("xs", [TOK, D])
    rows_in = din("rows_in", [72, 128])
    w_mod = din("w_mod", [2, D, 6 * D])
    b_mod = din("b_mod", [2, 6 * D])
    w_in = din("w_in", [2, D, INW])
    n2row = din("n2row", [2, D])
    sgn = din("sgn", [2, 512])
    sgw = din("sgw", [2, 4, 128, 128])
    sgb = din("sgb", [2, 512])
    w_out = din("w_out", [2, D, D])
    wrt = din("wrt", [2, D, 36])
    brt = din("brt", [2, 36])
    esh = [8, 8] if dbg in ("s0", "l0", "p1a", "p1", "p2", "p3", "ta", "tb", "tc", "td", "te", "tf", "tg") else [8192, 2048]
    wg2 = [din("wg2_%d" % i, esh) for i in range(2)]
    wu2 = [din("wu2_%d" % i, esh) for i in range(2)]
    wd2 = [din("wd2_%d" % i, esh) for i in range(2)]
    fnorm = din("fnorm", [1, D])
    cst = din("cst", [128, CW])
    cst8 = din("cst8", [128, 256], U8)
    out = nc.dram_tensor("out", [TL, D], F32, kind="ExternalOutput").ap()

    MODROW = dint("MODROW", [2, 2, 6 * D])
    OBF = dint("OBF", [512, TOK])
    QF = dint("QF", [512, TOK], BF16)
    GS = dint("GS", [512, TOK], BF16)
    SGD = dint("SGD", [512, TOK], BF16)
    KFT = dint("KFT", [TOK, 512], BF16)
    VT = dint("VT", [TOK, 512], BF16)
    XM = dint("XM", [TOK, D])
    H2 = dint("H2", [TOK, D], BF16)
    XB = dint("XB", [NSB * 128, D], BF16)
    YB = dint("YB", [NSB * 128, D])
    XS1 = dint("XS1", [TOK, D])

    with ExitStack() as top:
        E = top.enter_context
        P = Prog(nc, top)

        uid = [0]

        def sb(name, shape, dt=F32, st=top):
            uid[0] += 1
            return st.enter_context(nc.sbuf_tensor("%s_u%d" % (name, uid[0]), shape, dt))

        def V(fn, r=(), w=()):
            return P.op('dve', fn, r, w)

        def A(fn, r=(), w=()):
            return P.op('act', fn, r, w)

        def G(fn, r=(), w=()):
            return P.op('pool', fn, r, w)

        def T(fn, r=(), w=()):
            return P.op('pe', fn, r, w)

        def LD(fn, r=(), w=(), is_out=False):
            return P.dma('sp', fn, r, w, is_out)

        def GD(fn, r=(), w=()):
            return P.dma('pool', fn, r, w)

        cs = sb("cs", [128, CW])
        m8 = sb("m8", [128, 256], U8)
        identb = sb("identb", [128, 128], BF16)
        trib = sb("trib", [128, 128], BF16)
        onesb = sb("onesb", [128, 128], BF16)
        colsA = sb("colsA", [128, 72])
        colsM = sb("colsM", [128, 192])
        scv = sb("scv", [128, 16])
        lbc = sb("lbc", [128, 2, 8])
        oml = sb("oml", [128, 2, 8])
        lbm1 = sb("lbm1", [128, 2, 8])
        a1c = sb("a1c", [128, 2, 2, 8])
        ERF = sb("ERF", [128, 4, NCH])
        ETRF = sb("ETRF", [128, 4, NCH])
        DECF = sb("DECF", [128, 4, NCH])
        M1all = sb("M1all", [128, NSUB, NE], BF16)
        M2all = sb("M2all", [128, NSUB, NE], BF16)
        W12 = sb("W12", [128, NSUB, 2])
        fn_bc = sb("fn_bc", [128, D])
        Sst = sb("Sst", [128, 4, 128])
        Sbf = sb("Sbf", [128, 4, 4, 128], BF16)
        scb = [sb("scb%d" % i, [128, 128], BF16) for i in range(2)]
        scf = [sb("scf%d" % i, [128, 128], BF16) for i in range(2)]

        ps = [E(nc.psum_tensor("ps%d" % i, [128, 512], F32)) for i in range(8)]

        ident = cs[:, 0:128]
        tri32 = cs[:, 128:256]
        mskrow = cs[:, 256:512]
        ones32 = cs[:, 512:640]
        nblk = cs[:, 640:640 + NB]
        pcol = cs[:, 760:761]
        maskF = m8[:, 0:128]
        maskB = m8[:, 128:256]

        open_stacks = []

        def newstack():
            stx = ExitStack()
            open_stacks.append(stx)
            return stx

        def stop(tag):
            if dbg == tag:
                P.barrier_all()
                raise _Stop()

        try:
            LD(lambda e: e.dma_start(out=cs[:], in_=cst), w=["cs"])
            LD(lambda e: e.dma_start(out=m8[:], in_=cst8), w=["m8"])
            V(lambda e: e.tensor_copy(identb[:], ident), r=["cs"], w=["identb"])
            V(lambda e: e.tensor_copy(trib[:], tri32), r=["cs"], w=["trib"])
            V(lambda e: e.tensor_copy(onesb[:], ones32), r=["cs"], w=["onesb"])
            LD(lambda e: e.dma_start(out=fn_bc[:], in_=fnorm.to_broadcast([128, D])), w=["fn_bc"])
            for i in range(2):
                V(lambda e, i=i: e.memset(scb[i][:], 0.0), w=["scb%d" % i])
                V(lambda e, i=i: e.memset(scf[i][:], 0.0), w=["scf%d" % i])

            s0 = newstack()
            rowsA = sb("rowsA", [72, 128], st=s0)
            LD(lambda e: e.dma_start(out=rowsA[:], in_=rows_in), w=["rowsA"])
            T(lambda e: e.transpose(out=ps[0][:, 0:72], in_=rowsA[:], identity=cs[0:72, 0:72]), r=["rowsA", "cs"], w=["ps0"])
            V(lambda e: e.tensor_copy(colsA[:], ps[0][:, 0:72]), r=["ps0"], w=["colsA"])
            A(lambda e: e.activation(out=scv[:], in_=colsA[:, 56:72], func=AF.Silu), r=["colsA"], w=["scv"])
            V(lambda e: e.memset(lbc[:], 0.0), w=["lbc"])
            V(lambda e: e.tensor_tensor(out=lbc[:, 1, :], in0=colsA[:, 40:48], in1=colsA[:, 32:40], op=ALU.subtract), r=["colsA"], w=["lbc"])
            A(lambda e: e.activation(out=lbc[:, 1, :], in_=lbc[:, 1, :], func=AF.Sigmoid), r=["lbc"], w=["lbc"])
            V(lambda e: e.tensor_scalar(out=oml[:], in0=lbc[:], scalar1=-1.0, scalar2=1.0, op0=ALU.mult, op1=ALU.add), r=["lbc"], w=["oml"])
            V(lambda e: e.tensor_scalar(out=lbm1[:], in0=lbc[:], scalar1=-1.0, scalar2=None, op0=ALU.add), r=["lbc"], w=["lbm1"])

            wmb = [sb("wmb%d" % i, [128, 8, 512], st=s0) for i in range(2)]
            bmod_sb = sb("bmod_sb", [2, 6 * D], st=s0)
            modsb = sb("modsb", [2, 6 * D], st=s0)
            it = 0
            for l in range(2):
                LD(lambda e, l=l: e.dma_start(out=bmod_sb[:], in_=b_mod[l:l + 1, :].to_broadcast([2, 6 * D])), w=["bmod_sb"])
                for n in range(12):
                    wb = wmb[it % 2]
                    wn = "wmb%d" % (it % 2)
                    LD(lambda e, l=l, n=n, wb=wb: e.dma_start(
                        out=wb[:], in_=w_mod[l, :, n * 512:(n + 1) * 512].rearrange("(kc p) f -> p kc f", p=128)), w=[wn])
                    pb = ps[it % 2]
                    pn = "ps%d" % (it % 2)
                    for kc in range(8):
                        T(lambda e, kc=kc, wb=wb, pb=pb: e.matmul(pb[0:2, :], lhsT=scv[:, kc:16:8], rhs=wb[:, kc, :],
                                                                 start=(kc == 0), stop=(kc == 7)), r=[wn, "scv"], w=[pn])
                    V(lambda e, n=n, pb=pb: e.tensor_tensor(out=modsb[:, n * 512:(n + 1) * 512], in0=pb[0:2, :],
                                                            in1=bmod_sb[:, n * 512:(n + 1) * 512], op=ALU.add),
                      r=[pn, "bmod_sb"], w=["modsb"])
                    it += 1
                LD(lambda e, l=l: e.dma_start(out=MODROW[l], in_=modsb[:]), r=["modsb"], w=["MODROW"])
            rowsM = sb("rowsM", [96, 2, 128], st=s0)
            MR = MODROW.rearrange("l s (r q) -> l (s r) q", q=128)
            for l in range(2):
                LD(lambda e, l=l: e.dma_start(out=rowsM[:, l, :], in_=MR[l]), r=["MODROW"], w=["rowsM"])
            for l in range(2):
                T(lambda e, l=l: e.transpose(out=ps[2][:, l * 96:(l + 1) * 96], in_=rowsM[:, l, :], identity=cs[0:96, 0:96]),
                  r=["rowsM", "cs"], w=["ps2"])
            V(lambda e: e.tensor_copy(colsM[:], ps[2][:, 0:192]), r=["ps2"], w=["colsM"])

            def cm(l, s, k):
                o = l * 96 + s * 48 + k * 8
                return colsM[:, o:o + 8]

            for l in range(2):
                for s in range(2):
                    V(lambda e, l=l, s=s: e.scalar_tensor_tensor(out=a1c[:, l, s, :], in0=cm(l, s, 1), scalar=1.0,
                                                                 in1=colsA[:, l * 16:l * 16 + 8], op0=ALU.add, op1=ALU.mult),
                      r=["colsM", "colsA"], w=["a1c"])
            P.barrier_all()
            s0.close(); open_stacks.remove(s0)
            stop("s0")

            for l in range(n_layers):
                last = (l == 1)
                XIN = xs if l == 0 else XS1
                sm = newstack()
                winb = sb("winb", [128, 8, INW], BF16, st=sm)
                woutb = sb("woutb", [128, 8, D], BF16, st=sm)
                wr32 = sb("wr32", [128, 8, 36], st=sm)
                brow = sb("brow", [1, 36], st=sm)
                wsT = sb("wsT", [128, 4, 128], BF16, st=sm)
                bsrow = sb("bsrow", [1, 512], BF16, st=sm)
                sgn_bc = sb("sgn_bc", [128, 512], st=sm)
                g1_bc = sb("g1_bc", [128, 2, D], st=sm)
                a2_bc = sb("a2_bc", [128, 2, D], st=sm)
                b2_bc = sb("b2_bc", [128, 2, D], st=sm)
                for kc in range(8):
                    for hf in range(2):
                        GD(lambda e, kc=kc, hf=hf: e.dma_start(out=winb[:, kc, hf * 1792:(hf + 1) * 1792],
                                                               in_=w_in[l, kc * 128:(kc + 1) * 128, hf * 1792:(hf + 1) * 1792]),
                           w=["winb"])
                    GD(lambda e, kc=kc: e.dma_start(out=woutb[:, kc, :], in_=w_out[l, kc * 128:(kc + 1) * 128, :]), w=["woutb"])
                LD(lambda e: e.dma_start(out=wr32[:], in_=wrt[l].rearrange("(kc p) f -> p kc f", p=128)), w=["wr32"])
                LD(lambda e: e.dma_start(out=brow[:], in_=brt[l:l + 1, :]), w=["brow"])
                GD(lambda e: e.dma_start(out=bsrow[:], in_=sgb[l:l + 1, :]), w=["bsrow"])
                LD(lambda e: e.dma_start(out=sgn_bc[:], in_=sgn[l:l + 1, :].to_broadcast([128, 512])), w=["sgn_bc"])
                sl = newstack()
                wsn = sb("wsn", [128, 4, 128], st=sl)
                LD(lambda e: e.dma_start(out=wsn[:], in_=sgw[l].rearrange("h i j -> i h j")), w=["wsn"])
                for h in range(4):
                    T(lambda e, h=h: e.transpose(out=ps[0][:, h * 128:(h + 1) * 128], in_=wsn[:, h, :], identity=ident),
                      r=["wsn", "cs"], w=["ps0"])
                V(lambda e: e.tensor_copy(wsT[:].rearrange("p h i -> p (h i)"), ps[0][:]), r=["ps0"], w=["wsT"])
                tmpb = sb("tmpb", [128, D], st=sl)
                for s in range(2):
                    LD(lambda e, s=s: e.dma_start(out=g1_bc[:, s, :], in_=MODROW[l, s:s + 1, 2 * D:3 * D].to_broadcast([128, D])), w=["g1_bc"])
                    LD(lambda e, s=s: e.dma_start(out=b2_bc[:, s, :], in_=MODROW[l, s:s + 1, 3 * D:4 * D].to_broadcast([128, D])), w=["b2_bc"])
                    LD(lambda e, s=s: e.dma_start(out=a2_bc[:, s, :], in_=MODROW[l, s:s + 1, 4 * D:5 * D].to_broadcast([128, D])), w=["a2_bc"])
                LD(lambda e: e.dma_start(out=tmpb[:], in_=n2row[l:l + 1, :].to_broadcast([128, D])), w=["tmpb"])
                for s in range(2):
                    V(lambda e, s=s: e.scalar_tensor_tensor(out=a2_bc[:, s, :], in0=a2_bc[:, s, :], scalar=1.0, in1=tmpb[:],
                                                            op0=ALU.add, op1=ALU.mult), r=["a2_bc", "tmpb"], w=["a2_bc"])
                P.barrier_all()
                sl.close(); open_stacks.remove(sl)
                stop("l0")

                xt = [sb("xt%d" % i, [128, 2, D], st=sm) for i in range(2)]
                hT = sb("hT", [128, 8, NT], BF16, st=sm)
                q32 = sb("q32", [128, 4, NT], st=sm)
                sgt = [sb("sgt%d" % i, [128, NT], st=sm) for i in range(2)]
                lft = [sb("lft%d" % i, [128, NT], st=sm) for i in range(2)]
                kkt = [sb("kkt%d" % i, [128, NT], st=sm) for i in range(2)]
                Att = [sb("Att%d" % i, [128, NT], st=sm) for i in range(2)]
                e1t = [sb("e1t%d" % i, [128, NT], st=sm) for i in range(2)]
                qtf = sb("qtf", [128, 4, NT], BF16, st=sm)
                ktf = sb("ktf", [128, 4, NT], BF16, st=sm)
                qtb = sb("qtb", [128, 4, NT], BF16, st=sm)
                ktb = sb("ktb", [128, 4, NT], BF16, st=sm)
                gsb = sb("gsb", [128, 4, NT], BF16, st=sm)
                ug = sb("ug", [128, 4, NT], BF16, st=sm)
                mixT = sb("mixT", [128, 8, NT], BF16, st=sm)
                vtok = sb("vtok", [128, 2, 512], BF16, st=sm)
                vg = sb("vg", [128, 512], st=sm)
                vntok = sb("vntok", [128, 2, 512], BF16, st=sm)
                kbtok = sb("kbtok", [128, 2, 512], BF16, st=sm)
                kftok = sb("kftok", [128, 2, 512], BF16, st=sm)
                obf = sb("obf", [128, 4, NT], st=sm)
                ofull = obf
                sqt = sb("sqt", [128, NT], st=sm)
                rt = sb("rt", [128, NT], st=sm)
                mt = sb("mt", [128, NT], st=sm)
                stat = sb("stat", [128, 8], st=sm)
                erb = sb("erb", [128, 4, 4], st=sm)
                etrb = sb("etrb", [128, 4, 4], st=sm)
                decb = sb("decb", [128, 4, 4], st=sm)
                dtmp = sb("dtmp", [128, 4, 4], st=sm)
                utmp = sb("utmp", [128, 4, 128], st=sm)
                tmp2 = sb("tmp2", [128, 512], st=sm)
                h2t = sb("h2t", [128, D], st=sm)
                junk = h2t
                h2T = sb("h2T", [128, 8, 128], st=sm)
                Ls = sb("Ls", [128, 36], st=sm)
                rsm = sb("rsm", [128, 64], st=sm)

                OBFv = OBF.rearrange("(h p) t -> p h t", p=128)
                QFv = QF.rearrange("(h p) t -> p h t", p=128)
                GSv = GS.rearrange("(h p) t -> p h t", p=128)
                SGv = SGD.rearrange("(h p) t -> p h t", p=128)
                KFTv = KFT.rearrange("(n p) f -> p n f", p=128)
                VTv = VT.rearrange("(n p) f -> p n f", p=128)
                XINv = XIN.rearrange("(n p) f -> p n f", p=128)
                XMv = XM.rearrange("(n p) f -> p n f", p=128)
                H2v = H2.rearrange("(n p) f -> p n f", p=128)

                def rstd_from_ss(ssap, rap, n, inv):
                    V(lambda e: e.tensor_scalar(out=rap, in0=ssap, scalar1=inv, scalar2=EPS, op0=ALU.mult, op1=ALU.add),
                      r=["stat"], w=["stat"])
                    A(lambda e: e.activation(out=rap, in_=rap, func=AF.Sqrt), r=["stat"], w=["stat"])
                    V(lambda e: e.reciprocal(out=rap, in_=rap), r=["stat"], w=["stat"])

                def load_x(ti, bi):
                    LD(lambda e: e.dma_start(out=xt[bi][:], in_=XINv[:, 2 * ti:2 * ti + 2, :]), r=["XIN%d" % ti], w=["xt%d" % bi])

                def norm_to_hT(ti, bi, s):
                    x = xt[bi]
                    xn = "xt%d" % bi
                    for j in range(2):
                        A(lambda e, j=j: e.activation(out=junk[:], in_=x[:, j, :], func=AF.Square, accum_out=stat[:, j:j + 1]),
                          r=[xn], w=["h2t", "stat"])
                    rstd_from_ss(stat[:, 0:2], stat[:, 2:4], 2, 1.0 / D)
                    for j in range(2):
                        V(lambda e, j=j: e.tensor_scalar(out=x[:, j, :], in0=x[:, j, :], scalar1=stat[:, 2 + j:3 + j], scalar2=None,
                                                         op0=ALU.mult), r=[xn, "stat"], w=[xn])
                    for c in range(8):
                        pb = ps[2 + (c % 2)]
                        pn = "ps%d" % (2 + (c % 2))
                        for j in range(2):
                            T(lambda e, c=c, j=j, pb=pb: e.transpose(out=pb[:, j * 128:(j + 1) * 128], in_=x[:, j, c * 128:(c + 1) * 128],
                                                                     identity=ident), r=[xn, "cs"], w=[pn])
                        if c % 2 == 0:
                            V(lambda e, c=c, pb=pb: e.tensor_scalar(out=hT[:, c, :], in0=pb[:, 0:NT], scalar1=a1c[:, l, s, c:c + 1],
                                                                    scalar2=cm(l, s, 0)[:, c:c + 1], op0=ALU.mult, op1=ALU.add),
                              r=[pn, "a1c", "colsM"], w=["hT"])
                        else:
                            A(lambda e, c=c, pb=pb: e.activation(out=hT[:, c, :], in_=pb[:, 0:NT], func=AF.Identity,
                                                                 bias=cm(l, s, 0)[:, c:c + 1], scale=a1c[:, l, s, c:c + 1]),
                              r=[pn, "a1c", "colsM"], w=["hT"])

                pj = [0]

                def proj_fm(m):
                    k = pj[0] % 2
                    pj[0] += 1
                    pb = ps[k]
                    for c in range(8):
                        T(lambda e, c=c, pb=pb: e.matmul(pb[:, 0:NT], lhsT=winb[:, c, m * 128:(m + 1) * 128], rhs=hT[:, c, :],
                                                         start=(c == 0), stop=(c == 7)), r=["winb", "hT"], w=["ps%d" % k])
                    return pb, "ps%d" % k

                def proj_tm(j, col0):
                    k = pj[0] % 2
                    pj[0] += 1
                    pb = ps[k]
                    for c in range(8):
                        T(lambda e, c=c, pb=pb: e.matmul(pb[:, :], lhsT=hT[:, c, j * 128:(j + 1) * 128], rhs=winb[:, c, col0:col0 + 512],
                                                         start=(c == 0), stop=(c == 7)), r=["winb", "hT"], w=["ps%d" % k])
                    return pb, "ps%d" % k

                gi = [0]

                def gates(h, dr, ti):
                    k = gi[0] % 2
                    gi[0] += 1
                    pb, pn = proj_fm(4 + 4 * dr + h)
                    sg_, lf_, kk_, I_, A_, e1_, e2_ = sgt[k], lft[k], kkt[k], sgt[k], Att[k], e1t[k], Att[k]
                    nm = ["sgt%d" % k, "lft%d" % k, "kkt%d" % k, "sgt%d" % k, "Att%d" % k, "e1t%d" % k, "Att%d" % k]
                    ci = dr * 4 + h
                    A(lambda e: e.activation(out=sg_[:], in_=pb[:, 0:NT], func=AF.Sigmoid), r=[pn], w=[nm[0]])
                    A(lambda e: e.activation(out=lf_[:], in_=sg_[:], func=AF.Ln, bias=lbc[:, l, ci:ci + 1], scale=oml[:, l, ci:ci + 1]),
                      r=[nm[0], "lbc", "oml"], w=[nm[1]])
                    V(lambda e: e.tensor_scalar(out=kk_[:], in0=sg_[:], scalar1=-1.0, scalar2=lbm1[:, l, ci:ci + 1], op0=ALU.add, op1=ALU.mult),
                      r=[nm[0], "lbm1"], w=[nm[2]])
                    V(lambda e: e.tensor_tensor_scan(out=I_[:], data0=mskrow, data1=lf_[:], initial=0.0, op0=ALU.mult, op1=ALU.add),
                      r=[nm[1], "cs"], w=[nm[3]])
                    I3 = I_[:].rearrange("p (c t) -> p c t", t=64)
                    A3 = A_[:].rearrange("p (c t) -> p c t", t=64)
                    if dr == 0:
                        V(lambda e: e.tensor_tensor(out=A3, in0=I3, in1=I3[:, :, 31:32].to_broadcast([128, 4, 64]), op=ALU.subtract),
                          r=[nm[3]], w=[nm[4]])
                        A(lambda e: e.activation(out=e1_[:], in_=A_[:], func=AF.Exp), r=[nm[4]], w=[nm[5]])
                        A(lambda e: e.activation(out=e2_[:], in_=A_[:], func=AF.Exp, scale=-1.0), r=[nm[4]], w=[nm[6]])
                        G(lambda e: e.tensor_tensor(out=qtf[:, h, :], in0=q32[:, h, :], in1=e1_[:], op=ALU.mult), r=["q32", nm[5]], w=["qtf"])
                        G(lambda e: e.tensor_tensor(out=ktf[:, h, :], in0=kk_[:], in1=e2_[:], op=ALU.mult), r=[nm[2], nm[6]], w=["ktf"])
                        gc = ti * 4
                        A(lambda e: e.activation(out=ERF[:, h, gc:gc + 4], in_=I3[:, :, 31], func=AF.Exp), r=[nm[3]], w=["ERF"])
                        A(lambda e: e.activation(out=DECF[:, h, gc:gc + 4], in_=I3[:, :, 63], func=AF.Exp), r=[nm[3]], w=["DECF"])
                        V(lambda e: e.tensor_tensor(out=dtmp[:, h, :], in0=I3[:, :, 63], in1=I3[:, :, 31], op=ALU.subtract), r=[nm[3]], w=["dtmp"])
                        A(lambda e: e.activation(out=ETRF[:, h, gc:gc + 4], in_=dtmp[:, h, :], func=AF.Exp), r=["dtmp"], w=["ETRF"])
                    else:
                        V(lambda e: e.tensor_tensor(out=lf_[:], in0=I_[:], in1=lf_[:], op=ALU.subtract), r=[nm[3], nm[1]], w=[nm[1]])
                        E3 = lf_[:].rearrange("p (c t) -> p c t", t=64)
                        V(lambda e: e.tensor_tensor(out=A3, in0=E3, in1=E3[:, :, 32:33].to_broadcast([128, 4, 64]), op=ALU.subtract),
                          r=[nm[1]], w=[nm[4]])
                        A(lambda e: e.activation(out=e1_[:], in_=A_[:], func=AF.Exp, scale=-1.0), r=[nm[4]], w=[nm[5]])
                        A(lambda e: e.activation(out=e2_[:], in_=A_[:], func=AF.Exp), r=[nm[4]], w=[nm[6]])
                        G(lambda e: e.tensor_tensor(out=qtb[:, h, :], in0=q32[:, h, :], in1=e1_[:], op=ALU.mult), r=["q32", nm[5]], w=["qtb"])
                        G(lambda e: e.tensor_tensor(out=ktb[:, h, :], in0=kk_[:], in1=e2_[:], op=ALU.mult), r=[nm[2], nm[6]], w=["ktb"])
                        A(lambda e: e.activation(out=etrb[:, h, :], in_=E3[:, :, 32], func=AF.Exp), r=[nm[1]], w=["etrb"])
                        A(lambda e: e.activation(out=decb[:, h, :], in_=I3[:, :, 63], func=AF.Exp), r=[nm[3]], w=["decb"])
                        V(lambda e: e.tensor_tensor(out=dtmp[:, h, :], in0=I3[:, :, 63], in1=E3[:, :, 32], op=ALU.subtract), r=[nm[3], nm[1]], w=["dtmp"])
                        A(lambda e: e.activation(out=erb[:, h, :], in_=dtmp[:, h, :], func=AF.Exp), r=["dtmp"], w=["erb"])

                def state_step(c, j, cc, ktok, ktn, er_ap, etr_ap, dec_ap, kU, scn):
                    pb = ps[4 + kU % 2]
                    pn = "ps%d" % (4 + kU % 2)
                    for h in range(4):
                        T(lambda e, h=h, pb=pb: e.matmul(pb[:, h * 128:(h + 1) * 128],
                                                         lhsT=ktok[cc * 64:(cc + 1) * 64, j, h * 128:(h + 1) * 128],
                                                         rhs=vtok[cc * 64:(cc + 1) * 64, j, h * 128:(h + 1) * 128], start=True, stop=True),
                          r=[ktn, "vtok"], w=[pn])
                    V(lambda e: e.tensor_tensor(out=Sbf[:, c, :, :], in0=Sst[:], in1=er_ap.to_broadcast([128, 4, 128]), op=ALU.mult),
                      r=["Sst"] + scn, w=["Sbf%d" % c])
                    V(lambda e, pb=pb: e.tensor_tensor(out=utmp[:], in0=pb[:].rearrange("p (h e) -> p h e", e=128),
                                                       in1=etr_ap.to_broadcast([128, 4, 128]), op=ALU.mult), r=[pn] + scn, w=["utmp"])
                    V(lambda e: e.tensor_tensor(out=Sst[:], in0=Sst[:], in1=dec_ap.to_broadcast([128, 4, 128]), op=ALU.mult),
                      r=["Sst"] + scn, w=["Sst"])
                    V(lambda e: e.tensor_tensor(out=Sst[:], in0=Sst[:], in1=utmp[:], op=ALU.add), r=["Sst", "utmp"], w=["Sst"])

                ku = [0]

                V(lambda e: e.memset(Sst[:], 0.0), w=["Sst"])
                order1 = [0] + list(range(NTILES - 1, 0, -1))
                load_x(order1[0], 0)
                for oi, ti in enumerate(order1):
                    bi = oi % 2
                    s = 1 if ti == 0 else 0
                    if oi + 1 < len(order1):
                        load_x(order1[oi + 1], 1 - bi)
                    norm_to_hT(ti, bi, s)
                    stop("ta")
                    for j in range(2):
                        pb, pn = proj_tm(j, 1536)
                        A(lambda e, j=j, pb=pb: e.activation(out=vtok[:, j, :], in_=pb[:, :], func=AF.Copy), r=[pn], w=["vtok"])
                        pb, pn = proj_tm(j, 3072)
                        A(lambda e, pb=pb: e.activation(out=vg[:], in_=pb[:, :], func=AF.Gelu_apprx_tanh), r=[pn], w=["vg"])
                        A(lambda e, j=j: e.activation(out=junk[:, 0:512], in_=vg[:], func=AF.Square, accum_out=stat[:, 4 + j:5 + j]),
                          r=["vg"], w=["h2t", "stat"])
                        rstd_from_ss(stat[:, 4 + j:5 + j], stat[:, 6 + j:7 + j], 1, 1.0 / 512)
                        V(lambda e, j=j: e.scalar_tensor_tensor(out=vntok[:, j, :], in0=vg[:], scalar=stat[:, 6 + j:7 + j], in1=sgn_bc[:],
                                                                op0=ALU.mult, op1=ALU.mult), r=["vg", "stat", "sgn_bc"], w=["vntok"])
                    stop("tb")
                    for h in range(4):
                        pb, pn = proj_fm(h)
                        A(lambda e, h=h, pb=pb: e.activation(out=q32[:, h, :], in_=pb[:, 0:NT], func=AF.Copy), r=[pn], w=["q32"])
                    for h in range(4):
                        gates(h, 0, ti)
                        gates(h, 1, ti)
                    for h in range(4):
                        pb, pn = proj_fm(16 + h)
                        A(lambda e, h=h, pb=pb: e.activation(out=gsb[:, h, :], in_=pb[:, 0:NT], func=AF.Silu), r=[pn], w=["gsb"])
                        pb, pn = proj_fm(20 + h)
                        A(lambda e, h=h, pb=pb: e.activation(out=ug[:, h, :], in_=pb[:, 0:NT], func=AF.Gelu_apprx_tanh), r=[pn], w=["ug"])
                    stop("tc")
                    for j in range(2):
                        pb = ps[2 + j]
                        pn = "ps%d" % (2 + j)
                        for h in range(4):
                            T(lambda e, h=h, j=j, pb=pb: e.matmul(pb[:, h * 128:(h + 1) * 128], lhsT=vntok[:, j, h * 128:(h + 1) * 128],
                                                                  rhs=wsT[:, h, :], start=True, stop=False), r=["vntok", "wsT"], w=[pn])
                            T(lambda e, h=h, pb=pb: e.matmul(pb[:, h * 128:(h + 1) * 128], lhsT=onesb[0:1, :],
                                                             rhs=bsrow[0:1, h * 128:(h + 1) * 128], start=False, stop=True),
                              r=["onesb", "bsrow"], w=[pn])
                        V(lambda e, j=j, pb=pb: e.tensor_tensor(out=mixT[:, 4:8, j * 128:(j + 1) * 128],
                                                                in0=pb[:].rearrange("p (h i) -> p h i", i=128),
                                                                in1=ug[:, :, j * 128:(j + 1) * 128], op=ALU.mult), r=[pn, "ug"], w=["mixT"])
                    stop("td")
                    for j in range(2):
                        for h in range(4):
                            T(lambda e, h=h, j=j: e.matmul(ps[2][:, h * 128:(h + 1) * 128], lhsT=ktb[:, h, j * 128:(j + 1) * 128],
                                                           rhs=identb[:], start=True, stop=True), r=["ktb", "identb"], w=["ps2"])
                            T(lambda e, h=h, j=j: e.matmul(ps[3][:, h * 128:(h + 1) * 128], lhsT=ktf[:, h, j * 128:(j + 1) * 128],
                                                           rhs=identb[:], start=True, stop=True), r=["ktf", "identb"], w=["ps3"])
                        A(lambda e, j=j: e.activation(out=kbtok[:, j, :], in_=ps[2][:, :], func=AF.Copy), r=["ps2"], w=["kbtok"])
                        V(lambda e, j=j: e.tensor_copy(kftok[:, j, :], ps[3][:, :]), r=["ps3"], w=["kftok"])
                    stop("te")
                    for c in (3, 2, 1, 0):
                        j, cc = c // 2, c % 2
                        state_step(c, j, cc, kbtok, "kbtok", erb[:, :, c:c + 1], etrb[:, :, c:c + 1], decb[:, :, c:c + 1], ku[0], ["erb", "etrb", "decb"])
                        ku[0] += 1
                    stop("tf")
                    for j in (1, 0):
                        for h in range(4):
                            k = h % 2
                            off = k * 256
                            jc = slice(j * 128, (j + 1) * 128)
                            T(lambda e, h=h, jc=jc, off=off: e.matmul(ps[6][:, off:off + 128], lhsT=ktb[:, h, jc], rhs=qtb[:, h, jc],
                                                                      start=True, stop=True), r=["ktb", "qtb"], w=["ps6_%d" % k])
                            T(lambda e, h=h, jc=jc, off=off: e.matmul(ps[6][:, off + 128:off + 256], lhsT=ktf[:, h, jc], rhs=qtf[:, h, jc],
                                                                      start=True, stop=True), r=["ktf", "qtf"], w=["ps6_%d" % k])
                            V(lambda e, k=k, off=off: e.copy_predicated(out=scb[k][:], mask=maskB, data=ps[6][:, off:off + 128]),
                              r=["ps6_%d" % k, "m8"], w=["scb%d" % k])
                            V(lambda e, k=k, off=off: e.copy_predicated(out=scf[k][:], mask=maskF, data=ps[6][:, off + 128:off + 256]),
                              r=["ps6_%d" % k, "m8"], w=["scf%d" % k])
                            oo = ps[5][:, h * 128:(h + 1) * 128]
                            T(lambda e, h=h, j=j, k=k, oo=oo: e.matmul(oo, lhsT=vtok[:, j, h * 128:(h + 1) * 128], rhs=scb[k][:],
                                                                       start=True, stop=False), r=["vtok", "scb%d" % k], w=["ps5"])
                            T(lambda e, h=h, j=j, k=k, oo=oo: e.matmul(oo, lhsT=vtok[:, j, h * 128:(h + 1) * 128], rhs=scf[k][:],
                                                                       start=False, stop=False), r=["vtok", "scf%d" % k], w=["ps5"])
                            for cc in (1, 0):
                                c = 2 * j + cc
                                T(lambda e, h=h, c=c, cc=cc, j=j: e.matmul(ps[5][:, h * 128 + cc * 64:h * 128 + (cc + 1) * 64],
                                                                           lhsT=Sbf[:, c, h, :],
                                                                           rhs=qtb[:, h, j * 128 + cc * 64:j * 128 + (cc + 1) * 64],
                                                                           start=False, stop=(cc == 0)), r=["Sbf%d" % c, "qtb"], w=["ps5"])
                        A(lambda e, j=j: e.activation(out=obf[:, :, j * 128:(j + 1) * 128], in_=ps[5][:].rearrange("p (h i) -> p h i", i=128),
                                                      func=AF.Copy), r=["ps5"], w=["obf"])
                    stop("tg")
                    t0 = ti * NT
                    LD(lambda e, t0=t0: e.dma_start(out=OBFv[:, :, t0:t0 + NT], in_=obf[:]), r=["obf"], w=["OBF%d" % ti])
                    LD(lambda e, t0=t0: e.dma_start(out=QFv[:, :, t0:t0 + NT], in_=qtf[:]), r=["qtf"], w=["QF%d" % ti])
                    LD(lambda e, t0=t0: e.dma_start(out=GSv[:, :, t0:t0 + NT], in_=gsb[:]), r=["gsb"], w=["GS%d" % ti])
                    LD(lambda e, t0=t0: e.dma_start(out=SGv[:, :, t0:t0 + NT], in_=mixT[:, 4:8, :]), r=["mixT"], w=["SG%d" % ti])
                    LD(lambda e, ti=ti: e.dma_start(out=KFTv[:, 2 * ti:2 * ti + 2, :], in_=kftok[:]), r=["kftok"], w=["KFT%d" % ti])
                    LD(lambda e, ti=ti: e.dma_start(out=VTv[:, 2 * ti:2 * ti + 2, :], in_=vtok[:]), r=["vtok"], w=["VT%d" % ti])
                    stop("p1a")

                stop("p1")
                V(lambda e: e.memset(Sst[:], 0.0), w=["Sst"])

                def load2(ti, bi):
                    t0 = ti * NT
                    load_x(ti, bi)

                load_x(0, 0)
                for ti in range(NTILES):
                    bi = ti % 2
                    s = 1 if ti == 0 else 0
                    t0 = ti * NT
                    x = xt[bi]
                    xn = "xt%d" % bi
                    LD(lambda e, t0=t0: e.dma_start(out=obf[:], in_=OBFv[:, :, t0:t0 + NT]), r=["OBF%d" % ti], w=["obf"])
                    LD(lambda e, t0=t0: e.dma_start(out=qtf[:], in_=QFv[:, :, t0:t0 + NT]), r=["QF%d" % ti], w=["qtf"])
                    LD(lambda e, t0=t0: e.dma_start(out=gsb[:], in_=GSv[:, :, t0:t0 + NT]), r=["GS%d" % ti], w=["gsb"])
                    LD(lambda e, t0=t0: e.dma_start(out=mixT[:, 4:8, :], in_=SGv[:, :, t0:t0 + NT]), r=["SG%d" % ti], w=["mixT"])
                    LD(lambda e, ti=ti: e.dma_start(out=kftok[:], in_=KFTv[:, 2 * ti:2 * ti + 2, :]), r=["KFT%d" % ti], w=["kftok"])
                    LD(lambda e, ti=ti: e.dma_start(out=vtok[:], in_=VTv[:, 2 * ti:2 * ti + 2, :]), r=["VT%d" % ti], w=["vtok"])
                    if ti + 1 < NTILES:
                        load_x(ti + 1, 1 - bi)
                    gc = ti * 4
                    for c in range(4):
                        j, cc = c // 2, c % 2
                        state_step(c, j, cc, kftok, "kftok", ERF[:, :, gc + c:gc + c + 1], ETRF[:, :, gc + c:gc + c + 1],
                                   DECF[:, :, gc + c:gc + c + 1], ku[0], ["ERF", "ETRF", "DECF"])
                        ku[0] += 1
                    for j in range(2):
                        for h in range(4):
                            for cc in range(2):
                                c = 2 * j + cc
                                T(lambda e, h=h, c=c, cc=cc, j=j: e.matmul(ps[5][:, h * 128 + cc * 64:h * 128 + (cc + 1) * 64],
                                                                           lhsT=Sbf[:, c, h, :],
                                                                           rhs=qtf[:, h, j * 128 + cc * 64:j * 128 + (cc + 1) * 64],
                                                                           start=True, stop=True), r=["Sbf%d" % c, "qtf"], w=["ps5"])
                        V(lambda e, j=j: e.tensor_tensor(out=ofull[:, :, j * 128:(j + 1) * 128], in0=ps[5][:].rearrange("p (h i) -> p h i", i=128),
                                                         in1=obf[:, :, j * 128:(j + 1) * 128], op=ALU.add), r=["ps5", "obf"], w=["obf"])
                    for h in range(4):
                        A(lambda e, h=h: e.activation(out=sqt[:], in_=ofull[:, h, :], func=AF.Square), r=["obf"], w=["sqt"])
                        T(lambda e: e.matmul(ps[6][:, 0:NT], lhsT=ones32, rhs=sqt[:], start=True, stop=True), r=["sqt", "cs"], w=["ps6_0", "ps6_1"])
                        V(lambda e: e.tensor_scalar(out=rt[:], in0=ps[6][:, 0:NT], scalar1=1.0 / 128, scalar2=EPS, op0=ALU.mult, op1=ALU.add),
                          r=["ps6_0", "ps6_1"], w=["rt"])
                        A(lambda e: e.activation(out=rt[:], in_=rt[:], func=AF.Sqrt), r=["rt"], w=["rt"])
                        V(lambda e: e.reciprocal(out=rt[:], in_=rt[:]), r=["rt"], w=["rt"])
                        V(lambda e, h=h: e.scalar_tensor_tensor(out=mt[:], in0=ofull[:, h, :], scalar=colsA[:, 48 + l * 4 + h:49 + l * 4 + h],
                                                                in1=rt[:], op0=ALU.mult, op1=ALU.mult), r=["obf", "rt", "colsA"], w=["mt"])
                        G(lambda e, h=h: e.tensor_tensor(out=mixT[:, h, :], in0=mt[:], in1=gsb[:, h, :], op=ALU.mult), r=["mt", "gsb"], w=["mixT"])
                    for j in range(2):
                        for hf in range(2):
                            k = pj[0] % 2
                            pj[0] += 1
                            pb = ps[k]
                            pn = "ps%d" % k
                            for c in range(8):
                                T(lambda e, c=c, j=j, hf=hf, pb=pb: e.matmul(pb[:, :], lhsT=mixT[:, c, j * 128:(j + 1) * 128],
                                                                             rhs=woutb[:, c, hf * 512:(hf + 1) * 512],
                                                                             start=(c == 0), stop=(c == 7)), r=["mixT", "woutb"], w=[pn])
                            V(lambda e, hf=hf, pb=pb: e.tensor_tensor(out=tmp2[:], in0=pb[:, :], in1=g1_bc[:, s, hf * 512:(hf + 1) * 512],
                                                                      op=ALU.mult), r=[pn, "g1_bc"], w=["tmp2"])
                            V(lambda e, j=j, hf=hf: e.tensor_tensor(out=x[:, j, hf * 512:(hf + 1) * 512], in0=tmp2[:],
                                                                    in1=x[:, j, hf * 512:(hf + 1) * 512], op=ALU.add), r=["tmp2", xn], w=[xn])
                    LD(lambda e, ti=ti: e.dma_start(out=XMv[:, 2 * ti:2 * ti + 2, :], in_=x[:]), r=[xn], w=["XM%d" % ti])
                    for j in range(2):
                        A(lambda e, j=j: e.activation(out=junk[:], in_=x[:, j, :], func=AF.Square, accum_out=stat[:, j:j + 1]),
                          r=[xn], w=["h2t", "stat"])
                    rstd_from_ss(stat[:, 0:2], stat[:, 2:4], 2, 1.0 / D)
                    for j in range(2):
                        sub = 2 * ti + j
                        V(lambda e, j=j: e.scalar_tensor_tensor(out=h2t[:], in0=x[:, j, :], scalar=stat[:, 2 + j:3 + j], in1=a2_bc[:, s, :],
                                                                op0=ALU.mult, op1=ALU.mult), r=[xn, "stat", "a2_bc"], w=["h2t"])
                        G(lambda e: e.tensor_tensor(out=h2t[:], in0=h2t[:], in1=b2_bc[:, s, :], op=ALU.add), r=["h2t", "b2_bc"], w=["h2t"])
                        GD(lambda e, sub=sub: e.dma_start(out=H2v[:, sub, :], in_=h2t[:]), r=["h2t"], w=["H2_%d" % sub])
                        for c in range(8):
                            pb = ps[2 + c // 4]
                            pn = "ps%d" % (2 + c // 4)
                            T(lambda e, c=c, pb=pb: e.transpose(out=pb[:, (c % 4) * 128:(c % 4 + 1) * 128], in_=h2t[:, c * 128:(c + 1) * 128],
                                                                identity=ident), r=["h2t", "cs"], w=[pn])
                        A(lambda e: e.activation(out=h2T[:, 0:4, :], in_=ps[2][:].rearrange("p (c t) -> p c t", t=128), func=AF.Copy),
                          r=["ps2"], w=["h2T"])
                        A(lambda e: e.activation(out=h2T[:, 4:8, :], in_=ps[3][:].rearrange("p (c t) -> p c t", t=128), func=AF.Copy),
                          r=["ps3"], w=["h2T"])
                        for c in range(8):
                            T(lambda e, c=c: e.matmul(ps[4][:, 0:36], lhsT=h2T[:, c, :], rhs=wr32[:, c, :], start=(c == 0), stop=False),
                              r=["h2T", "wr32"], w=["ps4"])
                        T(lambda e: e.matmul(ps[4][:, 0:36], lhsT=ones32[0:1, :], rhs=brow[0:1, :], start=False, stop=True),
                          r=["cs", "brow"], w=["ps4"])
                        V(lambda e: e.tensor_copy(Ls[:], ps[4][:, 0:36]), r=["ps4"], w=["Ls"])
                        R_ = ["rsm"]
                        V(lambda e: e.tensor_reduce(out=rsm[:, 0:1], in_=Ls[:, 0:4], axis=AX.X, op=ALU.max), r=["Ls"], w=R_)
                        V(lambda e: e.tensor_scalar(out=rsm[:, 1:2], in0=rsm[:, 0:1], scalar1=-1.0, scalar2=None, op0=ALU.mult), r=R_, w=R_)
                        A(lambda e: e.activation(out=rsm[:, 48:52], in_=Ls[:, 0:4], func=AF.Exp, bias=rsm[:, 1:2], accum_out=rsm[:, 2:3]),
                          r=["Ls"] + R_, w=R_)
                        V(lambda e: e.reciprocal(out=rsm[:, 3:4], in_=rsm[:, 2:3]), r=R_, w=R_)
                        V(lambda e: e.tensor_scalar(out=rsm[:, 4:8], in0=Ls[:, 0:4], scalar1=rsm[:, 0:1], scalar2=None, op0=ALU.is_equal),
                          r=["Ls"] + R_, w=R_)
                        V(lambda e: e.tensor_scalar(out=rsm[:, 8:16], in0=Ls[:, 4:12], scalar1=rsm[:, 4:5], scalar2=None, op0=ALU.mult),
                          r=["Ls"] + R_, w=R_)
                        for g in range(1, 4):
                            V(lambda e, g=g: e.scalar_tensor_tensor(out=rsm[:, 8:16], in0=Ls[:, 4 + 8 * g:12 + 8 * g], scalar=rsm[:, 4 + g:5 + g],
                                                                    in1=rsm[:, 8:16], op0=ALU.mult, op1=ALU.add), r=["Ls"] + R_, w=R_)
                        V(lambda e: e.max(out=rsm[:, 16:24], in_=rsm[:, 8:16]), r=R_, w=R_)
                        V(lambda e: e.tensor_scalar(out=rsm[:, 24:32], in0=rsm[:, 8:16], scalar1=rsm[:, 16:17], scalar2=None, op0=ALU.is_equal), r=R_, w=R_)
                        V(lambda e: e.tensor_scalar(out=rsm[:, 32:40], in0=rsm[:, 8:16], scalar1=rsm[:, 17:18], scalar2=None, op0=ALU.is_equal), r=R_, w=R_)
                        V(lambda e: e.tensor_tensor(out=rsm[:, 40:41], in0=rsm[:, 16:17], in1=rsm[:, 17:18], op=ALU.subtract), r=R_, w=R_)
                        A(lambda e: e.activation(out=rsm[:, 41:42], in_=rsm[:, 40:41], func=AF.Sigmoid), r=R_, w=R_)
                        V(lambda e, sub=sub: e.tensor_tensor(out=W12[:, sub, 0:1], in0=rsm[:, 41:42], in1=rsm[:, 3:4], op=ALU.mult), r=R_, w=["W12"])
                        V(lambda e, sub=sub: e.tensor_tensor(out=W12[:, sub, 1:2], in0=rsm[:, 3:4], in1=W12[:, sub, 0:1], op=ALU.subtract),
                          r=R_ + ["W12"], w=["W12"])
                        for g in range(4):
                            V(lambda e, g=g, sub=sub: e.tensor_scalar(out=M1all[:, sub, g * 8:(g + 1) * 8], in0=rsm[:, 24:32],
                                                                      scalar1=rsm[:, 4 + g:5 + g], scalar2=None, op0=ALU.mult), r=R_, w=["M1all"])
                            V(lambda e, g=g, sub=sub: e.tensor_scalar(out=M2all[:, sub, g * 8:(g + 1) * 8], in0=rsm[:, 32:40],
                                                                      scalar1=rsm[:, 4 + g:5 + g], scalar2=None, op0=ALU.mult), r=R_, w=["M2all"])
                P.barrier_all()
                sm.close(); open_stacks.remove(sm)

                stop("p2")
                se = newstack()
                pre = sb("pre", [128, NE], st=se)
                cnt_i = sb("cnt_i", [128, NE], I32, st=se)
                padf = sb("padf", [128, NE], st=se)
                incl = sb("incl", [128, NE], st=se)
                offs = sb("offs", [128, NE], st=se)
                cmp3 = sb("cmp3", [128, NB, NE], st=se)
                bef = sb("bef", [128, NB], st=se)
                tmpr = sb("tmpr", [128, NE], st=se)
                tmpr2 = sb("tmpr2", [128, NE], st=se)
                hrow = [sb("hrow%d" % i, [128, D], BF16, st=se) for i in range(2)]
                NW = 3
                wgb = [sb("wgb%d" % i, [128, 4096], BF16, st=se) for i in range(NW)]
                wub = [sb("wub%d" % i, [128, 4096], BF16, st=se) for i in range(NW)]
                wdb = [sb("wdb%d" % i, [128, 4096], BF16, st=se) for i in range(NW)]
                xbT = [sb("xbT%d" % i, [128, 8, 128], BF16, st=se) for i in range(2)]
                sgate = [sb("sgate%d" % i, [128, 512], st=se) for i in range(2)]
                actb = [sb("actb%d" % i, [128, 512], BF16, st=se) for i in range(2)]
                actT = [sb("actT%d" % i, [128, 4, 128], BF16, st=se) for i in range(2)]
                ybuf = [sb("ybuf%d" % i, [128, D], st=se) for i in range(2)]
                y1 = [sb("y1_%d" % i, [128, D], st=se) for i in range(2)]
                y2 = [sb("y2_%d" % i, [128, D], st=se) for i in range(2)]
                xm = [sb("xm%d" % i, [128, D], st=se) for i in range(2)]
                junk2 = sb("junk2", [128, D], st=se)
                st2 = sb("st2", [128, 4], st=se)
                g2_bc = sb("g2_bc", [128, 2, D], st=se)
                Rall = sb("Rall", [128, NSUB, NE], st=se)
                Mall = sb("Mall", [128, NSUB, NE], BF16, st=se)
                dest_f = sb("dest_f", [128, NSUB, 2], st=se)
                dest_i = sb("dest_i", [128, NSUB, 2], I32, st=se)
                widx = sb("widx", [128, NB, 2], I32, st=se)
                for s in range(2):
                    LD(lambda e, s=s: e.dma_start(out=g2_bc[:, s, :], in_=MODROW[l, s:s + 1, 5 * D:6 * D].to_broadcast([128, D])), w=["g2_bc"])

                zrow = sb("zrow", [128, D], BF16, st=se)
                V(lambda e: e.memset(zrow[:], 0.0), w=["zrow"])
                XBz = XB.rearrange("(n p) f -> p n f", p=128)
                ZS = 10 if NSB % 10 == 0 else 12
                assert NSB % ZS == 0
                for n0 in range(0, NSB, ZS):
                    LD(lambda e, n0=n0: e.dma_start(out=XBz[:, n0:n0 + ZS, :], in_=zrow[:].unsqueeze(1).to_broadcast([128, ZS, D])),
                       r=["zrow"], w=["XBz"])
                V(lambda e: e.tensor_tensor(out=Mall[:], in0=M1all[:], in1=M2all[:], op=ALU.add), r=["M1all", "M2all"], w=["Mall"])
                V(lambda e: e.memset(pre[:], 0.0), w=["pre"])
                for i in range(NSUB):
                    T(lambda e, i=i: e.matmul(ps[0][:, 0:NE], lhsT=trib[:], rhs=Mall[:, i, :], start=True, stop=True), r=["Mall", "trib"], w=["ps0"])
                    T(lambda e, i=i: e.matmul(ps[1][:, 0:NE], lhsT=onesb[:], rhs=Mall[:, i, :], start=True, stop=True), r=["Mall", "onesb"], w=["ps1"])
                    V(lambda e, i=i: e.tensor_tensor(out=Rall[:, i, :], in0=ps[0][:, 0:NE], in1=pre[:], op=ALU.add), r=["ps0", "pre"], w=["Rall"])
                    V(lambda e: e.tensor_tensor(out=pre[:], in0=ps[1][:, 0:NE], in1=pre[:], op=ALU.add), r=["ps1", "pre"], w=["pre"])
                cmpT = cmp3[:].rearrange("p n e -> p (n e)").rearrange("p (e n) -> p e n", n=NB)
                V(lambda e: e.tensor_tensor(out=cmpT, in0=pre[:].unsqueeze(2).to_broadcast([128, NE, NB]),
                                            in1=nblk.unsqueeze(1).to_broadcast([128, NE, NB]), op=ALU.is_gt), r=["pre", "cs"], w=["cmp3"])
                V(lambda e: e.tensor_reduce(out=padf[:], in_=cmpT, axis=AX.X, op=ALU.add), r=["cmp3"], w=["padf"])
                V(lambda e: e.tensor_scalar(out=padf[:], in0=padf[:], scalar1=float(BLK), scalar2=None, op0=ALU.mult), r=["padf"], w=["padf"])
                V(lambda e: e.tensor_tensor_scan(out=incl[:], data0=ones32[:, 0:NE], data1=padf[:], initial=0.0, op0=ALU.mult, op1=ALU.add),
                  r=["padf", "cs"], w=["incl"])
                V(lambda e: e.tensor_tensor(out=offs[:], in0=incl[:], in1=padf[:], op=ALU.subtract), r=["incl", "padf"], w=["offs"])
                V(lambda e: e.tensor_tensor(out=cmp3[:], in0=nblk.unsqueeze(2).to_broadcast([128, NB, NE]),
                                            in1=incl[:].unsqueeze(1).to_broadcast([128, NB, NE]), op=ALU.is_ge), r=["incl", "cs"], w=["cmp3"])
                V(lambda e: e.tensor_reduce(out=bef[:], in_=cmp3[:], axis=AX.X, op=ALU.add), r=["cmp3"], w=["bef"])
                V(lambda e: e.tensor_scalar(out=bef[:], in0=bef[:], scalar1=31.0, scalar2=128.0, op0=ALU.min, op1=ALU.mult), r=["bef"], w=["bef"])
                V(lambda e: e.tensor_scalar(out=bef[:], in0=bef[:], scalar1=pcol, scalar2=None, op0=ALU.add), r=["bef", "cs"], w=["bef"])
                V(lambda e: e.tensor_copy(widx[:, :, 0], bef[:]), r=["bef"], w=["widx"])
                V(lambda e: e.tensor_scalar(out=bef[:], in0=bef[:], scalar1=4096.0, scalar2=None, op0=ALU.add), r=["bef"], w=["bef"])
                V(lambda e: e.tensor_copy(widx[:, :, 1], bef[:]), r=["bef"], w=["widx"])
                for i in range(NSUB):
                    V(lambda e, i=i: e.tensor_tensor(out=tmpr[:], in0=Rall[:, i, :], in1=offs[:], op=ALU.add), r=["Rall", "offs"], w=["tmpr"])
                    V(lambda e, i=i: e.scalar_tensor_tensor(out=tmpr2[:], in0=tmpr[:], scalar=1.0, in1=M1all[:, i, :], op0=ALU.mult, op1=ALU.mult,
                                                            accum_out=dest_f[:, i, 0:1]), r=["tmpr", "M1all"], w=["tmpr2", "dest_f"])
                    V(lambda e, i=i: e.scalar_tensor_tensor(out=tmpr2[:], in0=tmpr[:], scalar=1.0, in1=M2all[:, i, :], op0=ALU.mult, op1=ALU.mult,
                                                            accum_out=dest_f[:, i, 1:2]), r=["tmpr", "M2all"], w=["tmpr2", "dest_f"])
                V(lambda e: e.tensor_copy(dest_i[:], dest_f[:]), r=["dest_f"], w=["dest_i"])
                for i in range(NSUB):
                    hb = hrow[i % 2]
                    hn = "hrow%d" % (i % 2)
                    LD(lambda e, i=i, hb=hb: e.dma_start(out=hb[:], in_=H2v[:, i, :]), r=["H2_%d" % i], w=[hn])
                    for k in range(2):
                        GD(lambda e, i=i, k=k, hb=hb: e.indirect_dma_start(
                            out=XB, out_offset=bass.IndirectOffsetOnAxis(ap=dest_i[:, i, k:k + 1], axis=0), in_=hb[:], in_offset=None),
                           r=[hn, "dest_i", "XBz"], w=["XBs%d_%d" % (i, k)])
                P.barrier_all()

                stop("p3")
                XBv = XB.rearrange("(n p) f -> p n f", p=128)
                YBv = YB.rearrange("(n p) f -> p n f", p=128)

                def wload(n):
                    k = n % NW
                    for hf in range(2):
                        GD(lambda e, n=n, hf=hf, k=k: e.indirect_dma_start(
                            out=wgb[k][:, hf * 2048:(hf + 1) * 2048], out_offset=None, in_=wg2[l],
                            in_offset=bass.IndirectOffsetOnAxis(ap=widx[:, n, hf:hf + 1], axis=0)), r=["widx"], w=["wgb%d" % k])
                        GD(lambda e, n=n, hf=hf, k=k: e.indirect_dma_start(
                            out=wub[k][:, hf * 2048:(hf + 1) * 2048], out_offset=None, in_=wu2[l],
                            in_offset=bass.IndirectOffsetOnAxis(ap=widx[:, n, hf:hf + 1], axis=0)), r=["widx"], w=["wub%d" % k])
                        GD(lambda e, n=n, hf=hf, k=k: e.indirect_dma_start(
                            out=wdb[k][:, hf * 2048:(hf + 1) * 2048], out_offset=None, in_=wd2[l],
                            in_offset=bass.IndirectOffsetOnAxis(ap=widx[:, n, hf:hf + 1], axis=0)), r=["widx"], w=["wdb%d" % k])

                def stA1(sbi):
                    p = sbi % 2
                    hb, hn = hrow[p], "hrow%d" % p
                    LD(lambda e: e.dma_start(out=hb[:], in_=XBv[:, sbi, :]), r=[], w=[hn])
                    for c in range(8):
                        T(lambda e, c=c: e.matmul(ps[4 + c // 4][:, (c % 4) * 128:(c % 4 + 1) * 128], lhsT=hb[:, c * 128:(c + 1) * 128],
                                                  rhs=identb[:], start=True, stop=True), r=[hn, "identb"], w=["ps%d" % (4 + c // 4)])
                    A(lambda e: e.activation(out=xbT[p][:, 0:4, :].rearrange("p c t -> p (c t)"), in_=ps[4][:, :], func=AF.Copy),
                      r=["ps4"], w=["xbT%d" % p])
                    V(lambda e: e.tensor_copy(xbT[p][:, 4:8, :].rearrange("p c t -> p (c t)"), ps[5][:, :]), r=["ps5"], w=["xbT%d" % p])

                def stA2(sbi):
                    p = sbi % 2
                    kw = (sbi // (BLK // 128)) % NW
                    wg3 = wgb[kw][:].rearrange("p (c f) -> p c f", f=512)
                    wu3 = wub[kw][:].rearrange("p (c f) -> p c f", f=512)
                    for c in range(8):
                        T(lambda e, c=c: e.matmul(ps[2 * p][:, :], lhsT=xbT[p][:, c, :], rhs=wg3[:, c, :], start=(c == 0), stop=(c == 7)),
                          r=["xbT%d" % p, "wgb%d" % kw], w=["ps%d" % (2 * p)])
                    for c in range(8):
                        T(lambda e, c=c: e.matmul(ps[2 * p + 1][:, :], lhsT=xbT[p][:, c, :], rhs=wu3[:, c, :], start=(c == 0), stop=(c == 7)),
                          r=["xbT%d" % p, "wub%d" % kw], w=["ps%d" % (2 * p + 1)])

                def stA3(sbi):
                    p = sbi % 2
                    A(lambda e: e.activation(out=sgate[p][:], in_=ps[2 * p][:, :], func=AF.Silu), r=["ps%d" % (2 * p)], w=["sgate%d" % p])
                    V(lambda e: e.tensor_tensor(out=actb[p][:], in0=ps[2 * p + 1][:, :], in1=sgate[p][:], op=ALU.mult),
                      r=["ps%d" % (2 * p + 1), "sgate%d" % p], w=["actb%d" % p])

                def stB1(sbi):
                    p = sbi % 2
                    for c in range(4):
                        T(lambda e, c=c: e.matmul(ps[6][:, c * 128:(c + 1) * 128], lhsT=actb[p][:, c * 128:(c + 1) * 128], rhs=identb[:],
                                                  start=True, stop=True), r=["actb%d" % p, "identb"], w=["ps6"])
                    V(lambda e: e.tensor_copy(actT[p][:].rearrange("p c t -> p (c t)"), ps[6][:, :]), r=["ps6"], w=["actT%d" % p])

                def stB2(sbi):
                    p = sbi % 2
                    kw = (sbi // (BLK // 128)) % NW
                    wd3 = wdb[kw][:].rearrange("p (c f) -> p c f", f=1024)
                    for hf in range(2):
                        for c in range(4):
                            T(lambda e, c=c, hf=hf: e.matmul(ps[6 + hf][:, :], lhsT=actT[p][:, c, :], rhs=wd3[:, c, hf * 512:(hf + 1) * 512],
                                                             start=(c == 0), stop=(c == 3)), r=["actT%d" % p, "wdb%d" % kw], w=["ps%d" % (6 + hf)])
                    yb, yn = ybuf[p], "ybuf%d" % p
                    A(lambda e: e.activation(out=yb[:, 0:512], in_=ps[6][:, :], func=AF.Copy), r=["ps6"], w=[yn])
                    V(lambda e: e.tensor_copy(yb[:, 512:1024], ps[7][:, :]), r=["ps7"], w=[yn])
                    LD(lambda e: e.dma_start(out=YBv[:, sbi, :], in_=yb[:]), r=[yn], w=["YB%d" % sbi])

                SPB = BLK // 128
                wload(0)
                wload(1)
                stA1(0)
                stA2(0)
                stA3(0)
                for sbi in range(NSB):
                    n = sbi // SPB
                    if sbi % SPB == 0 and n + 2 < NB:
                        wload(n + 2)
                    if sbi + 1 < NSB:
                        stA1(sbi + 1)
                    stB1(sbi)
                    if sbi + 1 < NSB:
                        stA2(sbi + 1)
                        stA3(sbi + 1)
                    stB2(sbi)
                P.barrier_all()

                stop("p4")
                XOv = XS1.rearrange("(n p) f -> p n f", p=128)
                OUTv = out.rearrange("(n p) f -> p n f", p=128)
                for i in range(NSUB):
                    if last and i < 2:
                        continue
                    k = i % 2
                    s = 1 if i < 2 else 0
                    GD(lambda e, i=i, k=k: e.indirect_dma_start(out=y1[k][:], out_offset=None, in_=YB,
                                                                in_offset=bass.IndirectOffsetOnAxis(ap=dest_i[:, i, 0:1], axis=0)),
                       r=["dest_i"], w=["y1_%d" % k])
                    GD(lambda e, i=i, k=k: e.indirect_dma_start(out=y2[k][:], out_offset=None, in_=YB,
                                                                in_offset=bass.IndirectOffsetOnAxis(ap=dest_i[:, i, 1:2], axis=0)),
                       r=["dest_i"], w=["y2_%d" % k])
                    LD(lambda e, i=i, k=k: e.dma_start(out=xm[k][:], in_=XMv[:, i, :]), r=["XM%d" % (i // 2)], w=["xm%d" % k])
                    V(lambda e, i=i, k=k: e.tensor_scalar(out=y1[k][:], in0=y1[k][:], scalar1=W12[:, i, 0:1], scalar2=None, op0=ALU.mult),
                      r=["y1_%d" % k, "W12"], w=["y1_%d" % k])
                    V(lambda e, i=i, k=k: e.scalar_tensor_tensor(out=y1[k][:], in0=y2[k][:], scalar=W12[:, i, 1:2], in1=y1[k][:],
                                                                 op0=ALU.mult, op1=ALU.add), r=["y1_%d" % k, "y2_%d" % k, "W12"], w=["y1_%d" % k])
                    G(lambda e, k=k, s=s: e.tensor_tensor(out=y1[k][:], in0=y1[k][:], in1=g2_bc[:, s, :], op=ALU.mult),
                      r=["y1_%d" % k, "g2_bc"], w=["y1_%d" % k])
                    V(lambda e, k=k: e.tensor_tensor(out=xm[k][:], in0=xm[k][:], in1=y1[k][:], op=ALU.add), r=["xm%d" % k, "y1_%d" % k], w=["xm%d" % k])
                    if not last:
                        LD(lambda e, i=i, k=k: e.dma_start(out=XOv[:, i, :], in_=xm[k][:]), r=["xm%d" % k], w=["XIN%d" % (i // 2)])
                    else:
                        if dbg:
                            LD(lambda e, i=i, k=k: e.dma_start(out=XOv[:, i, :], in_=xm[k][:]), r=["xm%d" % k], w=["XIN%d" % (i // 2)])
                        A(lambda e, k=k: e.activation(out=junk2[:], in_=xm[k][:], func=AF.Square, accum_out=st2[:, 0:1]), r=["xm%d" % k], w=["junk2", "st2"])
                        V(lambda e: e.tensor_scalar(out=st2[:, 1:2], in0=st2[:, 0:1], scalar1=1.0 / D, scalar2=EPS, op0=ALU.mult, op1=ALU.add),
                          r=["st2"], w=["st2"])
                        A(lambda e: e.activation(out=st2[:, 1:2], in_=st2[:, 1:2], func=AF.Sqrt), r=["st2"], w=["st2"])
                        V(lambda e: e.reciprocal(out=st2[:, 2:3], in_=st2[:, 1:2]), r=["st2"], w=["st2"])
                        V(lambda e, k=k: e.scalar_tensor_tensor(out=xm[k][:], in0=xm[k][:], scalar=st2[:, 2:3], in1=fn_bc[:], op0=ALU.mult, op1=ALU.mult),
                          r=["xm%d" % k, "st2", "fn_bc"], w=["xm%d" % k])
                        LD(lambda e, i=i, k=k: e.dma_start(out=OUTv[:, i - 2, :], in_=xm[k][:]), r=["xm%d" % k], w=["OUT"], is_out=True)
                if dbg and last:
                    LD(lambda e: e.dma_start(out=YBv[:, NSB - 1, :], in_=fn_bc[:]), r=["fn_bc"], w=["YBdbg"])
                    LD(lambda e: e.dma_start(out=YBv[:, NSB - 2, 0:4], in_=st2[:]), r=["st2"], w=["YBdbg2"])
                P.barrier_all()
                se.close(); open_stacks.remove(se)
        except _Stop:
            for stx in reversed(open_stacks):
                stx.close()
        P.finish()
        block = E(nc.Block())
        P.replay(block)
    return nc


def _consts():
    c = np.zeros((128, CW), np.float32)
    c[:, 0:128] = np.eye(128, dtype=np.float32)
    tp = np.arange(128)[:, None]
    tt = np.arange(128)[None, :]
    c[:, 128:256] = (tp < tt).astype(np.float32)
    m = np.ones(256, np.float32)
    m[::64] = 0.0
    c[:, 256:512] = m[None, :]
    c[:, 512:640] = 1.0
    c[:, 640:640 + NB] = (np.arange(NB) * float(BLK))[None, :]
    c[:, 760] = np.arange(128)
    same = (tp // 64) == (tt // 64)
    m8 = np.zeros((128, 256), np.uint8)
    m8[:, 0:128] = (same & (tt >= tp)).astype(np.uint8)
    m8[:, 128:256] = (same & (tt <= tp)).astype(np.uint8)
    return c, m8


def _relay_gate(w):
    a = w.reshape(NE, 8, 128, 512).transpose(0, 2, 1, 3).reshape(NE, 128, 2, 2048)
    return np.ascontiguousarray(a.transpose(2, 0, 1, 3).reshape(2 * NE * 128, 2048))


def _relay_down(w):
    a = w.reshape(NE, 4, 128, 1024).transpose(0, 2, 1, 3).reshape(NE, 128, 2, 2048)
    return np.ascontiguousarray(a.transpose(2, 0, 1, 3).reshape(2 * NE * 128, 2048))


def make_in_maps(inputs, cores, small=False):
    f = lambda a: np.ascontiguousarray(np.asarray(a, dtype=np.float32))
    x, c, ctx, c_ctx = f(inputs['x']), f(inputs['c']), f(inputs['ctx']), f(inputs['c_ctx'])
    norm1, norm2 = f(inputs['norm1']), f(inputs['norm2'])
    cst, cst8 = _consts()
    shared = dict(
        w_mod=f(inputs['w_mod']), b_mod=f(inputs['b_mod']), w_in=f(inputs['w_in']), n2row=norm2,
        sgn=f(inputs['sgu_norm']), sgw=f(inputs['sgu_w']), sgb=f(inputs['sgu_b']).reshape(2, 512),
        w_out=f(inputs['w_out']),
        wrt=np.ascontiguousarray(np.concatenate([f(inputs['w_group']), f(inputs['w_router'])], axis=2)),
        brt=np.ascontiguousarray(np.concatenate([f(inputs['b_group']), f(inputs['b_router'])], axis=1)),
        wg2_0=_relay_gate(f(inputs['w_gate'][0])), wg2_1=_relay_gate(f(inputs['w_gate'][1])),
        wu2_0=_relay_gate(f(inputs['w_up'][0])), wu2_1=_relay_gate(f(inputs['w_up'][1])),
        wd2_0=_relay_down(f(inputs['w_down'][0])), wd2_1=_relay_down(f(inputs['w_down'][1])),
        fnorm=f(inputs['final_norm']).reshape(1, D), cst=cst, cst8=cst8,
    )
    if small:
        for k in list(shared):
            if k[:3] in ("wg2", "wu2", "wd2"):
                shared[k] = np.zeros((8, 8), np.float32)
    maps = []
    for b in cores:
        rows = np.zeros((72, 128), np.float32)
        for l in range(2):
            rows[l * 16:l * 16 + 8] = norm1[l].reshape(8, 128)
            rows[l * 16 + 8:l * 16 + 16] = norm2[l].reshape(8, 128)
        rows[32:48] = f(inputs['lb_logits']).reshape(16, 128)
        rows[48:56] = f(inputs['hgrn_norm']).reshape(8, 128)
        rows[56:64] = c[b].reshape(8, 128)
        rows[64:72] = c_ctx.reshape(8, 128)
        m = dict(shared)
        m['xs'] = np.ascontiguousarray(np.concatenate([ctx[b], x[b]], axis=0))
        m['rows_in'] = rows
        maps.append(m)
    return maps


def kernel(**inputs):
    n = 8
    nc = build_nc()
    in_maps = make_in_maps(inputs, list(range(n)))
    res = run_bass_kernel_spmd(nc, in_maps, core_ids=list(range(n)))
    return np.stack([np.asarray(r["out"], dtype=np.float32) for r in res.results], axis=0)
```

```python
import numpy as np
from contextlib import ExitStack
import concourse.bass as bass
import concourse.mybir as mybir
from concourse.bass_utils import run_bass_kernel_spmd

F32 = mybir.dt.float32
BF16 = mybir.dt.bfloat16
I32 = mybir.dt.int32
U8 = mybir.dt.uint8
AF = mybir.ActivationFunctionType
ALU = mybir.AluOpType
AX = mybir.AxisListType

D = 1024
TL = 4096
TC = 256
TOK = TL + TC
NT = 256
NTILES = TOK // NT
NSUB = TOK // 128
NCH = TOK // 64
INW = 3584
NE = 32
BLK = 128
NB = (NSUB * 2 * 128 + BLK - 1) // BLK + NE
NSB = NB * (BLK // 128)
EPS = 1e-6
CW = 1024


class _Rec:
    def __init__(self):
        self.calls = []

    def __getattr__(self, name):
        def f(*a, **k):
            self.calls.append((name, a, k))
            return self
        return f


def _bind(fn):
    rec = _Rec()
    fn(rec)
    assert len(rec.calls) == 1, rec.calls
    name, a, k = rec.calls[0]
    return lambda e: getattr(e, name)(*a, **k)


class Prog:
    ENG = ['pe', 'dve', 'act', 'pool', 'sp']
    NDS = 16

    def __init__(self, nc, stack):
        self.nc = nc
        self.q = {e: [] for e in self.ENG}
        self.cnt = {e: 0 for e in self.ENG}
        self.sems = {}
        for e in self.ENG:
            self.sems['s_' + e] = stack.enter_context(nc.semaphore('s_' + e))
        for e in ('sp', 'pool', 'act'):
            for i in range(self.NDS):
                self.sems['d_%s%d' % (e, i)] = stack.enter_context(nc.semaphore('d_%s%d' % (e, i)))
        self.dcnt = {e: 0 for e in ('sp', 'pool', 'act')}
        self.waited = {e: {} for e in self.ENG}
        self.lastw = {}
        self.readers = {}
        self.out_toks = []

    def _deps(self, reads, writes):
        deps = []
        for b in reads:
            if b in self.lastw:
                deps.append(self.lastw[b])
        for b in writes:
            if b in self.lastw:
                deps.append(self.lastw[b])
            deps.extend(self.readers.get(b, []))
        return deps

    def _wait(self, eng, tok):
        key, val, src = tok
        if src == 'pe' and eng == 'pe' and key == 's_pe':
            return
        if self.waited[eng].get(key, 0) >= val:
            return
        self.waited[eng][key] = val
        sem = self.sems[key]
        self.q[eng].append(lambda e, sem=sem, val=val: e.wait_ge(sem, val))

    def _record(self, tok, reads, writes):
        for b in reads:
            self.readers.setdefault(b, []).append(tok)
        for b in writes:
            self.lastw[b] = tok
            self.readers[b] = []

    def op(self, eng, fn, reads=(), writes=()):
        fn = _bind(fn)
        for tok in self._deps(reads, writes):
            self._wait(eng, tok)
        self.cnt[eng] += 1
        seq = self.cnt[eng]
        sem = self.sems['s_' + eng]
        self.q[eng].append(lambda e, fn=fn, sem=sem: fn(e).then_inc(sem, 1))
        tok = ('s_' + eng, seq, eng)
        self._record(tok, reads, writes)
        return tok

    def dma(self, eng, fn, reads=(), writes=(), is_out=False):
        fn = _bind(fn)
        for tok in self._deps(reads, writes):
            self._wait(eng, tok)
        k = self.dcnt[eng]
        self.dcnt[eng] += 1
        slot = k % self.NDS
        val = 16 * (k // self.NDS + 1)
        key = 'd_%s%d' % (eng, slot)
        if k >= self.NDS:
            self._wait(eng, (key, val - 16, eng))
        sem = self.sems[key]
        self.q[eng].append(lambda e, fn=fn, sem=sem: fn(e).then_inc(sem, 16))
        tok = (key, val, eng)
        self._record(tok, reads, writes)
        if is_out:
            self.out_toks.append(tok)
        return tok

    def barrier_all(self):
        toks = []
        for e in self.ENG:
            if self.cnt[e] > 0:
                toks.append(('s_' + e, self.cnt[e], e))
        for e in ('sp', 'pool', 'act'):
            k = self.dcnt[e]
            for slot in range(self.NDS):
                n = (k - slot + self.NDS - 1) // self.NDS if k > slot else 0
                if n > 0:
                    toks.append(('d_%s%d' % (e, slot), 16 * n, e))
        for e in self.ENG:
            for t in toks:
                if t[2] == 'pe' and e == 'pe' and t[0] == 's_pe':
                    pass
                key, val, src = t
                if self.waited[e].get(key, 0) >= val:
                    continue
                self.waited[e][key] = val
                sem = self.sems[key]
                self.q[e].append(lambda en, sem=sem, val=val: en.wait_ge(sem, val))
        self.lastw = {}
        self.readers = {}

    def finish(self):
        for tok in self.out_toks:
            self._wait('sp', tok)

    def replay(self, block):
        q = self.q

        @block.tensor
        def _(e):
            for f in q['pe']:
                f(e)

        @block.vector
        def _(e):
            for f in q['dve']:
                f(e)

        @block.scalar
        def _(e):
            for f in q['act']:
                f(e)

        @block.gpsimd
        def _(e):
            for f in q['pool']:
                f(e)

        @block.sync
        def _(e):
            for f in q['sp']:
                f(e)


class _Stop(Exception):
    pass


def build_nc(n_layers=2, dbg=None):
    nc = bass.Bass("TRN2", target_bir_lowering=False)

    def din(name, shape, dt=F32):
        return nc.dram_tensor(name, shape, dt, kind="ExternalInput").ap()

    def dint(name, shape, dt=F32):
        return nc.dram_tensor(name, shape, dt, kind=("ExternalOutput" if dbg else "Internal")).ap()

    xs = din("xs", [TOK, D])
    rows_in = din("rows_in", [72, 128])
    w_mod = din("w_mod", [2, D, 6 * D])
    b_mod = din("b_mod", [2, 6 * D])
    w_in = din("w_in", [2, D, INW])
    n2row = din("n2row", [2, D])
    sgn = din("sgn", [2, 512])
    sgw = din("sgw", [2, 4, 128, 128])
    sgb = din("sgb", [2, 512])
    w_out = din("w_out", [2, D, D])
    wrt = din("wrt", [2, D, 36])
    brt = din("brt", [2, 36])
    esh = [8, 8] if dbg in ("s0", "l0", "p1a", "p1", "p2", "p3", "ta", "tb", "tc", "td", "te", "tf", "tg") else [8192, 2048]
    wg2 = [din("wg2_%d" % i, esh) for i in range(2)]
    wu2 = [din("wu2_%d" % i, esh) for i in range(2)]
    wd2 = [din("wd2_%d" % i, esh) for i in range(2)]
    fnorm = din("fnorm", [1, D])
    cst = din("cst", [128, CW])
    cst8 = din("cst8", [128, 256], U8)
    out = nc.dram_tensor("out", [TL, D], F32, kind="ExternalOutput").ap()

    MODROW = dint("MODROW", [2, 2, 6 * D])
    OBF = dint("OBF", [512, TOK])
    QF = dint("QF", [512, TOK], BF16)
    GS = dint("GS", [512, TOK], BF16)
    SGD = dint("SGD", [512, TOK], BF16)
    KFT = dint("KFT", [TOK, 512], BF16)
    VT = dint("VT", [TOK, 512], BF16)
    XM = dint("XM", [TOK, D])
    H2 = dint("H2", [TOK, D], BF16)
    XB = dint("XB", [NSB * 128, D], BF16)
    YB = dint("YB", [NSB * 128, D])
    XS1 = dint("XS1", [TOK, D])

    with ExitStack() as top:
        E = top.enter_context
        P = Prog(nc, top)

        uid = [0]

        def sb(name, shape, dt=F32, st=top):
            uid[0] += 1
            return st.enter_context(nc.sbuf_tensor("%s_u%d" % (name, uid[0]), shape, dt))

        def V(fn, r=(), w=()):
            return P.op('dve', fn, r, w)

        def A(fn, r=(), w=()):
            return P.op('act', fn, r, w)

        def G(fn, r=(), w=()):
            return P.op('pool', fn, r, w)

        def T(fn, r=(), w=()):
            return P.op('pe', fn, r, w)

        def LD(fn, r=(), w=(), is_out=False):
            return P.dma('sp', fn, r, w, is_out)

        def GD(fn, r=(), w=()):
            return P.dma('pool', fn, r, w)

        cs = sb("cs", [128, CW])
        m8 = sb("m8", [128, 256], U8)
        identb = sb("identb", [128, 128], BF16)
        trib = sb("trib", [128, 128], BF16)
        onesb = sb("onesb", [128, 128], BF16)
        colsA = sb("colsA", [128, 72])
        colsM = sb("colsM", [128, 192])
        scv = sb("scv", [128, 16])
        lbc = sb("lbc", [128, 2, 8])
        oml = sb("oml", [128, 2, 8])
        lbm1 = sb("lbm1", [128, 2, 8])
        a1c = sb("a1c", [128, 2, 2, 8])
        ERF = sb("ERF", [128, 4, NCH])
        ETRF = sb("ETRF", [128, 4, NCH])
        DECF = sb("DECF", [128, 4, NCH])
        M1all = sb("M1all", [128, NSUB, NE], BF16)
        M2all = sb("M2all", [128, NSUB, NE], BF16)
        W12 = sb("W12", [128, NSUB, 2])
        fn_bc = sb("fn_bc", [128, D])
        Sst = sb("Sst", [128, 4, 128])
        Sbf = sb("Sbf", [128, 4, 4, 128], BF16)
        scb = [sb("scb%d" % i, [128, 128], BF16) for i in range(2)]
        scf = [sb("scf%d" % i, [128, 128], BF16) for i in range(2)]

        ps = [E(nc.psum_tensor("ps%d" % i, [128, 512], F32)) for i in range(8)]

        ident = cs[:, 0:128]
        tri32 = cs[:, 128:256]
        mskrow = cs[:, 256:512]
        ones32 = cs[:, 512:640]
        nblk = cs[:, 640:640 + NB]
        pcol = cs[:, 760:761]
        maskF = m8[:, 0:128]
        maskB = m8[:, 128:256]

        open_stacks = []

        def newstack():
            stx = ExitStack()
            open_stacks.append(stx)
            return stx

        def stop(tag):
            if dbg == tag:
                P.barrier_all()
                raise _Stop()

        try:
            LD(lambda e: e.dma_start(out=cs[:], in_=cst), w=["cs"])
            LD(lambda e: e.dma_start(out=m8[:], in_=cst8), w=["m8"])
            V(lambda e: e.tensor_copy(identb[:], ident), r=["cs"], w=["identb"])
            V(lambda e: e.tensor_copy(trib[:], tri32), r=["cs"], w=["trib"])
            V(lambda e: e.tensor_copy(onesb[:], ones32), r=["cs"], w=["onesb"])
            LD(lambda e: e.dma_start(out=fn_bc[:], in_=fnorm.to_broadcast([128, D])), w=["fn_bc"])
            for i in range(2):
                V(lambda e, i=i: e.memset(scb[i][:], 0.0), w=["scb%d" % i])
                V(lambda e, i=i: e.memset(scf[i][:], 0.0), w=["scf%d" % i])

            s0 = newstack()
            rowsA = sb("rowsA", [72, 128], st=s0)
            LD(lambda e: e.dma_start(out=rowsA[:], in_=rows_in), w=["rowsA"])
            T(lambda e: e.transpose(out=ps[0][:, 0:72], in_=rowsA[:], identity=cs[0:72, 0:72]), r=["rowsA", "cs"], w=["ps0"])
            V(lambda e: e.tensor_copy(colsA[:], ps[0][:, 0:72]), r=["ps0"], w=["colsA"])
            A(lambda e: e.activation(out=scv[:], in_=colsA[:, 56:72], func=AF.Silu), r=["colsA"], w=["scv"])
            V(lambda e: e.memset(lbc[:], 0.0), w=["lbc"])
            V(lambda e: e.tensor_tensor(out=lbc[:, 1, :], in0=colsA[:, 40:48], in1=colsA[:, 32:40], op=ALU.subtract), r=["colsA"], w=["lbc"])
            A(lambda e: e.activation(out=lbc[:, 1, :], in_=lbc[:, 1, :], func=AF.Sigmoid), r=["lbc"], w=["lbc"])
            V(lambda e: e.tensor_scalar(out=oml[:], in0=lbc[:], scalar1=-1.0, scalar2=1.0, op0=ALU.mult, op1=ALU.add), r=["lbc"], w=["oml"])
            V(lambda e: e.tensor_scalar(out=lbm1[:], in0=lbc[:], scalar1=-1.0, scalar2=None, op0=ALU.add), r=["lbc"], w=["lbm1"])

            wmb = [sb("wmb%d" % i, [128, 8, 512], st=s0) for i in range(2)]
            bmod_sb = sb("bmod_sb", [2, 6 * D], st=s0)
            modsb = sb("modsb", [2, 6 * D], st=s0)
            it = 0
            for l in range(2):
                LD(lambda e, l=l: e.dma_start(out=bmod_sb[:], in_=b_mod[l:l + 1, :].to_broadcast([2, 6 * D])), w=["bmod_sb"])
                for n in range(12):
                    wb = wmb[it % 2]
                    wn = "wmb%d" % (it % 2)
                    LD(lambda e, l=l, n=n, wb=wb: e.dma_start(
                        out=wb[:], in_=w_mod[l, :, n * 512:(n + 1) * 512].rearrange("(kc p) f -> p kc f", p=128)), w=[wn])
                    pb = ps[it % 2]
                    pn = "ps%d" % (it % 2)
                    for kc in range(8):
                        T(lambda e, kc=kc, wb=wb, pb=pb: e.matmul(pb[0:2, :], lhsT=scv[:, kc:16:8], rhs=wb[:, kc, :],
                                                                 start=(kc == 0), stop=(kc == 7)), r=[wn, "scv"], w=[pn])
                    V(lambda e, n=n, pb=pb: e.tensor_tensor(out=modsb[:, n * 512:(n + 1) * 512], in0=pb[0:2, :],
                                                            in1=bmod_sb[:, n * 512:(n + 1) * 512], op=ALU.add),
                      r=[pn, "bmod_sb"], w=["modsb"])
                    it += 1
                LD(lambda e, l=l: e.dma_start(out=MODROW[l], in_=modsb[:]), r=["modsb"], w=["MODROW"])
            rowsM = sb("rowsM", [96, 2, 128], st=s0)
            MR = MODROW.rearrange("l s (r q) -> l (s r) q", q=128)
            for l in range(2):
                LD(lambda e, l=l: e.dma_start(out=rowsM[:, l, :], in_=MR[l]), r=["MODROW"], w=["rowsM"])
            for l in range(2):
                T(lambda e, l=l: e.transpose(out=ps[2][:, l * 96:(l + 1) * 96], in_=rowsM[:, l, :], identity=cs[0:96, 0:96]),
                  r=["rowsM", "cs"], w=["ps2"])
            V(lambda e: e.tensor_copy(colsM[:], ps[2][:, 0:192]), r=["ps2"], w=["colsM"])

            def cm(l, s, k):
                o = l * 96 + s * 48 + k * 8
                return colsM[:, o:o + 8]

            for l in range(2):
                for s in range(2):
                    V(lambda e, l=l, s=s: e.scalar_tensor_tensor(out=a1c[:, l, s, :], in0=cm(l, s, 1), scalar=1.0,
                                                                 in1=colsA[:, l * 16:l * 16 + 8], op0=ALU.add, op1=ALU.mult),
                      r=["colsM", "colsA"], w=["a1c"])
            P.barrier_all()
            s0.close(); open_stacks.remove(s0)
            stop("s0")

            for l in range(n_layers):
                last = (l == 1)
                XIN = xs if l == 0 else XS1
                sm = newstack()
                winb = sb("winb", [128, 8, INW], BF16, st=sm)
                woutb = sb("woutb", [128, 8, D], BF16, st=sm)
                wr32 = sb("wr32", [128, 8, 36], st=sm)
                brow = sb("brow", [1, 36], st=sm)
                wsT = sb("wsT", [128, 4, 128], BF16, st=sm)
                bsrow = sb("bsrow", [1, 512], BF16, st=sm)
                sgn_bc = sb("sgn_bc", [128, 512], st=sm)
                g1_bc = sb("g1_bc", [128, 2, D], st=sm)
                a2_bc = sb("a2_bc", [128, 2, D], st=sm)
                b2_bc = sb("b2_bc", [128, 2, D], st=sm)
                for kc in range(8):
                    for hf in range(2):
                        GD(lambda e, kc=kc, hf=hf: e.dma_start(out=winb[:, kc, hf * 1792:(hf + 1) * 1792],
                                                               in_=w_in[l, kc * 128:(kc + 1) * 128, hf * 1792:(hf + 1) * 1792]),
                           w=["winb"])
                    GD(lambda e, kc=kc: e.dma_start(out=woutb[:, kc, :], in_=w_out[l, kc * 128:(kc + 1) * 128, :]), w=["woutb"])
                LD(lambda e: e.dma_start(out=wr32[:], in_=wrt[l].rearrange("(kc p) f -> p kc f", p=128)), w=["wr32"])
                LD(lambda e: e.dma_start(out=brow[:], in_=brt[l:l + 1, :]), w=["brow"])
                GD(lambda e: e.dma_start(out=bsrow[:], in_=sgb[l:l + 1, :]), w=["bsrow"])
                LD(lambda e: e.dma_start(out=sgn_bc[:], in_=sgn[l:l + 1, :].to_broadcast([128, 512])), w=["sgn_bc"])
                sl = newstack()
                wsn = sb("wsn", [128, 4, 128], st=sl)
                LD(lambda e: e.dma_start(out=wsn[:], in_=sgw[l].rearrange("h i j -> i h j")), w=["wsn"])
                for h in range(4):
                    T(lambda e, h=h: e.transpose(out=ps[0][:, h * 128:(h + 1) * 128], in_=wsn[:, h, :], identity=ident),
                      r=["wsn", "cs"], w=["ps0"])
                V(lambda e: e.tensor_copy(wsT[:].rearrange("p h i -> p (h i)"), ps[0][:]), r=["ps0"], w=["wsT"])
                tmpb = sb("tmpb", [128, D], st=sl)
                for s in range(2):
                    LD(lambda e, s=s: e.dma_start(out=g1_bc[:, s, :], in_=MODROW[l, s:s + 1, 2 * D:3 * D].to_broadcast([128, D])), w=["g1_bc"])
                    LD(lambda e, s=s: e.dma_start(out=b2_bc[:, s, :], in_=MODROW[l, s:s + 1, 3 * D:4 * D].to_broadcast([128, D])), w=["b2_bc"])
                    LD(lambda e, s=s: e.dma_start(out=a2_bc[:, s, :], in_=MODROW[l, s:s + 1, 4 * D:5 * D].to_broadcast([128, D])), w=["a2_bc"])
                LD(lambda e: e.dma_start(out=tmpb[:], in_=n2row[l:l + 1, :].to_broadcast([128, D])), w=["tmpb"])
                for s in range(2):
                    V(lambda e, s=s: e.scalar_tensor_tensor(out=a2_bc[:, s, :], in0=a2_bc[:, s, :], scalar=1.0, in1=tmpb[:],
                                                            op0=ALU.add, op1=ALU.mult), r=["a2_bc", "tmpb"], w=["a2_bc"])
                P.barrier_all()
                sl.close(); open_stacks.remove(sl)
                stop("l0")

                xt = [sb("xt%d" % i, [128, 2, D], st=sm) for i in range(2)]
                hT = sb("hT", [128, 8, NT], BF16, st=sm)
                q32 = sb("q32", [128, 4, NT], st=sm)
                sgt = [sb("sgt%d" % i, [128, NT], st=sm) for i in range(2)]
                lft = [sb("lft%d" % i, [128, NT], st=sm) for i in range(2)]
                kkt = [sb("kkt%d" % i, [128, NT], st=sm) for i in range(2)]
                Att = [sb("Att%d" % i, [128, NT], st=sm) for i in range(2)]
                e1t = [sb("e1t%d" % i, [128, NT], st=sm) for i in range(2)]
                qtf = sb("qtf", [128, 4, NT], BF16, st=sm)
                ktf = sb("ktf", [128, 4, NT], BF16, st=sm)
                qtb = sb("qtb", [128, 4, NT], BF16, st=sm)
                ktb = sb("ktb", [128, 4, NT], BF16, st=sm)
                gsb = sb("gsb", [128, 4, NT], BF16, st=sm)
                ug = sb("ug", [128, 4, NT], BF16, st=sm)
                mixT = sb("mixT", [128, 8, NT], BF16, st=sm)
                vtok = sb("vtok", [128, 2, 512], BF16, st=sm)
                vg = sb("vg", [128, 512], st=sm)
                vntok = sb("vntok", [128, 2, 512], BF16, st=sm)
                kbtok = sb("kbtok", [128, 2, 512], BF16, st=sm)
                kftok = sb("kftok", [128, 2, 512], BF16, st=sm)
                obf = sb("obf", [128, 4, NT], st=sm)
                ofull = obf
                sqt = sb("sqt", [128, NT], st=sm)
                rt = sb("rt", [128, NT], st=sm)
                mt = sb("mt", [128, NT], st=sm)
                stat = sb("stat", [128, 8], st=sm)
                erb = sb("erb", [128, 4, 4], st=sm)
                etrb = sb("etrb", [128, 4, 4], st=sm)
                decb = sb("decb", [128, 4, 4], st=sm)
                dtmp = sb("dtmp", [128, 4, 4], st=sm)
                utmp = sb("utmp", [128, 4, 128], st=sm)
                tmp2 = sb("tmp2", [128, 512], st=sm)
                h2t = sb("h2t", [128, D], st=sm)
                junk = h2t
                h2T = sb("h2T", [128, 8, 128], st=sm)
                Ls = sb("Ls", [128, 36], st=sm)
                rsm = sb("rsm", [128, 64], st=sm)

                OBFv = OBF.rearrange("(h p) t -> p h t", p=128)
                QFv = QF.rearrange("(h p) t -> p h t", p=128)
                GSv = GS.rearrange("(h p) t -> p h t", p=128)
                SGv = SGD.rearrange("(h p) t -> p h t", p=128)
                KFTv = KFT.rearrange("(n p) f -> p n f", p=128)
                VTv = VT.rearrange("(n p) f -> p n f", p=128)
                XINv = XIN.rearrange("(n p) f -> p n f", p=128)
                XMv = XM.rearrange("(n p) f -> p n f", p=128)
                H2v = H2.rearrange("(n p) f -> p n f", p=128)

                def rstd_from_ss(ssap, rap, n, inv):
                    V(lambda e: e.tensor_scalar(out=rap, in0=ssap, scalar1=inv, scalar2=EPS, op0=ALU.mult, op1=ALU.add),
                      r=["stat"], w=["stat"])
                    A(lambda e: e.activation(out=rap, in_=rap, func=AF.Sqrt), r=["stat"], w=["stat"])
                    V(lambda e: e.reciprocal(out=rap, in_=rap), r=["stat"], w=["stat"])

                def load_x(ti, bi):
                    LD(lambda e: e.dma_start(out=xt[bi][:], in_=XINv[:, 2 * ti:2 * ti + 2, :]), r=["XIN%d" % ti], w=["xt%d" % bi])

                def norm_to_hT(ti, bi, s):
                    x = xt[bi]
                    xn = "xt%d" % bi
                    for j in range(2):
                        A(lambda e, j=j: e.activation(out=junk[:], in_=x[:, j, :], func=AF.Square, accum_out=stat[:, j:j + 1]),
                          r=[xn], w=["h2t", "stat"])
                    rstd_from_ss(stat[:, 0:2], stat[:, 2:4], 2, 1.0 / D)
                    for j in range(2):
                        V(lambda e, j=j: e.tensor_scalar(out=x[:, j, :], in0=x[:, j, :], scalar1=stat[:, 2 + j:3 + j], scalar2=None,
                                                         op0=ALU.mult), r=[xn, "stat"], w=[xn])
                    for c in range(8):
                        pb = ps[2 + (c % 2)]
                        pn = "ps%d" % (2 + (c % 2))
                        for j in range(2):
                            T(lambda e, c=c, j=j, pb=pb: e.transpose(out=pb[:, j * 128:(j + 1) * 128], in_=x[:, j, c * 128:(c + 1) * 128],
                                                                     identity=ident), r=[xn, "cs"], w=[pn])
                        if c % 2 == 0:
                            V(lambda e, c=c, pb=pb: e.tensor_scalar(out=hT[:, c, :], in0=pb[:, 0:NT], scalar1=a1c[:, l, s, c:c + 1],
                                                                    scalar2=cm(l, s, 0)[:, c:c + 1], op0=ALU.mult, op1=ALU.add),
                              r=[pn, "a1c", "colsM"], w=["hT"])
                        else:
                            A(lambda e, c=c, pb=pb: e.activation(out=hT[:, c, :], in_=pb[:, 0:NT], func=AF.Identity,
                                                                 bias=cm(l, s, 0)[:, c:c + 1], scale=a1c[:, l, s, c:c + 1]),
                              r=[pn, "a1c", "colsM"], w=["hT"])

                pj = [0]

                def proj_fm(m):
                    k = pj[0] % 2
                    pj[0] += 1
                    pb = ps[k]
                    for c in range(8):
                        T(lambda e, c=c, pb=pb: e.matmul(pb[:, 0:NT], lhsT=winb[:, c, m * 128:(m + 1) * 128], rhs=hT[:, c, :],
                                                         start=(c == 0), stop=(c == 7)), r=["winb", "hT"], w=["ps%d" % k])
                    return pb, "ps%d" % k

                def proj_tm(j, col0):
                    k = pj[0] % 2
                    pj[0] += 1
                    pb = ps[k]
                    for c in range(8):
                        T(lambda e, c=c, pb=pb: e.matmul(pb[:, :], lhsT=hT[:, c, j * 128:(j + 1) * 128], rhs=winb[:, c, col0:col0 + 512],
                                                         start=(c == 0), stop=(c == 7)), r=["winb", "hT"], w=["ps%d" % k])
                    return pb, "ps%d" % k

                gi = [0]

                def gates(h, dr, ti):
                    k = gi[0] % 2
                    gi[0] += 1
                    pb, pn = proj_fm(4 + 4 * dr + h)
                    sg_, lf_, kk_, I_, A_, e1_, e2_ = sgt[k], lft[k], kkt[k], sgt[k], Att[k], e1t[k], Att[k]
                    nm = ["sgt%d" % k, "lft%d" % k, "kkt%d" % k, "sgt%d" % k, "Att%d" % k, "e1t%d" % k, "Att%d" % k]
                    ci = dr * 4 + h
                    A(lambda e: e.activation(out=sg_[:], in_=pb[:, 0:NT], func=AF.Sigmoid), r=[pn], w=[nm[0]])
                    A(lambda e: e.activation(out=lf_[:], in_=sg_[:], func=AF.Ln, bias=lbc[:, l, ci:ci + 1], scale=oml[:, l, ci:ci + 1]),
                      r=[nm[0], "lbc", "oml"], w=[nm[1]])
                    V(lambda e: e.tensor_scalar(out=kk_[:], in0=sg_[:], scalar1=-1.0, scalar2=lbm1[:, l, ci:ci + 1], op0=ALU.add, op1=ALU.mult),
                      r=[nm[0], "lbm1"], w=[nm[2]])
                    V(lambda e: e.tensor_tensor_scan(out=I_[:], data0=mskrow, data1=lf_[:], initial=0.0, op0=ALU.mult, op1=ALU.add),
                      r=[nm[1], "cs"], w=[nm[3]])
                    I3 = I_[:].rearrange("p (c t) -> p c t", t=64)
                    A3 = A_[:].rearrange("p (c t) -> p c t", t=64)
                    if dr == 0:
                        V(lambda e: e.tensor_tensor(out=A3, in0=I3, in1=I3[:, :, 31:32].to_broadcast([128, 4, 64]), op=ALU.subtract),
                          r=[nm[3]], w=[nm[4]])
                        A(lambda e: e.activation(out=e1_[:], in_=A_[:], func=AF.Exp), r=[nm[4]], w=[nm[5]])
                        A(lambda e: e.activation(out=e2_[:], in_=A_[:], func=AF.Exp, scale=-1.0), r=[nm[4]], w=[nm[6]])
                        G(lambda e: e.tensor_tensor(out=qtf[:, h, :], in0=q32[:, h, :], in1=e1_[:], op=ALU.mult), r=["q32", nm[5]], w=["qtf"])
                        G(lambda e: e.tensor_tensor(out=ktf[:, h, :], in0=kk_[:], in1=e2_[:], op=ALU.mult), r=[nm[2], nm[6]], w=["ktf"])
                        gc = ti * 4
                        A(lambda e: e.activation(out=ERF[:, h, gc:gc + 4], in_=I3[:, :, 31], func=AF.Exp), r=[nm[3]], w=["ERF"])
                        A(lambda e: e.activation(out=DECF[:, h, gc:gc + 4], in_=I3[:, :, 63], func=AF.Exp), r=[nm[3]], w=["DECF"])
                        V(lambda e: e.tensor_tensor(out=dtmp[:, h, :], in0=I3[:, :, 63], in1=I3[:, :, 31], op=ALU.subtract), r=[nm[3]], w=["dtmp"])
                        A(lambda e: e.activation(out=ETRF[:, h, gc:gc + 4], in_=dtmp[:, h, :], func=AF.Exp), r=["dtmp"], w=["ETRF"])
                    else:
                        V(lambda e: e.tensor_tensor(out=lf_[:], in0=I_[:], in1=lf_[:], op=ALU.subtract), r=[nm[3], nm[1]], w=[nm[1]])
                        E3 = lf_[:].rearrange("p (c t) -> p c t", t=64)
                        V(lambda e: e.tensor_tensor(out=A3, in0=E3, in1=E3[:, :, 32:33].to_broadcast([128, 4, 64]), op=ALU.subtract),
                          r=[nm[1]], w=[nm[4]])
                        A(lambda e: e.activation(out=e1_[:], in_=A_[:], func=AF.Exp, scale=-1.0), r=[nm[4]], w=[nm[5]])
                        A(lambda e: e.activation(out=e2_[:], in_=A_[:], func=AF.Exp), r=[nm[4]], w=[nm[6]])
                        G(lambda e: e.tensor_tensor(out=qtb[:, h, :], in0=q32[:, h, :], in1=e1_[:], op=ALU.mult), r=["q32", nm[5]], w=["qtb"])
                        G(lambda e: e.tensor_tensor(out=ktb[:, h, :], in0=kk_[:], in1=e2_[:], op=ALU.mult), r=[nm[2], nm[6]], w=["ktb"])
                        A(lambda e: e.activation(out=etrb[:, h, :], in_=E3[:, :, 32], func=AF.Exp), r=[nm[1]], w=["etrb"])
                        A(lambda e: e.activation(out=decb[:, h, :], in_=I3[:, :, 63], func=AF.Exp), r=[nm[3]], w=["decb"])
                        V(lambda e: e.tensor_tensor(out=dtmp[:, h, :], in0=I3[:, :, 63], in1=E3[:, :, 32], op=ALU.subtract), r=[nm[3], nm[1]], w=["dtmp"])
                        A(lambda e: e.activation(out=erb[:, h, :], in_=dtmp[:, h, :], func=AF.Exp), r=["dtmp"], w=["erb"])

                def state_step(c, j, cc, ktok, ktn, er_ap, etr_ap, dec_ap, kU, scn):
                    pb = ps[4 + kU % 2]
                    pn = "ps%d" % (4 + kU % 2)
                    for h in range(4):
                        T(lambda e, h=h, pb=pb: e.matmul(pb[:, h * 128:(h + 1) * 128],
                                                         lhsT=ktok[cc * 64:(cc + 1) * 64, j, h * 128:(h + 1) * 128],
                                                         rhs=vtok[cc * 64:(cc + 1) * 64, j, h * 128:(h + 1) * 128], start=True, stop=True),
                          r=[ktn, "vtok"], w=[pn])
                    V(lambda e: e.tensor_tensor(out=Sbf[:, c, :, :], in0=Sst[:], in1=er_ap.to_broadcast([128, 4, 128]), op=ALU.mult),
                      r=["Sst"] + scn, w=["Sbf%d" % c])
                    V(lambda e, pb=pb: e.tensor_tensor(out=utmp[:], in0=pb[:].rearrange("p (h e) -> p h e", e=128),
                                                       in1=etr_ap.to_broadcast([128, 4, 128]), op=ALU.mult), r=[pn] + scn, w=["utmp"])
                    V(lambda e: e.tensor_tensor(out=Sst[:], in0=Sst[:], in1=dec_ap.to_broadcast([128, 4, 128]), op=ALU.mult),
                      r=["Sst"] + scn, w=["Sst"])
                    V(lambda e: e.tensor_tensor(out=Sst[:], in0=Sst[:], in1=utmp[:], op=ALU.add), r=["Sst", "utmp"], w=["Sst"])

                ku = [0]

                V(lambda e: e.memset(Sst[:], 0.0), w=["Sst"])
                order1 = [0] + list(range(NTILES - 1, 0, -1))
                load_x(order1[0], 0)
                for oi, ti in enumerate(order1):
                    bi = oi % 2
                    s = 1 if ti == 0 else 0
                    if oi + 1 < len(order1):
                        load_x(order1[oi + 1], 1 - bi)
                    norm_to_hT(ti, bi, s)
                    stop("ta")
                    for j in range(2):
                        pb, pn = proj_tm(j, 1536)
                        A(lambda e, j=j, pb=pb: e.activation(out=vtok[:, j, :], in_=pb[:, :], func=AF.Copy), r=[pn], w=["vtok"])
                        pb, pn = proj_tm(j, 3072)
                        A(lambda e, pb=pb: e.activation(out=vg[:], in_=pb[:, :], func=AF.Gelu_apprx_tanh), r=[pn], w=["vg"])
                        A(lambda e, j=j: e.activation(out=junk[:, 0:512], in_=vg[:], func=AF.Square, accum_out=stat[:, 4 + j:5 + j]),
                          r=["vg"], w=["h2t", "stat"])
                        rstd_from_ss(stat[:, 4 + j:5 + j], stat[:, 6 + j:7 + j], 1, 1.0 / 512)
                        V(lambda e, j=j: e.scalar_tensor_tensor(out=vntok[:, j, :], in0=vg[:], scalar=stat[:, 6 + j:7 + j], in1=sgn_bc[:],
                                                                op0=ALU.mult, op1=ALU.mult), r=["vg", "stat", "sgn_bc"], w=["vntok"])
                    stop("tb")
                    for h in range(4):
                        pb, pn = proj_fm(h)
                        A(lambda e, h=h, pb=pb: e.activation(out=q32[:, h, :], in_=pb[:, 0:NT], func=AF.Copy), r=[pn], w=["q32"])
                    for h in range(4):
                        gates(h, 0, ti)
                        gates(h, 1, ti)
                    for h in range(4):
                        pb, pn = proj_fm(16 + h)
                        A(lambda e, h=h, pb=pb: e.activation(out=gsb[:, h, :], in_=pb[:, 0:NT], func=AF.Silu), r=[pn], w=["gsb"])
                        pb, pn = proj_fm(20 + h)
                        A(lambda e, h=h, pb=pb: e.activation(out=ug[:, h, :], in_=pb[:, 0:NT], func=AF.Gelu_apprx_tanh), r=[pn], w=["ug"])
                    stop("tc")
                    for j in range(2):
                        pb = ps[2 + j]
                        pn = "ps%d" % (2 + j)
                        for h in range(4):
                            T(lambda e, h=h, j=j, pb=pb: e.matmul(pb[:, h * 128:(h + 1) * 128], lhsT=vntok[:, j, h * 128:(h + 1) * 128],
                                                                  rhs=wsT[:, h, :], start=True, stop=False), r=["vntok", "wsT"], w=[pn])
                            T(lambda e, h=h, pb=pb: e.matmul(pb[:, h * 128:(h + 1) * 128], lhsT=onesb[0:1, :],
                                                             rhs=bsrow[0:1, h * 128:(h + 1) * 128], start=False, stop=True),
                              r=["onesb", "bsrow"], w=[pn])
                        V(lambda e, j=j, pb=pb: e.tensor_tensor(out=mixT[:, 4:8, j * 128:(j + 1) * 128],
                                                                in0=pb[:].rearrange("p (h i) -> p h i", i=128),
                                                                in1=ug[:, :, j * 128:(j + 1) * 128], op=ALU.mult), r=[pn, "ug"], w=["mixT"])
                    stop("td")
                    for j in range(2):
                        for h in range(4):
                            T(lambda e, h=h, j=j: e.matmul(ps[2][:, h * 128:(h + 1) * 128], lhsT=ktb[:, h, j * 128:(j + 1) * 128],
                                                           rhs=identb[:], start=True, stop=True), r=["ktb", "identb"], w=["ps2"])
                            T(lambda e, h=h, j=j: e.matmul(ps[3][:, h * 128:(h + 1) * 128], lhsT=ktf[:, h, j * 128:(j + 1) * 128],
                                                           rhs=identb[:], start=True, stop=True), r=["ktf", "identb"], w=["ps3"])
                        A(lambda e, j=j: e.activation(out=kbtok[:, j, :], in_=ps[2][:, :], func=AF.Copy), r=["ps2"], w=["kbtok"])
                        V(lambda e, j=j: e.tensor_copy(kftok[:, j, :], ps[3][:, :]), r=["ps3"], w=["kftok"])
                    stop("te")
                    for c in (3, 2, 1, 0):
                        j, cc = c // 2, c % 2
                        state_step(c, j, cc, kbtok, "kbtok", erb[:, :, c:c + 1], etrb[:, :, c:c + 1], decb[:, :, c:c + 1], ku[0], ["erb", "etrb", "decb"])
                        ku[0] += 1
                    stop("tf")
                    for j in (1, 0):
                        for h in range(4):
                            k = h % 2
                            off = k * 256
                            jc = slice(j * 128, (j + 1) * 128)
                            T(lambda e, h=h, jc=jc, off=off: e.matmul(ps[6][:, off:off + 128], lhsT=ktb[:, h, jc], rhs=qtb[:, h, jc],
                                                                      start=True, stop=True), r=["ktb", "qtb"], w=["ps6_%d" % k])
                            T(lambda e, h=h, jc=jc, off=off: e.matmul(ps[6][:, off + 128:off + 256], lhsT=ktf[:, h, jc], rhs=qtf[:, h, jc],
                                                                      start=True, stop=True), r=["ktf", "qtf"], w=["ps6_%d" % k])
                            V(lambda e, k=k, off=off: e.copy_predicated(out=scb[k][:], mask=maskB, data=ps[6][:, off:off + 128]),
                              r=["ps6_%d" % k, "m8"], w=["scb%d" % k])
                            V(lambda e, k=k, off=off: e.copy_predicated(out=scf[k][:], mask=maskF, data=ps[6][:, off + 128:off + 256]),
                              r=["ps6_%d" % k, "m8"], w=["scf%d" % k])
                            oo = ps[5][:, h * 128:(h + 1) * 128]
                            T(lambda e, h=h, j=j, k=k, oo=oo: e.matmul(oo, lhsT=vtok[:, j, h * 128:(h + 1) * 128], rhs=scb[k][:],
                                                                       start=True, stop=False), r=["vtok", "scb%d" % k], w=["ps5"])
                            T(lambda e, h=h, j=j, k=k, oo=oo: e.matmul(oo, lhsT=vtok[:, j, h * 128:(h + 1) * 128], rhs=scf[k][:],
                                                                       start=False, stop=False), r=["vtok", "scf%d" % k], w=["ps5"])
                            for cc in (1, 0):
                                c = 2 * j + cc
                                T(lambda e, h=h, c=c, cc=cc, j=j: e.matmul(ps[5][:, h * 128 + cc * 64:h * 128 + (cc + 1) * 64],
                                                                           lhsT=Sbf[:, c, h, :],
                                                                           rhs=qtb[:, h, j * 128 + cc * 64:j * 128 + (cc + 1) * 64],
                                                                           start=False, stop=(cc == 0)), r=["Sbf%d" % c, "qtb"], w=["ps5"])
                        A(lambda e, j=j: e.activation(out=obf[:, :, j * 128:(j + 1) * 128], in_=ps[5][:].rearrange("p (h i) -> p h i", i=128),
                                                      func=AF.Copy), r=["ps5"], w=["obf"])
                    stop("tg")
                    t0 = ti * NT
                    LD(lambda e, t0=t0: e.dma_start(out=OBFv[:, :, t0:t0 + NT], in_=obf[:]), r=["obf"], w=["OBF%d" % ti])
                    LD(lambda e, t0=t0: e.dma_start(out=QFv[:, :, t0:t0 + NT], in_=qtf[:]), r=["qtf"], w=["QF%d" % ti])
                    LD(lambda e, t0=t0: e.dma_start(out=GSv[:, :, t0:t0 + NT], in_=gsb[:]), r=["gsb"], w=["GS%d" % ti])
                    LD(lambda e, t0=t0: e.dma_start(out=SGv[:, :, t0:t0 + NT], in_=mixT[:, 4:8, :]), r=["mixT"], w=["SG%d" % ti])
                    LD(lambda e, ti=ti: e.dma_start(out=KFTv[:, 2 * ti:2 * ti + 2, :], in_=kftok[:]), r=["kftok"], w=["KFT%d" % ti])
                    LD(lambda e, ti=ti: e.dma_start(out=VTv[:, 2 * ti:2 * ti + 2, :], in_=vtok[:]), r=["vtok"], w=["VT%d" % ti])
                    stop("p1a")

                stop("p1")
                V(lambda e: e.memset(Sst[:], 0.0), w=["Sst"])

                def load2(ti, bi):
                    t0 = ti * NT
                    load_x(ti, bi)

                load_x(0, 0)
                for ti in range(NTILES):
                    bi = ti % 2
                    s = 1 if ti == 0 else 0
                    t0 = ti * NT
                    x = xt[bi]
                    xn = "xt%d" % bi
                    LD(lambda e, t0=t0: e.dma_start(out=obf[:], in_=OBFv[:, :, t0:t0 + NT]), r=["OBF%d" % ti], w=["obf"])
                    LD(lambda e, t0=t0: e.dma_start(out=qtf[:], in_=QFv[:, :, t0:t0 + NT]), r=["QF%d" % ti], w=["qtf"])
                    LD(lambda e, t0=t0: e.dma_start(out=gsb[:], in_=GSv[:, :, t0:t0 + NT]), r=["GS%d" % ti], w=["gsb"])
                    LD(lambda e, t0=t0: e.dma_start(out=mixT[:, 4:8, :], in_=SGv[:, :, t0:t0 + NT]), r=["SG%d" % ti], w=["mixT"])
                    LD(lambda e, ti=ti: e.dma_start(out=kftok[:], in_=KFTv[:, 2 * ti:2 * ti + 2, :]), r=["KFT%d" % ti], w=["kftok"])
                    LD(lambda e, ti=ti: e.dma_start(out=vtok[:], in_=VTv[:, 2 * ti:2 * ti + 2, :]), r=["VT%d" % ti], w=["vtok"])
                    if ti + 1 < NTILES:
                        load_x(ti + 1, 1 - bi)
                    gc = ti * 4
                    for c in range(4):
                        j, cc = c // 2, c % 2
                        state_step(c, j, cc, kftok, "kftok", ERF[:, :, gc + c:gc + c + 1], ETRF[:, :, gc + c:gc + c + 1],
                                   DECF[:, :, gc + c:gc + c + 1], ku[0], ["ERF", "ETRF", "DECF"])
                        ku[0] += 1
                    for j in range(2):
                        for h in range(4):
                            for cc in range(2):
                                c = 2 * j + cc
                                T(lambda e, h=h, c=c, cc=cc, j=j: e.matmul(ps[5][:, h * 128 + cc * 64:h * 128 + (cc + 1) * 64],
                                                                           lhsT=Sbf[:, c, h, :],
                                                                           rhs=qtf[:, h, j * 128 + cc * 64:j * 128 + (cc + 1) * 64],
                                                                           start=True, stop=True), r=["Sbf%d" % c, "qtf"], w=["ps5"])
                        V(lambda e, j=j: e.tensor_tensor(out=ofull[:, :, j * 128:(j + 1) * 128], in0=ps[5][:].rearrange("p (h i) -> p h i", i=128),
                                                         in1=obf[:, :, j * 128:(j + 1) * 128], op=ALU.add), r=["ps5", "obf"], w=["obf"])
                    for h in range(4):
                        A(lambda e, h=h: e.activation(out=sqt[:], in_=ofull[:, h, :], func=AF.Square), r=["obf"], w=["sqt"])
                        T(lambda e: e.matmul(ps[6][:, 0:NT], lhsT=ones32, rhs=sqt[:], start=True, stop=True), r=["sqt", "cs"], w=["ps6_0", "ps6_1"])
                        V(lambda e: e.tensor_scalar(out=rt[:], in0=ps[6][:, 0:NT], scalar1=1.0 / 128, scalar2=EPS, op0=ALU.mult, op1=ALU.add),
                          r=["ps6_0", "ps6_1"], w=["rt"])
                        A(lambda e: e.activation(out=rt[:], in_=rt[:], func=AF.Sqrt), r=["rt"], w=["rt"])
                        V(lambda e: e.reciprocal(out=rt[:], in_=rt[:]), r=["rt"], w=["rt"])
                        V(lambda e, h=h: e.scalar_tensor_tensor(out=mt[:], in0=ofull[:, h, :], scalar=colsA[:, 48 + l * 4 + h:49 + l * 4 + h],
                                                                in1=rt[:], op0=ALU.mult, op1=ALU.mult), r=["obf", "rt", "colsA"], w=["mt"])
                        G(lambda e, h=h: e.tensor_tensor(out=mixT[:, h, :], in0=mt[:], in1=gsb[:, h, :], op=ALU.mult), r=["mt", "gsb"], w=["mixT"])
                    for j in range(2):
                        for hf in range(2):
                            k = pj[0] % 2
                            pj[0] += 1
                            pb = ps[k]
                            pn = "ps%d" % k
                            for c in range(8):
                                T(lambda e, c=c, j=j, hf=hf, pb=pb: e.matmul(pb[:, :], lhsT=mixT[:, c, j * 128:(j + 1) * 128],
                                                                             rhs=woutb[:, c, hf * 512:(hf + 1) * 512],
                                                                             start=(c == 0), stop=(c == 7)), r=["mixT", "woutb"], w=[pn])
                            V(lambda e, hf=hf, pb=pb: e.tensor_tensor(out=tmp2[:], in0=pb[:, :], in1=g1_bc[:, s, hf * 512:(hf + 1) * 512],
                                                                      op=ALU.mult), r=[pn, "g1_bc"], w=["tmp2"])
                            V(lambda e, j=j, hf=hf: e.tensor_tensor(out=x[:, j, hf * 512:(hf + 1) * 512], in0=tmp2[:],
                                                                    in1=x[:, j, hf * 512:(hf + 1) * 512], op=ALU.add), r=["tmp2", xn], w=[xn])
                    LD(lambda e, ti=ti: e.dma_start(out=XMv[:, 2 * ti:2 * ti + 2, :], in_=x[:]), r=[xn], w=["XM%d" % ti])
                    for j in range(2):
                        A(lambda e, j=j: e.activation(out=junk[:], in_=x[:, j, :], func=AF.Square, accum_out=stat[:, j:j + 1]),
                          r=[xn], w=["h2t", "stat"])
                    rstd_from_ss(stat[:, 0:2], stat[:, 2:4], 2, 1.0 / D)
                    for j in range(2):
                        sub = 2 * ti + j
                        V(lambda e, j=j: e.scalar_tensor_tensor(out=h2t[:], in0=x[:, j, :], scalar=stat[:, 2 + j:3 + j], in1=a2_bc[:, s, :],
                                                                op0=ALU.mult, op1=ALU.mult), r=[xn, "stat", "a2_bc"], w=["h2t"])
                        G(lambda e: e.tensor_tensor(out=h2t[:], in0=h2t[:], in1=b2_bc[:, s, :], op=ALU.add), r=["h2t", "b2_bc"], w=["h2t"])
                        GD(lambda e, sub=sub: e.dma_start(out=H2v[:, sub, :], in_=h2t[:]), r=["h2t"], w=["H2_%d" % sub])
                        for c in range(8):
                            pb = ps[2 + c // 4]
                            pn = "ps%d" % (2 + c // 4)
                            T(lambda e, c=c, pb=pb: e.transpose(out=pb[:, (c % 4) * 128:(c % 4 + 1) * 128], in_=h2t[:, c * 128:(c + 1) * 128],
                                                                identity=ident), r=["h2t", "cs"], w=[pn])
                        A(lambda e: e.activation(out=h2T[:, 0:4, :], in_=ps[2][:].rearrange("p (c t) -> p c t", t=128), func=AF.Copy),
                          r=["ps2"], w=["h2T"])
                        A(lambda e: e.activation(out=h2T[:, 4:8, :], in_=ps[3][:].rearrange("p (c t) -> p c t", t=128), func=AF.Copy),
                          r=["ps3"], w=["h2T"])
                        for c in range(8):
                            T(lambda e, c=c: e.matmul(ps[4][:, 0:36], lhsT=h2T[:, c, :], rhs=wr32[:, c, :], start=(c == 0), stop=False),
                              r=["h2T", "wr32"], w=["ps4"])
                        T(lambda e: e.matmul(ps[4][:, 0:36], lhsT=ones32[0:1, :], rhs=brow[0:1, :], start=False, stop=True),
                          r=["cs", "brow"], w=["ps4"])
                        V(lambda e: e.tensor_copy(Ls[:], ps[4][:, 0:36]), r=["ps4"], w=["Ls"])
                        R_ = ["rsm"]
                        V(lambda e: e.tensor_reduce(out=rsm[:, 0:1], in_=Ls[:, 0:4], axis=AX.X, op=ALU.max), r=["Ls"], w=R_)
                        V(lambda e: e.tensor_scalar(out=rsm[:, 1:2], in0=rsm[:, 0:1], scalar1=-1.0, scalar2=None, op0=ALU.mult), r=R_, w=R_)
                        A(lambda e: e.activation(out=rsm[:, 48:52], in_=Ls[:, 0:4], func=AF.Exp, bias=rsm[:, 1:2], accum_out=rsm[:, 2:3]),
                          r=["Ls"] + R_, w=R_)
                        V(lambda e: e.reciprocal(out=rsm[:, 3:4], in_=rsm[:, 2:3]), r=R_, w=R_)
                        V(lambda e: e.tensor_scalar(out=rsm[:, 4:8], in0=Ls[:, 0:4], scalar1=rsm[:, 0:1], scalar2=None, op0=ALU.is_equal),
                          r=["Ls"] + R_, w=R_)
                        V(lambda e: e.tensor_scalar(out=rsm[:, 8:16], in0=Ls[:, 4:12], scalar1=rsm[:, 4:5], scalar2=None, op0=ALU.mult),
                          r=["Ls"] + R_, w=R_)
                        for g in range(1, 4):
                            V(lambda e, g=g: e.scalar_tensor_tensor(out=rsm[:, 8:16], in0=Ls[:, 4 + 8 * g:12 + 8 * g], scalar=rsm[:, 4 + g:5 + g],
                                                                    in1=rsm[:, 8:16], op0=ALU.mult, op1=ALU.add), r=["Ls"] + R_, w=R_)
                        V(lambda e: e.max(out=rsm[:, 16:24], in_=rsm[:, 8:16]), r=R_, w=R_)
                        V(lambda e: e.tensor_scalar(out=rsm[:, 24:32], in0=rsm[:, 8:16], scalar1=rsm[:, 16:17], scalar2=None, op0=ALU.is_equal), r=R_, w=R_)
                        V(lambda e: e.tensor_scalar(out=rsm[:, 32:40], in0=rsm[:, 8:16], scalar1=rsm[:, 17:18], scalar2=None, op0=ALU.is_equal), r=R_, w=R_)
                        V(lambda e: e.tensor_tensor(out=rsm[:, 40:41], in0=rsm[:, 16:17], in1=rsm[:, 17:18], op=ALU.subtract), r=R_, w=R_)
                        A(lambda e: e.activation(out=rsm[:, 41:42], in_=rsm[:, 40:41], func=AF.Sigmoid), r=R_, w=R_)
                        V(lambda e, sub=sub: e.tensor_tensor(out=W12[:, sub, 0:1], in0=rsm[:, 41:42], in1=rsm[:, 3:4], op=ALU.mult), r=R_, w=["W12"])
                        V(lambda e, sub=sub: e.tensor_tensor(out=W12[:, sub, 1:2], in0=rsm[:, 3:4], in1=W12[:, sub, 0:1], op=ALU.subtract),
                          r=R_ + ["W12"], w=["W12"])
                        for g in range(4):
                            V(lambda e, g=g, sub=sub: e.tensor_scalar(out=M1all[:, sub, g * 8:(g + 1) * 8], in0=rsm[:, 24:32],
                                                                      scalar1=rsm[:, 4 + g:5 + g], scalar2=None, op0=ALU.mult), r=R_, w=["M1all"])
                            V(lambda e, g=g, sub=sub: e.tensor_scalar(out=M2all[:, sub, g * 8:(g + 1) * 8], in0=rsm[:, 32:40],
                                                                      scalar1=rsm[:, 4 + g:5 + g], scalar2=None, op0=ALU.mult), r=R_, w=["M2all"])
                P.barrier_all()
                sm.close(); open_stacks.remove(sm)

                stop("p2")
                se = newstack()
                pre = sb("pre", [128, NE], st=se)
                cnt_i = sb("cnt_i", [128, NE], I32, st=se)
                padf = sb("padf", [128, NE], st=se)
                incl = sb("incl", [128, NE], st=se)
                offs = sb("offs", [128, NE], st=se)
                cmp3 = sb("cmp3", [128, NB, NE], st=se)
                bef = sb("bef", [128, NB], st=se)
                tmpr = sb("tmpr", [128, NE], st=se)
                tmpr2 = sb("tmpr2", [128, NE], st=se)
                hrow = [sb("hrow%d" % i, [128, D], BF16, st=se) for i in range(2)]
                NW = 3
                wgb = [sb("wgb%d" % i, [128, 4096], BF16, st=se) for i in range(NW)]
                wub = [sb("wub%d" % i, [128, 4096], BF16, st=se) for i in range(NW)]
                wdb = [sb("wdb%d" % i, [128, 4096], BF16, st=se) for i in range(NW)]
                xbT = [sb("xbT%d" % i, [128, 8, 128], BF16, st=se) for i in range(2)]
                sgate = [sb("sgate%d" % i, [128, 512], st=se) for i in range(2)]
                actb = [sb("actb%d" % i, [128, 512], BF16, st=se) for i in range(2)]
                actT = [sb("actT%d" % i, [128, 4, 128], BF16, st=se) for i in range(2)]
                ybuf = [sb("ybuf%d" % i, [128, D], st=se) for i in range(2)]
                y1 = [sb("y1_%d" % i, [128, D], st=se) for i in range(2)]
                y2 = [sb("y2_%d" % i, [128, D], st=se) for i in range(2)]
                xm = [sb("xm%d" % i, [128, D], st=se) for i in range(2)]
                junk2 = sb("junk2", [128, D], st=se)
                st2 = sb("st2", [128, 4], st=se)
                g2_bc = sb("g2_bc", [128, 2, D], st=se)
                Rall = sb("Rall", [128, NSUB, NE], st=se)
                Mall = sb("Mall", [128, NSUB, NE], BF16, st=se)
                dest_f = sb("dest_f", [128, NSUB, 2], st=se)
                dest_i = sb("dest_i", [128, NSUB, 2], I32, st=se)
                widx = sb("widx", [128, NB, 2], I32, st=se)
                for s in range(2):
                    LD(lambda e, s=s: e.dma_start(out=g2_bc[:, s, :], in_=MODROW[l, s:s + 1, 5 * D:6 * D].to_broadcast([128, D])), w=["g2_bc"])

                zrow = sb("zrow", [128, D], BF16, st=se)
                V(lambda e: e.memset(zrow[:], 0.0), w=["zrow"])
                XBz = XB.rearrange("(n p) f -> p n f", p=128)
                ZS = 10 if NSB % 10 == 0 else 12
                assert NSB % ZS == 0
                for n0 in range(0, NSB, ZS):
                    LD(lambda e, n0=n0: e.dma_start(out=XBz[:, n0:n0 + ZS, :], in_=zrow[:].unsqueeze(1).to_broadcast([128, ZS, D])),
                       r=["zrow"], w=["XBz"])
                V(lambda e: e.tensor_tensor(out=Mall[:], in0=M1all[:], in1=M2all[:], op=ALU.add), r=["M1all", "M2all"], w=["Mall"])
                V(lambda e: e.memset(pre[:], 0.0), w=["pre"])
                for i in range(NSUB):
                    T(lambda e, i=i: e.matmul(ps[0][:, 0:NE], lhsT=trib[:], rhs=Mall[:, i, :], start=True, stop=True), r=["Mall", "trib"], w=["ps0"])
                    T(lambda e, i=i: e.matmul(ps[1][:, 0:NE], lhsT=onesb[:], rhs=Mall[:, i, :], start=True, stop=True), r=["Mall", "onesb"], w=["ps1"])
                    V(lambda e, i=i: e.tensor_tensor(out=Rall[:, i, :], in0=ps[0][:, 0:NE], in1=pre[:], op=ALU.add), r=["ps0", "pre"], w=["Rall"])
                    V(lambda e: e.tensor_tensor(out=pre[:], in0=ps[1][:, 0:NE], in1=pre[:], op=ALU.add), r=["ps1", "pre"], w=["pre"])
                cmpT = cmp3[:].rearrange("p n e -> p (n e)").rearrange("p (e n) -> p e n", n=NB)
                V(lambda e: e.tensor_tensor(out=cmpT, in0=pre[:].unsqueeze(2).to_broadcast([128, NE, NB]),
                                            in1=nblk.unsqueeze(1).to_broadcast([128, NE, NB]), op=ALU.is_gt), r=["pre", "cs"], w=["cmp3"])
                V(lambda e: e.tensor_reduce(out=padf[:], in_=cmpT, axis=AX.X, op=ALU.add), r=["cmp3"], w=["padf"])
                V(lambda e: e.tensor_scalar(out=padf[:], in0=padf[:], scalar1=float(BLK), scalar2=None, op0=ALU.mult), r=["padf"], w=["padf"])
                V(lambda e: e.tensor_tensor_scan(out=incl[:], data0=ones32[:, 0:NE], data1=padf[:], initial=0.0, op0=ALU.mult, op1=ALU.add),
                  r=["padf", "cs"], w=["incl"])
                V(lambda e: e.tensor_tensor(out=offs[:], in0=incl[:], in1=padf[:], op=ALU.subtract), r=["incl", "padf"], w=["offs"])
                V(lambda e: e.tensor_tensor(out=cmp3[:], in0=nblk.unsqueeze(2).to_broadcast([128, NB, NE]),
                                            in1=incl[:].unsqueeze(1).to_broadcast([128, NB, NE]), op=ALU.is_ge), r=["incl", "cs"], w=["cmp3"])
                V(lambda e: e.tensor_reduce(out=bef[:], in_=cmp3[:], axis=AX.X, op=ALU.add), r=["cmp3"], w=["bef"])
                V(lambda e: e.tensor_scalar(out=bef[:], in0=bef[:], scalar1=31.0, scalar2=128.0, op0=ALU.min, op1=ALU.mult), r=["bef"], w=["bef"])
                V(lambda e: e.tensor_scalar(out=bef[:], in0=bef[:], scalar1=pcol, scalar2=None, op0=ALU.add), r=["bef", "cs"], w=["bef"])
                V(lambda e: e.tensor_copy(widx[:, :, 0], bef[:]), r=["bef"], w=["widx"])
                V(lambda e: e.tensor_scalar(out=bef[:], in0=bef[:], scalar1=4096.0, scalar2=None, op0=ALU.add), r=["bef"], w=["bef"])
                V(lambda e: e.tensor_copy(widx[:, :, 1], bef[:]), r=["bef"], w=["widx"])
                for i in range(NSUB):
                    V(lambda e, i=i: e.tensor_tensor(out=tmpr[:], in0=Rall[:, i, :], in1=offs[:], op=ALU.add), r=["Rall", "offs"], w=["tmpr"])
                    V(lambda e, i=i: e.scalar_tensor_tensor(out=tmpr2[:], in0=tmpr[:], scalar=1.0, in1=M1all[:, i, :], op0=ALU.mult, op1=ALU.mult,
                                                            accum_out=dest_f[:, i, 0:1]), r=["tmpr", "M1all"], w=["tmpr2", "dest_f"])
                    V(lambda e, i=i: e.scalar_tensor_tensor(out=tmpr2[:], in0=tmpr[:], scalar=1.0, in1=M2all[:, i, :], op0=ALU.mult, op1=ALU.mult,
                                                            accum_out=dest_f[:, i, 1:2]), r=["tmpr", "M2all"], w=["tmpr2", "dest_f"])
                V(lambda e: e.tensor_copy(dest_i[:], dest_f[:]), r=["dest_f"], w=["dest_i"])
                for i in range(NSUB):
                    hb = hrow[i % 2]
                    hn = "hrow%d" % (i % 2)
                    LD(lambda e, i=i, hb=hb: e.dma_start(out=hb[:], in_=H2v[:, i, :]), r=["H2_%d" % i], w=[hn])
                    for k in range(2):
                        GD(lambda e, i=i, k=k, hb=hb: e.indirect_dma_start(
                            out=XB, out_offset=bass.IndirectOffsetOnAxis(ap=dest_i[:, i, k:k + 1], axis=0), in_=hb[:], in_offset=None),
                           r=[hn, "dest_i", "XBz"], w=["XBs%d_%d" % (i, k)])
                P.barrier_all()

                stop("p3")
                XBv = XB.rearrange("(n p) f -> p n f", p=128)
                YBv = YB.rearrange("(n p) f -> p n f", p=128)

                def wload(n):
                    k = n % NW
                    for hf in range(2):
                        GD(lambda e, n=n, hf=hf, k=k: e.indirect_dma_start(
                            out=wgb[k][:, hf * 2048:(hf + 1) * 2048], out_offset=None, in_=wg2[l],
                            in_offset=bass.IndirectOffsetOnAxis(ap=widx[:, n, hf:hf + 1], axis=0)), r=["widx"], w=["wgb%d" % k])
                        GD(lambda e, n=n, hf=hf, k=k: e.indirect_dma_start(
                            out=wub[k][:, hf * 2048:(hf + 1) * 2048], out_offset=None, in_=wu2[l],
                            in_offset=bass.IndirectOffsetOnAxis(ap=widx[:, n, hf:hf + 1], axis=0)), r=["widx"], w=["wub%d" % k])
                        GD(lambda e, n=n, hf=hf, k=k: e.indirect_dma_start(
                            out=wdb[k][:, hf * 2048:(hf + 1) * 2048], out_offset=None, in_=wd2[l],
                            in_offset=bass.IndirectOffsetOnAxis(ap=widx[:, n, hf:hf + 1], axis=0)), r=["widx"], w=["wdb%d" % k])

                def stA1(sbi):
                    p = sbi % 2
                    hb, hn = hrow[p], "hrow%d" % p
                    LD(lambda e: e.dma_start(out=hb[:], in_=XBv[:, sbi, :]), r=[], w=[hn])
                    for c in range(8):
                        T(lambda e, c=c: e.matmul(ps[4 + c // 4][:, (c % 4) * 128:(c % 4 + 1) * 128], lhsT=hb[:, c * 128:(c + 1) * 128],
                                                  rhs=identb[:], start=True, stop=True), r=[hn, "identb"], w=["ps%d" % (4 + c // 4)])
                    A(lambda e: e.activation(out=xbT[p][:, 0:4, :].rearrange("p c t -> p (c t)"), in_=ps[4][:, :], func=AF.Copy),
                      r=["ps4"], w=["xbT%d" % p])
                    V(lambda e: e.tensor_copy(xbT[p][:, 4:8, :].rearrange("p c t -> p (c t)"), ps[5][:, :]), r=["ps5"], w=["xbT%d" % p])

                def stA2(sbi):
                    p = sbi % 2
                    kw = (sbi // (BLK // 128)) % NW
                    wg3 = wgb[kw][:].rearrange("p (c f) -> p c f", f=512)
                    wu3 = wub[kw][:].rearrange("p (c f) -> p c f", f=512)
                    for c in range(8):
                        T(lambda e, c=c: e.matmul(ps[2 * p][:, :], lhsT=xbT[p][:, c, :], rhs=wg3[:, c, :], start=(c == 0), stop=(c == 7)),
                          r=["xbT%d" % p, "wgb%d" % kw], w=["ps%d" % (2 * p)])
                    for c in range(8):
                        T(lambda e, c=c: e.matmul(ps[2 * p + 1][:, :], lhsT=xbT[p][:, c, :], rhs=wu3[:, c, :], start=(c == 0), stop=(c == 7)),
                          r=["xbT%d" % p, "wub%d" % kw], w=["ps%d" % (2 * p + 1)])

                def stA3(sbi):
                    p = sbi % 2
                    A(lambda e: e.activation(out=sgate[p][:], in_=ps[2 * p][:, :], func=AF.Silu), r=["ps%d" % (2 * p)], w=["sgate%d" % p])
                    V(lambda e: e.tensor_tensor(out=actb[p][:], in0=ps[2 * p + 1][:, :], in1=sgate[p][:], op=ALU.mult),
                      r=["ps%d" % (2 * p + 1), "sgate%d" % p], w=["actb%d" % p])

                def stB1(sbi):
                    p = sbi % 2
                    for c in range(4):
                        T(lambda e, c=c: e.matmul(ps[6][:, c * 128:(c + 1) * 128], lhsT=actb[p][:, c * 128:(c + 1) * 128], rhs=identb[:],
                                                  start=True, stop=True), r=["actb%d" % p, "identb"], w=["ps6"])
                    V(lambda e: e.tensor_copy(actT[p][:].rearrange("p c t -> p (c t)"), ps[6][:, :]), r=["ps6"], w=["actT%d" % p])

                def stB2(sbi):
                    p = sbi % 2
                    kw = (sbi // (BLK // 128)) % NW
                    wd3 = wdb[kw][:].rearrange("p (c f) -> p c f", f=1024)
                    for hf in range(2):
                        for c in range(4):
                            T(lambda e, c=c, hf=hf: e.matmul(ps[6 + hf][:, :], lhsT=actT[p][:, c, :], rhs=wd3[:, c, hf * 512:(hf + 1) * 512],
                                                             start=(c == 0), stop=(c == 3)), r=["actT%d" % p, "wdb%d" % kw], w=["ps%d" % (6 + hf)])
                    yb, yn = ybuf[p], "ybuf%d" % p
                    A(lambda e: e.activation(out=yb[:, 0:512], in_=ps[6][:, :], func=AF.Copy), r=["ps6"], w=[yn])
                    V(lambda e: e.tensor_copy(yb[:, 512:1024], ps[7][:, :]), r=["ps7"], w=[yn])
                    LD(lambda e: e.dma_start(out=YBv[:, sbi, :], in_=yb[:]), r=[yn], w=["YB%d" % sbi])

                SPB = BLK // 128
                wload(0)
                wload(1)
                stA1(0)
                stA2(0)
                stA3(0)
                for sbi in range(NSB):
                    n = sbi // SPB
                    if sbi % SPB == 0 and n + 2 < NB:
                        wload(n + 2)
                    if sbi + 1 < NSB:
                        stA1(sbi + 1)
                    stB1(sbi)
                    if sbi + 1 < NSB:
                        stA2(sbi + 1)
                        stA3(sbi + 1)
                    stB2(sbi)
                P.barrier_all()

                stop("p4")
                XOv = XS1.rearrange("(n p) f -> p n f", p=128)
                OUTv = out.rearrange("(n p) f -> p n f", p=128)
                for i in range(NSUB):
                    if last and i < 2:
                        continue
                    k = i % 2
                    s = 1 if i < 2 else 0
                    GD(lambda e, i=i, k=k: e.indirect_dma_start(out=y1[k][:], out_offset=None, in_=YB,
                                                                in_offset=bass.IndirectOffsetOnAxis(ap=dest_i[:, i, 0:1], axis=0)),
                       r=["dest_i"], w=["y1_%d" % k])
                    GD(lambda e, i=i, k=k: e.indirect_dma_start(out=y2[k][:], out_offset=None, in_=YB,
                                                                in_offset=bass.IndirectOffsetOnAxis(ap=dest_i[:, i, 1:2], axis=0)),
                       r=["dest_i"], w=["y2_%d" % k])
                    LD(lambda e, i=i, k=k: e.dma_start(out=xm[k][:], in_=XMv[:, i, :]), r=["XM%d" % (i // 2)], w=["xm%d" % k])
                    V(lambda e, i=i, k=k: e.tensor_scalar(out=y1[k][:], in0=y1[k][:], scalar1=W12[:, i, 0:1], scalar2=None, op0=ALU.mult),
                      r=["y1_%d" % k, "W12"], w=["y1_%d" % k])
                    V(lambda e, i=i, k=k: e.scalar_tensor_tensor(out=y1[k][:], in0=y2[k][:], scalar=W12[:, i, 1:2], in1=y1[k][:],
                                                                 op0=ALU.mult, op1=ALU.add), r=["y1_%d" % k, "y2_%d" % k, "W12"], w=["y1_%d" % k])
                    V(lambda e, k=k, s=s: e.tensor_tensor(out=y1[k][:], in0=y1[k][:], in1=g2_bc[:, s, :], op=ALU.mult),
                      r=["y1_%d" % k, "g2_bc"], w=["y1_%d" % k])
                    V(lambda e, k=k: e.tensor_tensor(out=xm[k][:], in0=xm[k][:], in1=y1[k][:], op=ALU.add), r=["xm%d" % k, "y1_%d" % k], w=["xm%d" % k])
                    if not last:
                        LD(lambda e, i=i, k=k: e.dma_start(out=XOv[:, i, :], in_=xm[k][:]), r=["xm%d" % k], w=["XIN%d" % (i // 2)])
                    else:
                        if dbg:
                            LD(lambda e, i=i, k=k: e.dma_start(out=XOv[:, i, :], in_=xm[k][:]), r=["xm%d" % k], w=["XIN%d" % (i // 2)])
                        A(lambda e, k=k: e.activation(out=junk2[:], in_=xm[k][:], func=AF.Square, accum_out=st2[:, 0:1]), r=["xm%d" % k], w=["junk2", "st2"])
                        V(lambda e: e.tensor_scalar(out=st2[:, 1:2], in0=st2[:, 0:1], scalar1=1.0 / D, scalar2=EPS, op0=ALU.mult, op1=ALU.add),
                          r=["st2"], w=["st2"])
                        A(lambda e: e.activation(out=st2[:, 1:2], in_=st2[:, 1:2], func=AF.Sqrt), r=["st2"], w=["st2"])
                        V(lambda e: e.reciprocal(out=st2[:, 2:3], in_=st2[:, 1:2]), r=["st2"], w=["st2"])
                        V(lambda e, k=k: e.scalar_tensor_tensor(out=xm[k][:], in0=xm[k][:], scalar=st2[:, 2:3], in1=fn_bc[:], op0=ALU.mult, op1=ALU.mult),
                          r=["xm%d" % k, "st2", "fn_bc"], w=["xm%d" % k])
                        LD(lambda e, i=i, k=k: e.dma_start(out=OUTv[:, i - 2, :], in_=xm[k][:]), r=["xm%d" % k], w=["OUT"], is_out=True)
                if dbg and last:
                    LD(lambda e: e.dma_start(out=YBv[:, NSB - 1, :], in_=fn_bc[:]), r=["fn_bc"], w=["YBdbg"])
                    LD(lambda e: e.dma_start(out=YBv[:, NSB - 2, 0:4], in_=st2[:]), r=["st2"], w=["YBdbg2"])
                P.barrier_all()
                se.close(); open_stacks.remove(se)
        except _Stop:
            for stx in reversed(open_stacks):
                stx.close()
        P.finish()
        block = E(nc.Block())
        P.replay(block)
    return nc


def _consts():
    c = np.zeros((128, CW), np.float32)
    c[:, 0:128] = np.eye(128, dtype=np.float32)
    tp = np.arange(128)[:, None]
    tt = np.arange(128)[None, :]
    c[:, 128:256] = (tp < tt).astype(np.float32)
    m = np.ones(256, np.float32)
    m[::64] = 0.0
    c[:, 256:512] = m[None, :]
    c[:, 512:640] = 1.0
    c[:, 640:640 + NB] = (np.arange(NB) * float(BLK))[None, :]
    c[:, 760] = np.arange(128)
    same = (tp // 64) == (tt // 64)
    m8 = np.zeros((128, 256), np.uint8)
    m8[:, 0:128] = (same & (tt >= tp)).astype(np.uint8)
    m8[:, 128:256] = (same & (tt <= tp)).astype(np.uint8)
    return c, m8


def _relay_gate(w):
    a = w.reshape(NE, 8, 128, 512).transpose(0, 2, 1, 3).reshape(NE, 128, 2, 2048)
    return np.ascontiguousarray(a.transpose(2, 0, 1, 3).reshape(2 * NE * 128, 2048))


def _relay_down(w):
    a = w.reshape(NE, 4, 128, 1024).transpose(0, 2, 1, 3).reshape(NE, 128, 2, 2048)
    return np.ascontiguousarray(a.transpose(2, 0, 1, 3).reshape(2 * NE * 128, 2048))


def make_in_maps(inputs, cores, small=False):
    f = lambda a: np.ascontiguousarray(np.asarray(a, dtype=np.float32))
    x, c, ctx, c_ctx = f(inputs['x']), f(inputs['c']), f(inputs['ctx']), f(inputs['c_ctx'])
    norm1, norm2 = f(inputs['norm1']), f(inputs['norm2'])
    cst, cst8 = _consts()
    shared = dict(
        w_mod=f(inputs['w_mod']), b_mod=f(inputs['b_mod']), w_in=f(inputs['w_in']), n2row=norm2,
        sgn=f(inputs['sgu_norm']), sgw=f(inputs['sgu_w']), sgb=f(inputs['sgu_b']).reshape(2, 512),
        w_out=f(inputs['w_out']),
        wrt=np.ascontiguousarray(np.concatenate([f(inputs['w_group']), f(inputs['w_router'])], axis=2)),
        brt=np.ascontiguousarray(np.concatenate([f(inputs['b_group']), f(inputs['b_router'])], axis=1)),
        wg2_0=_relay_gate(f(inputs['w_gate'][0])), wg2_1=_relay_gate(f(inputs['w_gate'][1])),
        wu2_0=_relay_gate(f(inputs['w_up'][0])), wu2_1=_relay_gate(f(inputs['w_up'][1])),
        wd2_0=_relay_down(f(inputs['w_down'][0])), wd2_1=_relay_down(f(inputs['w_down'][1])),
        fnorm=f(inputs['final_norm']).reshape(1, D), cst=cst, cst8=cst8,
    )
    if small:
        for k in list(shared):
            if k[:3] in ("wg2", "wu2", "wd2"):
                shared[k] = np.zeros((8, 8), np.float32)
    maps = []
    for b in cores:
        rows = np.zeros((72, 128), np.float32)
        for l in range(2):
            rows[l * 16:l * 16 + 8] = norm1[l].reshape(8, 128)
            rows[l * 16 + 8:l * 16 + 16] = norm2[l].reshape(8, 128)
        rows[32:48] = f(inputs['lb_logits']).reshape(16, 128)
        rows[48:56] = f(inputs['hgrn_norm']).reshape(8, 128)
        rows[56:64] = c[b].reshape(8, 128)
        rows[64:72] = c_ctx.reshape(8, 128)
        m = dict(shared)
        m['xs'] = np.ascontiguousarray(np.concatenate([ctx[b], x[b]], axis=0))
        m['rows_in'] = rows
        maps.append(m)
    return maps


def kernel(**inputs):
    n = 8
    nc = build_nc()
    in_maps = make_in_maps(inputs, list(range(n)))
    res = run_bass_kernel_spmd(nc, in_maps, core_ids=list(range(n)))
    return np.stack([np.asarray(r["out"], dtype=np.float32) for r in res.results], axis=0)
```

```python
import numpy as np
from contextlib import ExitStack
import concourse.bass as bass
import concourse.mybir as mybir
from concourse.bass_utils import run_bass_kernel_spmd

F32 = mybir.dt.float32
BF16 = mybir.dt.bfloat16
I32 = mybir.dt.int32
U8 = mybir.dt.uint8
AF = mybir.ActivationFunctionType
ALU = mybir.AluOpType
AX = mybir.AxisListType

D = 1024
TL = 4096
TC = 256
TOK = TL + TC
NT = 256
NTILES = TOK // NT
NSUB = TOK // 128
NCH = TOK // 64
INW = 3584
NE = 32
BLK = 128
NB = (NSUB * 2 * 128 + BLK - 1) // BLK + NE
NSB = NB * (BLK // 128)
EPS = 1e-6
CW = 1024


class _Rec:
    def __init__(self):
        self.calls = []

    def __getattr__(self, name):
        def f(*a, **k):
            self.calls.append((name, a, k))
            return self
        return f


def _bind(fn):
    rec = _Rec()
    fn(rec)
    assert len(rec.calls) == 1, rec.calls
    name, a, k = rec.calls[0]
    return lambda e: getattr(e, name)(*a, **k)


class Prog:
    ENG = ['pe', 'dve', 'act', 'pool', 'sp']
    NDS = 16

    def __init__(self, nc, stack):
        self.nc = nc
        self.q = {e: [] for e in self.ENG}
        self.cnt = {e: 0 for e in self.ENG}
        self.sems = {}
        for e in self.ENG:
            self.sems['s_' + e] = stack.enter_context(nc.semaphore('s_' + e))
        for e in ('sp', 'pool', 'act'):
            for i in range(self.NDS):
                self.sems['d_%s%d' % (e, i)] = stack.enter_context(nc.semaphore('d_%s%d' % (e, i)))
        self.dcnt = {e: 0 for e in ('sp', 'pool', 'act')}
        self.waited = {e: {} for e in self.ENG}
        self.lastw = {}
        self.readers = {}
        self.out_toks = []

    def _deps(self, reads, writes):
        deps = []
        for b in reads:
            if b in self.lastw:
                deps.append(self.lastw[b])
        for b in writes:
            if b in self.lastw:
                deps.append(self.lastw[b])
            deps.extend(self.readers.get(b, []))
        return deps

    def _wait(self, eng, tok):
        key, val, src = tok
        if src == 'pe' and eng == 'pe' and key == 's_pe':
            return
        if self.waited[eng].get(key, 0) >= val:
            return
        self.waited[eng][key] = val
        sem = self.sems[key]
        self.q[eng].append(lambda e, sem=sem, val=val: e.wait_ge(sem, val))

    def _record(self, tok, reads, writes):
        for b in reads:
            self.readers.setdefault(b, []).append(tok)
        for b in writes:
            self.lastw[b] = tok
            self.readers[b] = []

    def op(self, eng, fn, reads=(), writes=()):
        fn = _bind(fn)
        for tok in self._deps(reads, writes):
            self._wait(eng, tok)
        self.cnt[eng] += 1
        seq = self.cnt[eng]
        sem = self.sems['s_' + eng]
        self.q[eng].append(lambda e, fn=fn, sem=sem: fn(e).then_inc(sem, 1))
        tok = ('s_' + eng, seq, eng)
        self._record(tok, reads, writes)
        return tok

    def dma(self, eng, fn, reads=(), writes=(), is_out=False):
        fn = _bind(fn)
        for tok in self._deps(reads, writes):
            self._wait(eng, tok)
        k = self.dcnt[eng]
        self.dcnt[eng] += 1
        slot = k % self.NDS
        val = 16 * (k // self.NDS + 1)
        key = 'd_%s%d' % (eng, slot)
        if k >= self.NDS:
            self._wait(eng, (key, val - 16, eng))
        sem = self.sems[key]
        self.q[eng].append(lambda e, fn=fn, sem=sem: fn(e).then_inc(sem, 16))
        tok = (key, val, eng)
        self._record(tok, reads, writes)
        if is_out:
            self.out_toks.append(tok)
        return tok

    def barrier_all(self):
        toks = []
        for e in self.ENG:
            if self.cnt[e] > 0:
                toks.append(('s_' + e, self.cnt[e], e))
        for e in ('sp', 'pool', 'act'):
            k = self.dcnt[e]
            for slot in range(self.NDS):
                n = (k - slot + self.NDS - 1) // self.NDS if k > slot else 0
                if n > 0:
                    toks.append(('d_%s%d' % (e, slot), 16 * n, e))
        for e in self.ENG:
            for t in toks:
                if t[2] == 'pe' and e == 'pe' and t[0] == 's_pe':
                    pass
                key, val, src = t
                if self.waited[e].get(key, 0) >= val:
                    continue
                self.waited[e][key] = val
                sem = self.sems[key]
                self.q[e].append(lambda en, sem=sem, val=val: en.wait_ge(sem, val))
        self.lastw = {}
        self.readers = {}

    def finish(self):
        for tok in self.out_toks:
            self._wait('sp', tok)

    def replay(self, block):
        q = self.q

        @block.tensor
        def _(e):
            for f in q['pe']:
                f(e)

        @block.vector
        def _(e):
            for f in q['dve']:
                f(e)

        @block.scalar
        def _(e):
            for f in q['act']:
                f(e)

        @block.gpsimd
        def _(e):
            for f in q['pool']:
                f(e)

        @block.sync
        def _(e):
            for f in q['sp']:
                f(e)


class _Stop(Exception):
    pass


def build_nc(n_layers=2, dbg=None):
    nc = bass.Bass("TRN2", target_bir_lowering=False)

    def din(name, shape, dt=F32):
        return nc.dram_tensor(name, shape, dt, kind="ExternalInput").ap()

    def dint(name, shape, dt=F32):
        return nc.dram_tensor(name, shape, dt, kind=("ExternalOutput" if dbg else "Internal")).ap()

    xs = din("xs", [TOK, D])
    rows_in = din("rows_in", [72, 128])
    w_mod = din("w_mod", [2, D, 6 * D])
    b_mod = din("b_mod", [2, 6 * D])
    w_in = din("w_in", [2, D, INW])
    n2row = din("n2row", [2, D])
    sgn = din("sgn", [2, 512])
    sgw = din("sgw", [2, 4, 128, 128])
    sgb = din("sgb", [2, 512])
    w_out = din("w_out", [2, D, D])
    wrt = din("wrt", [2, D, 36])
    brt = din("brt", [2, 36])
    esh = [8, 8] if dbg in ("s0", "l0", "p1a", "p1", "p2", "p3", "ta", "tb", "tc", "td", "te", "tf", "tg") else [8192, 2048]
    wg2 = [din("wg2_%d" % i, esh) for i in range(2)]
    wu2 = [din("wu2_%d" % i, esh) for i in range(2)]
    wd2 = [din("wd2_%d" % i, esh) for i in range(2)]
    fnorm = din("fnorm", [1, D])
    cst = din("cst", [128, CW])
    cst8 = din("cst8", [128, 256], U8)
    out = nc.dram_tensor("out", [TL, D], F32, kind="ExternalOutput").ap()

    MODROW = dint("MODROW", [2, 2, 6 * D])
    OBF = dint("OBF", [512, TOK])
    QF = dint("QF", [512, TOK], BF16)
    GS = dint("GS", [512, TOK], BF16)
    SGD = dint("SGD", [512, TOK], BF16)
    KFT = dint("KFT", [TOK, 512], BF16)
    VT = dint("VT", [TOK, 512], BF16)
    XM = dint("XM", [TOK, D])
    H2 = dint("H2", [TOK, D], BF16)
    XB = dint("XB", [NSB * 128, D], BF16)
    YB = dint("YB", [NSB * 128, D])
    XS1 = dint("XS1", [TOK, D])

    with ExitStack() as top:
        E = top.enter_context
        P = Prog(nc, top)

        uid = [0]

        def sb(name, shape, dt=F32, st=top):
            uid[0] += 1
            return st.enter_context(nc.sbuf_tensor("%s_u%d" % (name, uid[0]), shape, dt))

        def V(fn, r=(), w=()):
            return P.op('dve', fn, r, w)

        def A(fn, r=(), w=()):
            return P.op('act', fn, r, w)

        def G(fn, r=(), w=()):
            return P.op('pool', fn, r, w)

        def T(fn, r=(), w=()):
            return P.op('pe', fn, r, w)

        def LD(fn, r=(), w=(), is_out=False):
            return P.dma('sp', fn, r, w, is_out)

        def GD(fn, r=(), w=()):
            return P.dma('pool', fn, r, w)

        cs = sb("cs", [128, CW])
        m8 = sb("m8", [128, 256], U8)
        identb = sb("identb", [128, 128], BF16)
        trib = sb("trib", [128, 128], BF16)
        onesb = sb("onesb", [128, 128], BF16)
        colsA = sb("colsA", [128, 72])
        colsM = sb("colsM", [128, 192])
        scv = sb("scv", [128, 16])
        lbc = sb("lbc", [128, 2, 8])
        oml = sb("oml", [128, 2, 8])
        lbm1 = sb("lbm1", [128, 2, 8])
        a1c = sb("a1c", [128, 2, 2, 8])
        ERF = sb("ERF", [128, 4, NCH])
        ETRF = sb("ETRF", [128, 4, NCH])
        DECF = sb("DECF", [128, 4, NCH])
        M1all = sb("M1all", [128, NSUB, NE], BF16)
        M2all = sb("M2all", [128, NSUB, NE], BF16)
        W12 = sb("W12", [128, NSUB, 2])
        fn_bc = sb("fn_bc", [128, D])
        Sst = sb("Sst", [128, 4, 128])
        Sbf = sb("Sbf", [128, 4, 4, 128], BF16)
        scb = [sb("scb%d" % i, [128, 128], BF16) for i in range(2)]
        scf = [sb("scf%d" % i, [128, 128], BF16) for i in range(2)]

        ps = [E(nc.psum_tensor("ps%d" % i, [128, 512], F32)) for i in range(8)]

        ident = cs[:, 0:128]
        tri32 = cs[:, 128:256]
        mskrow = cs[:, 256:512]
        ones32 = cs[:, 512:640]
        nblk = cs[:, 640:640 + NB]
        pcol = cs[:, 760:761]
        maskF = m8[:, 0:128]
        maskB = m8[:, 128:256]

        open_stacks = []

        def newstack():
            stx = ExitStack()
            open_stacks.append(stx)
            return stx

        def stop(tag):
            if dbg == tag:
                P.barrier_all()
                raise _Stop()

        try:
            LD(lambda e: e.dma_start(out=cs[:], in_=cst), w=["cs"])
            LD(lambda e: e.dma_start(out=m8[:], in_=cst8), w=["m8"])
            V(lambda e: e.tensor_copy(identb[:], ident), r=["cs"], w=["identb"])
            V(lambda e: e.tensor_copy(trib[:], tri32), r=["cs"], w=["trib"])
            V(lambda e: e.tensor_copy(onesb[:], ones32), r=["cs"], w=["onesb"])
            LD(lambda e: e.dma_start(out=fn_bc[:], in_=fnorm.to_broadcast([128, D])), w=["fn_bc"])
            for i in range(2):
                V(lambda e, i=i: e.memset(scb[i][:], 0.0), w=["scb%d" % i])
                V(lambda e, i=i: e.memset(scf[i][:], 0.0), w=["scf%d" % i])

            s0 = newstack()
            rowsA = sb("rowsA", [72, 128], st=s0)
            LD(lambda e: e.dma_start(out=rowsA[:], in_=rows_in), w=["rowsA"])
            T(lambda e: e.transpose(out=ps[0][:, 0:72], in_=rowsA[:], identity=cs[0:72, 0:72]), r=["rowsA", "cs"], w=["ps0"])
            V(lambda e: e.tensor_copy(colsA[:], ps[0][:, 0:72]), r=["ps0"], w=["colsA"])
            A(lambda e: e.activation(out=scv[:], in_=colsA[:, 56:72], func=AF.Silu), r=["colsA"], w=["scv"])
            V(lambda e: e.memset(lbc[:], 0.0), w=["lbc"])
            V(lambda e: e.tensor_tensor(out=lbc[:, 1, :], in0=colsA[:, 40:48], in1=colsA[:, 32:40], op=ALU.subtract), r=["colsA"], w=["lbc"])
            A(lambda e: e.activation(out=lbc[:, 1, :], in_=lbc[:, 1, :], func=AF.Sigmoid), r=["lbc"], w=["lbc"])
            V(lambda e: e.tensor_scalar(out=oml[:], in0=lbc[:], scalar1=-1.0, scalar2=1.0, op0=ALU.mult, op1=ALU.add), r=["lbc"], w=["oml"])
            V(lambda e: e.tensor_scalar(out=lbm1[:], in0=lbc[:], scalar1=-1.0, scalar2=None, op0=ALU.add), r=["lbc"], w=["lbm1"])

            wmb = [sb("wmb%d" % i, [128, 8, 512], st=s0) for i in range(2)]
            bmod_sb = sb("bmod_sb", [2, 6 * D], st=s0)
            modsb = sb("modsb", [2, 6 * D], st=s0)
            it = 0
            for l in range(2):
                LD(lambda e, l=l: e.dma_start(out=bmod_sb[:], in_=b_mod[l:l + 1, :].to_broadcast([2, 6 * D])), w=["bmod_sb"])
                for n in range(12):
                    wb = wmb[it % 2]
                    wn = "wmb%d" % (it % 2)
                    LD(lambda e, l=l, n=n, wb=wb: e.dma_start(
                        out=wb[:], in_=w_mod[l, :, n * 512:(n + 1) * 512].rearrange("(kc p) f -> p kc f", p=128)), w=[wn])
                    pb = ps[it % 2]
                    pn = "ps%d" % (it % 2)
                    for kc in range(8):
                        T(lambda e, kc=kc, wb=wb, pb=pb: e.matmul(pb[0:2, :], lhsT=scv[:, kc:16:8], rhs=wb[:, kc, :],
                                                                 start=(kc == 0), stop=(kc == 7)), r=[wn, "scv"], w=[pn])
                    V(lambda e, n=n, pb=pb: e.tensor_tensor(out=modsb[:, n * 512:(n + 1) * 512], in0=pb[0:2, :],
                                                            in1=bmod_sb[:, n * 512:(n + 1) * 512], op=ALU.add),
                      r=[pn, "bmod_sb"], w=["modsb"])
                    it += 1
                LD(lambda e, l=l: e.dma_start(out=MODROW[l], in_=modsb[:]), r=["modsb"], w=["MODROW"])
            rowsM = sb("rowsM", [96, 2, 128], st=s0)
            MR = MODROW.rearrange("l s (r q) -> l (s r) q", q=128)
            for l in range(2):
                LD(lambda e, l=l: e.dma_start(out=rowsM[:, l, :], in_=MR[l]), r=["MODROW"], w=["rowsM"])
            for l in range(2):
                T(lambda e, l=l: e.transpose(out=ps[2][:, l * 96:(l + 1) * 96], in_=rowsM[:, l, :], identity=cs[0:96, 0:96]),
                  r=["rowsM", "cs"], w=["ps2"])
            V(lambda e: e.tensor_copy(colsM[:], ps[2][:, 0:192]), r=["ps2"], w=["colsM"])

            def cm(l, s, k):
                o = l * 96 + s * 48 + k * 8
                return colsM[:, o:o + 8]

            for l in range(2):
                for s in range(2):
                    V(lambda e, l=l, s=s: e.scalar_tensor_tensor(out=a1c[:, l, s, :], in0=cm(l, s, 1), scalar=1.0,
                                                                 in1=colsA[:, l * 16:l * 16 + 8], op0=ALU.add, op1=ALU.mult),
                      r=["colsM", "colsA"], w=["a1c"])
            P.barrier_all()
            s0.close(); open_stacks.remove(s0)
            stop("s0")

            for l in range(n_layers):
                last = (l == 1)
                XIN = xs if l == 0 else XS1
                sm = newstack()
                winb = sb("winb", [128, 8, INW], BF16, st=sm)
                woutb = sb("woutb", [128, 8, D], BF16, st=sm)
                wr32 = sb("wr32", [128, 8, 36], st=sm)
                brow = sb("brow", [1, 36], st=sm)
                wsT = sb("wsT", [128, 4, 128], BF16, st=sm)
                bsrow = sb("bsrow", [1, 512], BF16, st=sm)
                sgn_bc = sb("sgn_bc", [128, 512], st=sm)
                g1_bc = sb("g1_bc", [128, 2, D], st=sm)
                a2_bc = sb("a2_bc", [128, 2, D], st=sm)
                b2_bc = sb("b2_bc", [128, 2, D], st=sm)
                for kc in range(8):
                    for hf in range(2):
                        GD(lambda e, kc=kc, hf=hf: e.dma_start(out=winb[:, kc, hf * 1792:(hf + 1) * 1792],
                                                               in_=w_in[l, kc * 128:(kc + 1) * 128, hf * 1792:(hf + 1) * 1792]),
                           w=["winb"])
                    GD(lambda e, kc=kc: e.dma_start(out=woutb[:, kc, :], in_=w_out[l, kc * 128:(kc + 1) * 128, :]), w=["woutb"])
                LD(lambda e: e.dma_start(out=wr32[:], in_=wrt[l].rearrange("(kc p) f -> p kc f", p=128)), w=["wr32"])
                LD(lambda e: e.dma_start(out=brow[:], in_=brt[l:l + 1, :]), w=["brow"])
                GD(lambda e: e.dma_start(out=bsrow[:], in_=sgb[l:l + 1, :]), w=["bsrow"])
                LD(lambda e: e.dma_start(out=sgn_bc[:], in_=sgn[l:l + 1, :].to_broadcast([128, 512])), w=["sgn_bc"])
                sl = newstack()
                wsn = sb("wsn", [128, 4, 128], st=sl)
                LD(lambda e: e.dma_start(out=wsn[:], in_=sgw[l].rearrange("h i j -> i h j")), w=["wsn"])
                for h in range(4):
                    T(lambda e, h=h: e.transpose(out=ps[0][:, h * 128:(h + 1) * 128], in_=wsn[:, h, :], identity=ident),
                      r=["wsn", "cs"], w=["ps0"])
                V(lambda e: e.tensor_copy(wsT[:].rearrange("p h i -> p (h i)"), ps[0][:]), r=["ps0"], w=["wsT"])
                tmpb = sb("tmpb", [128, D], st=sl)
                for s in range(2):
                    LD(lambda e, s=s: e.dma_start(out=g1_bc[:, s, :], in_=MODROW[l, s:s + 1, 2 * D:3 * D].to_broadcast([128, D])), w=["g1_bc"])
                    LD(lambda e, s=s: e.dma_start(out=b2_bc[:, s, :], in_=MODROW[l, s:s + 1, 3 * D:4 * D].to_broadcast([128, D])), w=["b2_bc"])
                    LD(lambda e, s=s: e.dma_start(out=a2_bc[:, s, :], in_=MODROW[l, s:s + 1, 4 * D:5 * D].to_broadcast([128, D])), w=["a2_bc"])
                LD(lambda e: e.dma_start(out=tmpb[:], in_=n2row[l:l + 1, :].to_broadcast([128, D])), w=["tmpb"])
                for s in range(2):
                    V(lambda e, s=s: e.scalar_tensor_tensor(out=a2_bc[:, s, :], in0=a2_bc[:, s, :], scalar=1.0, in1=tmpb[:],
                                                            op0=ALU.add, op1=ALU.mult), r=["a2_bc", "tmpb"], w=["a2_bc"])
                P.barrier_all()
                sl.close(); open_stacks.remove(sl)
                stop("l0")

                xt = [sb("xt%d" % i, [128, 2, D], st=sm) for i in range(2)]
                hT = sb("hT", [128, 8, NT], BF16, st=sm)
                q32 = sb("q32", [128, 4, NT], st=sm)
                sgt = [sb("sgt%d" % i, [128, NT], st=sm) for i in range(2)]
                lft = [sb("lft%d" % i, [128, NT], st=sm) for i in range(2)]
                kkt = [sb("kkt%d" % i, [128, NT], st=sm) for i in range(2)]
                Att = [sb("Att%d" % i, [128, NT], st=sm) for i in range(2)]
                e1t = [sb("e1t%d" % i, [128, NT], st=sm) for i in range(2)]
                qtf = sb("qtf", [128, 4, NT], BF16, st=sm)
                ktf = sb("ktf", [128, 4, NT], BF16, st=sm)
                qtb = sb("qtb", [128, 4, NT], BF16, st=sm)
                ktb = sb("ktb", [128, 4, NT], BF16, st=sm)
                gsb = sb("gsb", [128, 4, NT], BF16, st=sm)
                ug = sb("ug", [128, 4, NT], BF16, st=sm)
                mixT = sb("mixT", [128, 8, NT], BF16, st=sm)
                vtok = sb("vtok", [128, 2, 512], BF16, st=sm)
                vg = sb("vg", [128, 512], st=sm)
                vntok = sb("vntok", [128, 2, 512], BF16, st=sm)
                kbtok = sb("kbtok", [128, 2, 512], BF16, st=sm)
                kftok = sb("kftok", [128, 2, 512], BF16, st=sm)
                obf = sb("obf", [128, 4, NT], st=sm)
                ofull = obf
                sqt = sb("sqt", [128, NT], st=sm)
                rt = sb("rt", [128, NT], st=sm)
                mt = sb("mt", [128, NT], st=sm)
                stat = sb("stat", [128, 8], st=sm)
                erb = sb("erb", [128, 4, 4], st=sm)
                etrb = sb("etrb", [128, 4, 4], st=sm)
                decb = sb("decb", [128, 4, 4], st=sm)
                dtmp = sb("dtmp", [128, 4, 4], st=sm)
                utmp = sb("utmp", [128, 4, 128], st=sm)
                tmp2 = sb("tmp2", [128, 512], st=sm)
                h2t = sb("h2t", [128, D], st=sm)
                junk = h2t
                h2T = sb("h2T", [128, 8, 128], st=sm)
                Ls = sb("Ls", [128, 36], st=sm)
                rsm = sb("rsm", [128, 64], st=sm)

                OBFv = OBF.rearrange("(h p) t -> p h t", p=128)
                QFv = QF.rearrange("(h p) t -> p h t", p=128)
                GSv = GS.rearrange("(h p) t -> p h t", p=128)
                SGv = SGD.rearrange("(h p) t -> p h t", p=128)
                KFTv = KFT.rearrange("(n p) f -> p n f", p=128)
                VTv = VT.rearrange("(n p) f -> p n f", p=128)
                XINv = XIN.rearrange("(n p) f -> p n f", p=128)
                XMv = XM.rearrange("(n p) f -> p n f", p=128)
                H2v = H2.rearrange("(n p) f -> p n f", p=128)

                def rstd_from_ss(ssap, rap, n, inv):
                    V(lambda e: e.tensor_scalar(out=rap, in0=ssap, scalar1=inv, scalar2=EPS, op0=ALU.mult, op1=ALU.add),
                      r=["stat"], w=["stat"])
                    A(lambda e: e.activation(out=rap, in_=rap, func=AF.Sqrt), r=["stat"], w=["stat"])
                    V(lambda e: e.reciprocal(out=rap, in_=rap), r=["stat"], w=["stat"])

                def load_x(ti, bi):
                    LD(lambda e: e.dma_start(out=xt[bi][:], in_=XINv[:, 2 * ti:2 * ti + 2, :]), r=["XIN%d" % ti], w=["xt%d" % bi])

                def norm_to_hT(ti, bi, s):
                    x = xt[bi]
                    xn = "xt%d" % bi
                    for j in range(2):
                        A(lambda e, j=j: e.activation(out=junk[:], in_=x[:, j, :], func=AF.Square, accum_out=stat[:, j:j + 1]),
                          r=[xn], w=["h2t", "stat"])
                    rstd_from_ss(stat[:, 0:2], stat[:, 2:4], 2, 1.0 / D)
                    for j in range(2):
                        V(lambda e, j=j: e.tensor_scalar(out=x[:, j, :], in0=x[:, j, :], scalar1=stat[:, 2 + j:3 + j], scalar2=None,
                                                         op0=ALU.mult), r=[xn, "stat"], w=[xn])
                    for c in range(8):
                        pb = ps[2 + (c % 2)]
                        pn = "ps%d" % (2 + (c % 2))
                        for j in range(2):
                            T(lambda e, c=c, j=j, pb=pb: e.transpose(out=pb[:, j * 128:(j + 1) * 128], in_=x[:, j, c * 128:(c + 1) * 128],
                                                                     identity=ident), r=[xn, "cs"], w=[pn])
                        if c % 2 == 0:
                            V(lambda e, c=c, pb=pb: e.tensor_scalar(out=hT[:, c, :], in0=pb[:, 0:NT], scalar1=a1c[:, l, s, c:c + 1],
                                                                    scalar2=cm(l, s, 0)[:, c:c + 1], op0=ALU.mult, op1=ALU.add),
                              r=[pn, "a1c", "colsM"], w=["hT"])
                        else:
                            A(lambda e, c=c, pb=pb: e.activation(out=hT[:, c, :], in_=pb[:, 0:NT], func=AF.Identity,
                                                                 bias=cm(l, s, 0)[:, c:c + 1], scale=a1c[:, l, s, c:c + 1]),
                              r=[pn, "a1c", "colsM"], w=["hT"])

                pj = [0]

                def proj_fm(m):
                    k = pj[0] % 2
                    pj[0] += 1
                    pb = ps[k]
                    for c in range(8):
                        T(lambda e, c=c, pb=pb: e.matmul(pb[:, 0:NT], lhsT=winb[:, c, m * 128:(m + 1) * 128], rhs=hT[:, c, :],
                                                         start=(c == 0), stop=(c == 7)), r=["winb", "hT"], w=["ps%d" % k])
                    return pb, "ps%d" % k

                def proj_tm(j, col0):
                    k = pj[0] % 2
                    pj[0] += 1
                    pb = ps[k]
                    for c in range(8):
                        T(lambda e, c=c, pb=pb: e.matmul(pb[:, :], lhsT=hT[:, c, j * 128:(j + 1) * 128], rhs=winb[:, c, col0:col0 + 512],
                                                         start=(c == 0), stop=(c == 7)), r=["winb", "hT"], w=["ps%d" % k])
                    return pb, "ps%d" % k

                gi = [0]

                def gates(h, dr, ti):
                    k = gi[0] % 2
                    gi[0] += 1
                    pb, pn = proj_fm(4 + 4 * dr + h)
                    sg_, lf_, kk_, I_, A_, e1_, e2_ = sgt[k], lft[k], kkt[k], sgt[k], Att[k], e1t[k], Att[k]
                    nm = ["sgt%d" % k, "lft%d" % k, "kkt%d" % k, "sgt%d" % k, "Att%d" % k, "e1t%d" % k, "Att%d" % k]
                    ci = dr * 4 + h
                    A(lambda e: e.activation(out=sg_[:], in_=pb[:, 0:NT], func=AF.Sigmoid), r=[pn], w=[nm[0]])
                    A(lambda e: e.activation(out=lf_[:], in_=sg_[:], func=AF.Ln, bias=lbc[:, l, ci:ci + 1], scale=oml[:, l, ci:ci + 1]),
                      r=[nm[0], "lbc", "oml"], w=[nm[1]])
                    V(lambda e: e.tensor_scalar(out=kk_[:], in0=sg_[:], scalar1=-1.0, scalar2=lbm1[:, l, ci:ci + 1], op0=ALU.add, op1=ALU.mult),
                      r=[nm[0], "lbm1"], w=[nm[2]])
                    V(lambda e: e.tensor_tensor_scan(out=I_[:], data0=mskrow, data1=lf_[:], initial=0.0, op0=ALU.mult, op1=ALU.add),
                      r=[nm[1], "cs"], w=[nm[3]])
                    I3 = I_[:].rearrange("p (c t) -> p c t", t=64)
                    A3 = A_[:].rearrange("p (c t) -> p c t", t=64)
                    if dr == 0:
                        V(lambda e: e.tensor_tensor(out=A3, in0=I3, in1=I3[:, :, 31:32].to_broadcast([128, 4, 64]), op=ALU.subtract),
                          r=[nm[3]], w=[nm[4]])
                        A(lambda e: e.activation(out=e1_[:], in_=A_[:], func=AF.Exp), r=[nm[4]], w=[nm[5]])
                        A(lambda e: e.activation(out=e2_[:], in_=A_[:], func=AF.Exp, scale=-1.0), r=[nm[4]], w=[nm[6]])
                        G(lambda e: e.tensor_tensor(out=qtf[:, h, :], in0=q32[:, h, :], in1=e1_[:], op=ALU.mult), r=["q32", nm[5]], w=["qtf"])
                        G(lambda e: e.tensor_tensor(out=ktf[:, h, :], in0=kk_[:], in1=e2_[:], op=ALU.mult), r=[nm[2], nm[6]], w=["ktf"])
                        gc = ti * 4
                        A(lambda e: e.activation(out=ERF[:, h, gc:gc + 4], in_=I3[:, :, 31], func=AF.Exp), r=[nm[3]], w=["ERF"])
                        A(lambda e: e.activation(out=DECF[:, h, gc:gc + 4], in_=I3[:, :, 63], func=AF.Exp), r=[nm[3]], w=["DECF"])
                        V(lambda e: e.tensor_tensor(out=dtmp[:, h, :], in0=I3[:, :, 63], in1=I3[:, :, 31], op=ALU.subtract), r=[nm[3]], w=["dtmp"])
                        A(lambda e: e.activation(out=ETRF[:, h, gc:gc + 4], in_=dtmp[:, h, :], func=AF.Exp), r=["dtmp"], w=["ETRF"])
                    else:
                        V(lambda e: e.tensor_tensor(out=lf_[:], in0=I_[:], in1=lf_[:], op=ALU.subtract), r=[nm[3], nm[1]], w=[nm[1]])
                        E3 = lf_[:].rearrange("p (c t) -> p c t", t=64)
                        V(lambda e: e.tensor_tensor(out=A3, in0=E3, in1=E3[:, :, 32:33].to_broadcast([128, 4, 64]), op=ALU.subtract),
                          r=[nm[1]], w=[nm[4]])
                        A(lambda e: e.activation(out=e1_[:], in_=A_[:], func=AF.Exp, scale=-1.0), r=[nm[4]], w=[nm[5]])
                        A(lambda e: e.activation(out=e2_[:], in_=A_[:], func=AF.Exp), r=[nm[4]], w=[nm[6]])
                        G(lambda e: e.tensor_tensor(out=qtb[:, h, :], in0=q32[:, h, :], in1=e1_[:], op=ALU.mult), r=["q32", nm[5]], w=["qtb"])
                        G(lambda e: e.tensor_tensor(out=ktb[:, h, :], in0=kk_[:], in1=e2_[:], op=ALU.mult), r=[nm[2], nm[6]], w=["ktb"])
                        A(lambda e: e.activation(out=etrb[:, h, :], in_=E3[:, :, 32], func=AF.Exp), r=[nm[1]], w=["etrb"])
                        A(lambda e: e.activation(out=decb[:, h, :], in_=I3[:, :, 63], func=AF.Exp), r=[nm[3]], w=["decb"])
                        V(lambda e: e.tensor_tensor(out=dtmp[:, h, :], in0=I3[:, :, 63], in1=E3[:, :, 32], op=ALU.subtract), r=[nm[3], nm[1]], w=["dtmp"])
                        A(lambda e: e.activation(out=erb[:, h, :], in_=dtmp[:, h, :], func=AF.Exp), r=["dtmp"], w=["erb"])

                def state_step(c, j, cc, ktok, ktn, er_ap, etr_ap, dec_ap, kU, scn):
                    pb = ps[4 + kU % 2]
                    pn = "ps%d" % (4 + kU % 2)
                    for h in range(4):
                        T(lambda e, h=h, pb=pb: e.matmul(pb[:, h * 128:(h + 1) * 128],
                                                         lhsT=ktok[cc * 64:(cc + 1) * 64, j, h * 128:(h + 1) * 128],
                                                         rhs=vtok[cc * 64:(cc + 1) * 64, j, h * 128:(h + 1) * 128], start=True, stop=True),
                          r=[ktn, "vtok"], w=[pn])
                    V(lambda e: e.tensor_tensor(out=Sbf[:, c, :, :], in0=Sst[:], in1=er_ap.to_broadcast([128, 4, 128]), op=ALU.mult),
                      r=["Sst"] + scn, w=["Sbf%d" % c])
                    V(lambda e, pb=pb: e.tensor_tensor(out=utmp[:], in0=pb[:].rearrange("p (h e) -> p h e", e=128),
                                                       in1=etr_ap.to_broadcast([128, 4, 128]), op=ALU.mult), r=[pn] + scn, w=["utmp"])
                    V(lambda e: e.tensor_tensor(out=Sst[:], in0=Sst[:], in1=dec_ap.to_broadcast([128, 4, 128]), op=ALU.mult),
                      r=["Sst"] + scn, w=["Sst"])
                    V(lambda e: e.tensor_tensor(out=Sst[:], in0=Sst[:], in1=utmp[:], op=ALU.add), r=["Sst", "utmp"], w=["Sst"])

                ku = [0]

                V(lambda e: e.memset(Sst[:], 0.0), w=["Sst"])
                order1 = [0] + list(range(NTILES - 1, 0, -1))
                load_x(order1[0], 0)
                for oi, ti in enumerate(order1):
                    bi = oi % 2
                    s = 1 if ti == 0 else 0
                    if oi + 1 < len(order1):
                        load_x(order1[oi + 1], 1 - bi)
                    norm_to_hT(ti, bi, s)
                    stop("ta")
                    for j in range(2):
                        pb, pn = proj_tm(j, 1536)
                        V(lambda e, j=j, pb=pb: e.tensor_copy(vtok[:, j, :], pb[:, :]), r=[pn], w=["vtok"])
                        pb, pn = proj_tm(j, 3072)
                        A(lambda e, pb=pb: e.activation(out=vg[:], in_=pb[:, :], func=AF.Gelu_apprx_tanh), r=[pn], w=["vg"])
                        A(lambda e, j=j: e.activation(out=junk[:, 0:512], in_=vg[:], func=AF.Square, accum_out=stat[:, 4 + j:5 + j]),
                          r=["vg"], w=["h2t", "stat"])
                        rstd_from_ss(stat[:, 4 + j:5 + j], stat[:, 6 + j:7 + j], 1, 1.0 / 512)
                        V(lambda e, j=j: e.scalar_tensor_tensor(out=vntok[:, j, :], in0=vg[:], scalar=stat[:, 6 + j:7 + j], in1=sgn_bc[:],
                                                                op0=ALU.mult, op1=ALU.mult), r=["vg", "stat", "sgn_bc"], w=["vntok"])
                    stop("tb")
                    for h in range(4):
                        pb, pn = proj_fm(h)
                        V(lambda e, h=h, pb=pb: e.tensor_copy(q32[:, h, :], pb[:, 0:NT]), r=[pn], w=["q32"])
                    for h in range(4):
                        gates(h, 0, ti)
                        gates(h, 1, ti)
                    for h in range(4):
                        pb, pn = proj_fm(16 + h)
                        A(lambda e, h=h, pb=pb: e.activation(out=gsb[:, h, :], in_=pb[:, 0:NT], func=AF.Silu), r=[pn], w=["gsb"])
                        pb, pn = proj_fm(20 + h)
                        A(lambda e, h=h, pb=pb: e.activation(out=ug[:, h, :], in_=pb[:, 0:NT], func=AF.Gelu_apprx_tanh), r=[pn], w=["ug"])
                    stop("tc")
                    for j in range(2):
                        pb = ps[2 + j]
                        pn = "ps%d" % (2 + j)
                        for h in range(4):
                            T(lambda e, h=h, j=j, pb=pb: e.matmul(pb[:, h * 128:(h + 1) * 128], lhsT=vntok[:, j, h * 128:(h + 1) * 128],
                                                                  rhs=wsT[:, h, :], start=True, stop=False), r=["vntok", "wsT"], w=[pn])
                            T(lambda e, h=h, pb=pb: e.matmul(pb[:, h * 128:(h + 1) * 128], lhsT=onesb[0:1, :],
                                                             rhs=bsrow[0:1, h * 128:(h + 1) * 128], start=False, stop=True),
                              r=["onesb", "bsrow"], w=[pn])
                        V(lambda e, j=j, pb=pb: e.tensor_tensor(out=mixT[:, 4:8, j * 128:(j + 1) * 128],
                                                                in0=pb[:].rearrange("p (h i) -> p h i", i=128),
                                                                in1=ug[:, :, j * 128:(j + 1) * 128], op=ALU.mult), r=[pn, "ug"], w=["mixT"])
                    stop("td")
                    for j in range(2):
                        for h in range(4):
                            T(lambda e, h=h, j=j: e.matmul(ps[2][:, h * 128:(h + 1) * 128], lhsT=ktb[:, h, j * 128:(j + 1) * 128],
                                                           rhs=identb[:], start=True, stop=True), r=["ktb", "identb"], w=["ps2"])
                            T(lambda e, h=h, j=j: e.matmul(ps[3][:, h * 128:(h + 1) * 128], lhsT=ktf[:, h, j * 128:(j + 1) * 128],
                                                           rhs=identb[:], start=True, stop=True), r=["ktf", "identb"], w=["ps3"])
                        V(lambda e, j=j: e.tensor_copy(kbtok[:, j, :], ps[2][:, :]), r=["ps2"], w=["kbtok"])
                        V(lambda e, j=j: e.tensor_copy(kftok[:, j, :], ps[3][:, :]), r=["ps3"], w=["kftok"])
                    stop("te")
                    for c in (3, 2, 1, 0):
                        j, cc = c // 2, c % 2
                        state_step(c, j, cc, kbtok, "kbtok", erb[:, :, c:c + 1], etrb[:, :, c:c + 1], decb[:, :, c:c + 1], ku[0], ["erb", "etrb", "decb"])
                        ku[0] += 1
                    stop("tf")
                    for j in (1, 0):
                        for h in range(4):
                            k = h % 2
                            off = k * 256
                            jc = slice(j * 128, (j + 1) * 128)
                            T(lambda e, h=h, jc=jc, off=off: e.matmul(ps[6][:, off:off + 128], lhsT=ktb[:, h, jc], rhs=qtb[:, h, jc],
                                                                      start=True, stop=True), r=["ktb", "qtb"], w=["ps6_%d" % k])
                            T(lambda e, h=h, jc=jc, off=off: e.matmul(ps[6][:, off + 128:off + 256], lhsT=ktf[:, h, jc], rhs=qtf[:, h, jc],
                                                                      start=True, stop=True), r=["ktf", "qtf"], w=["ps6_%d" % k])
                            V(lambda e, k=k, off=off: e.copy_predicated(out=scb[k][:], mask=maskB, data=ps[6][:, off:off + 128]),
                              r=["ps6_%d" % k, "m8"], w=["scb%d" % k])
                            V(lambda e, k=k, off=off: e.copy_predicated(out=scf[k][:], mask=maskF, data=ps[6][:, off + 128:off + 256]),
                              r=["ps6_%d" % k, "m8"], w=["scf%d" % k])
                            oo = ps[5][:, h * 128:(h + 1) * 128]
                            T(lambda e, h=h, j=j, k=k, oo=oo: e.matmul(oo, lhsT=vtok[:, j, h * 128:(h + 1) * 128], rhs=scb[k][:],
                                                                       start=True, stop=False), r=["vtok", "scb%d" % k], w=["ps5"])
                            T(lambda e, h=h, j=j, k=k, oo=oo: e.matmul(oo, lhsT=vtok[:, j, h * 128:(h + 1) * 128], rhs=scf[k][:],
                                                                       start=False, stop=False), r=["vtok", "scf%d" % k], w=["ps5"])
                            for cc in (1, 0):
                                c = 2 * j + cc
                                T(lambda e, h=h, c=c, cc=cc, j=j: e.matmul(ps[5][:, h * 128 + cc * 64:h * 128 + (cc + 1) * 64],
                                                                           lhsT=Sbf[:, c, h, :],
                                                                           rhs=qtb[:, h, j * 128 + cc * 64:j * 128 + (cc + 1) * 64],
                                                                           start=False, stop=(cc == 0)), r=["Sbf%d" % c, "qtb"], w=["ps5"])
                        V(lambda e, j=j: e.tensor_copy(obf[:, :, j * 128:(j + 1) * 128], ps[5][:].rearrange("p (h i) -> p h i", i=128)), r=["ps5"], w=["obf"])
                    stop("tg")
                    t0 = ti * NT
                    LD(lambda e, t0=t0: e.dma_start(out=OBFv[:, :, t0:t0 + NT], in_=obf[:]), r=["obf"], w=["OBF%d" % ti])
                    LD(lambda e, t0=t0: e.dma_start(out=QFv[:, :, t0:t0 + NT], in_=qtf[:]), r=["qtf"], w=["QF%d" % ti])
                    LD(lambda e, t0=t0: e.dma_start(out=GSv[:, :, t0:t0 + NT], in_=gsb[:]), r=["gsb"], w=["GS%d" % ti])
                    LD(lambda e, t0=t0: e.dma_start(out=SGv[:, :, t0:t0 + NT], in_=mixT[:, 4:8, :]), r=["mixT"], w=["SG%d" % ti])
                    LD(lambda e, ti=ti: e.dma_start(out=KFTv[:, 2 * ti:2 * ti + 2, :], in_=kftok[:]), r=["kftok"], w=["KFT%d" % ti])
                    LD(lambda e, ti=ti: e.dma_start(out=VTv[:, 2 * ti:2 * ti + 2, :], in_=vtok[:]), r=["vtok"], w=["VT%d" % ti])
                    stop("p1a")

                stop("p1")
                V(lambda e: e.memset(Sst[:], 0.0), w=["Sst"])

                def load2(ti, bi):
                    t0 = ti * NT
                    load_x(ti, bi)

                load_x(0, 0)
                for ti in range(NTILES):
                    bi = ti % 2
                    s = 1 if ti == 0 else 0
                    t0 = ti * NT
                    x = xt[bi]
                    xn = "xt%d" % bi
                    LD(lambda e, t0=t0: e.dma_start(out=obf[:], in_=OBFv[:, :, t0:t0 + NT]), r=["OBF%d" % ti], w=["obf"])
                    LD(lambda e, t0=t0: e.dma_start(out=qtf[:], in_=QFv[:, :, t0:t0 + NT]), r=["QF%d" % ti], w=["qtf"])
                    LD(lambda e, t0=t0: e.dma_start(out=gsb[:], in_=GSv[:, :, t0:t0 + NT]), r=["GS%d" % ti], w=["gsb"])
                    LD(lambda e, t0=t0: e.dma_start(out=mixT[:, 4:8, :], in_=SGv[:, :, t0:t0 + NT]), r=["SG%d" % ti], w=["mixT"])
                    LD(lambda e, ti=ti: e.dma_start(out=kftok[:], in_=KFTv[:, 2 * ti:2 * ti + 2, :]), r=["KFT%d" % ti], w=["kftok"])
                    LD(lambda e, ti=ti: e.dma_start(out=vtok[:], in_=VTv[:, 2 * ti:2 * ti + 2, :]), r=["VT%d" % ti], w=["vtok"])
                    if ti + 1 < NTILES:
                        load_x(ti + 1, 1 - bi)
                    gc = ti * 4
                    for c in range(4):
                        j, cc = c // 2, c % 2
                        state_step(c, j, cc, kftok, "kftok", ERF[:, :, gc + c:gc + c + 1], ETRF[:, :, gc + c:gc + c + 1],
                                   DECF[:, :, gc + c:gc + c + 1], ku[0], ["ERF", "ETRF", "DECF"])
                        ku[0] += 1
                    for j in range(2):
                        for h in range(4):
                            for cc in range(2):
                                c = 2 * j + cc
                                T(lambda e, h=h, c=c, cc=cc, j=j: e.matmul(ps[5][:, h * 128 + cc * 64:h * 128 + (cc + 1) * 64],
                                                                           lhsT=Sbf[:, c, h, :],
                                                                           rhs=qtf[:, h, j * 128 + cc * 64:j * 128 + (cc + 1) * 64],
                                                                           start=True, stop=True), r=["Sbf%d" % c, "qtf"], w=["ps5"])
                        V(lambda e, j=j: e.tensor_tensor(out=ofull[:, :, j * 128:(j + 1) * 128], in0=ps[5][:].rearrange("p (h i) -> p h i", i=128),
                                                         in1=obf[:, :, j * 128:(j + 1) * 128], op=ALU.add), r=["ps5", "obf"], w=["obf"])
                    for h in range(4):
                        A(lambda e, h=h: e.activation(out=sqt[:], in_=ofull[:, h, :], func=AF.Square), r=["obf"], w=["sqt"])
                        T(lambda e: e.matmul(ps[6][:, 0:NT], lhsT=ones32, rhs=sqt[:], start=True, stop=True), r=["sqt", "cs"], w=["ps6_0", "ps6_1"])
                        V(lambda e: e.tensor_scalar(out=rt[:], in0=ps[6][:, 0:NT], scalar1=1.0 / 128, scalar2=EPS, op0=ALU.mult, op1=ALU.add),
                          r=["ps6_0", "ps6_1"], w=["rt"])
                        A(lambda e: e.activation(out=rt[:], in_=rt[:], func=AF.Sqrt), r=["rt"], w=["rt"])
                        V(lambda e: e.reciprocal(out=rt[:], in_=rt[:]), r=["rt"], w=["rt"])
                        V(lambda e, h=h: e.scalar_tensor_tensor(out=mt[:], in0=ofull[:, h, :], scalar=colsA[:, 48 + l * 4 + h:49 + l * 4 + h],
                                                                in1=rt[:], op0=ALU.mult, op1=ALU.mult), r=["obf", "rt", "colsA"], w=["mt"])
                        G(lambda e, h=h: e.tensor_tensor(out=mixT[:, h, :], in0=mt[:], in1=gsb[:, h, :], op=ALU.mult), r=["mt", "gsb"], w=["mixT"])
                    for j in range(2):
                        for hf in range(2):
                            k = pj[0] % 2
                            pj[0] += 1
                            pb = ps[k]
                            pn = "ps%d" % k
                            for c in range(8):
                                T(lambda e, c=c, j=j, hf=hf, pb=pb: e.matmul(pb[:, :], lhsT=mixT[:, c, j * 128:(j + 1) * 128],
                                                                             rhs=woutb[:, c, hf * 512:(hf + 1) * 512],
                                                                             start=(c == 0), stop=(c == 7)), r=["mixT", "woutb"], w=[pn])
                            V(lambda e, hf=hf, pb=pb: e.tensor_tensor(out=tmp2[:], in0=pb[:, :], in1=g1_bc[:, s, hf * 512:(hf + 1) * 512],
                                                                      op=ALU.mult), r=[pn, "g1_bc"], w=["tmp2"])
                            V(lambda e, j=j, hf=hf: e.tensor_tensor(out=x[:, j, hf * 512:(hf + 1) * 512], in0=tmp2[:],
                                                                    in1=x[:, j, hf * 512:(hf + 1) * 512], op=ALU.add), r=["tmp2", xn], w=[xn])
                    LD(lambda e, ti=ti: e.dma_start(out=XMv[:, 2 * ti:2 * ti + 2, :], in_=x[:]), r=[xn], w=["XM%d" % ti])
                    for j in range(2):
                        A(lambda e, j=j: e.activation(out=junk[:], in_=x[:, j, :], func=AF.Square, accum_out=stat[:, j:j + 1]),
                          r=[xn], w=["h2t", "stat"])
                    rstd_from_ss(stat[:, 0:2], stat[:, 2:4], 2, 1.0 / D)
                    for j in range(2):
                        sub = 2 * ti + j
                        V(lambda e, j=j: e.scalar_tensor_tensor(out=h2t[:], in0=x[:, j, :], scalar=stat[:, 2 + j:3 + j], in1=a2_bc[:, s, :],
                                                                op0=ALU.mult, op1=ALU.mult), r=[xn, "stat", "a2_bc"], w=["h2t"])
                        G(lambda e: e.tensor_tensor(out=h2t[:], in0=h2t[:], in1=b2_bc[:, s, :], op=ALU.add), r=["h2t", "b2_bc"], w=["h2t"])
                        GD(lambda e, sub=sub: e.dma_start(out=H2v[:, sub, :], in_=h2t[:]), r=["h2t"], w=["H2_%d" % sub])
                        for c in range(8):
                            pb = ps[2 + c // 4]
                            pn = "ps%d" % (2 + c // 4)
                            T(lambda e, c=c, pb=pb: e.transpose(out=pb[:, (c % 4) * 128:(c % 4 + 1) * 128], in_=h2t[:, c * 128:(c + 1) * 128],
                                                                identity=ident), r=["h2t", "cs"], w=[pn])
                        A(lambda e: e.activation(out=h2T[:, 0:4, :], in_=ps[2][:].rearrange("p (c t) -> p c t", t=128), func=AF.Copy),
                          r=["ps2"], w=["h2T"])
                        A(lambda e: e.activation(out=h2T[:, 4:8, :], in_=ps[3][:].rearrange("p (c t) -> p c t", t=128), func=AF.Copy),
                          r=["ps3"], w=["h2T"])
                        for c in range(8):
                            T(lambda e, c=c: e.matmul(ps[4][:, 0:36], lhsT=h2T[:, c, :], rhs=wr32[:, c, :], start=(c == 0), stop=False),
                              r=["h2T", "wr32"], w=["ps4"])
                        T(lambda e: e.matmul(ps[4][:, 0:36], lhsT=ones32[0:1, :], rhs=brow[0:1, :], start=False, stop=True),
                          r=["cs", "brow"], w=["ps4"])
                        V(lambda e: e.tensor_copy(Ls[:], ps[4][:, 0:36]), r=["ps4"], w=["Ls"])
                        R_ = ["rsm"]
                        V(lambda e: e.tensor_reduce(out=rsm[:, 0:1], in_=Ls[:, 0:4], axis=AX.X, op=ALU.max), r=["Ls"], w=R_)
                        V(lambda e: e.tensor_scalar(out=rsm[:, 1:2], in0=rsm[:, 0:1], scalar1=-1.0, scalar2=None, op0=ALU.mult), r=R_, w=R_)
                        A(lambda e: e.activation(out=rsm[:, 48:52], in_=Ls[:, 0:4], func=AF.Exp, bias=rsm[:, 1:2], accum_out=rsm[:, 2:3]),
                          r=["Ls"] + R_, w=R_)
                        V(lambda e: e.reciprocal(out=rsm[:, 3:4], in_=rsm[:, 2:3]), r=R_, w=R_)
                        V(lambda e: e.tensor_scalar(out=rsm[:, 4:8], in0=Ls[:, 0:4], scalar1=rsm[:, 0:1], scalar2=None, op0=ALU.is_equal),
                          r=["Ls"] + R_, w=R_)
                        V(lambda e: e.tensor_scalar(out=rsm[:, 8:16], in0=Ls[:, 4:12], scalar1=rsm[:, 4:5], scalar2=None, op0=ALU.mult),
                          r=["Ls"] + R_, w=R_)
                        for g in range(1, 4):
                            V(lambda e, g=g: e.scalar_tensor_tensor(out=rsm[:, 8:16], in0=Ls[:, 4 + 8 * g:12 + 8 * g], scalar=rsm[:, 4 + g:5 + g],
                                                                    in1=rsm[:, 8:16], op0=ALU.mult, op1=ALU.add), r=["Ls"] + R_, w=R_)
                        V(lambda e: e.max(out=rsm[:, 16:24], in_=rsm[:, 8:16]), r=R_, w=R_)
                        V(lambda e: e.tensor_scalar(out=rsm[:, 24:32], in0=rsm[:, 8:16], scalar1=rsm[:, 16:17], scalar2=None, op0=ALU.is_equal), r=R_, w=R_)
                        V(lambda e: e.tensor_scalar(out=rsm[:, 32:40], in0=rsm[:, 8:16], scalar1=rsm[:, 17:18], scalar2=None, op0=ALU.is_equal), r=R_, w=R_)
                        V(lambda e: e.tensor_tensor(out=rsm[:, 40:41], in0=rsm[:, 16:17], in1=rsm[:, 17:18], op=ALU.subtract), r=R_, w=R_)
                        A(lambda e: e.activation(out=rsm[:, 41:42], in_=rsm[:, 40:41], func=AF.Sigmoid), r=R_, w=R_)
                        V(lambda e, sub=sub: e.tensor_tensor(out=W12[:, sub, 0:1], in0=rsm[:, 41:42], in1=rsm[:, 3:4], op=ALU.mult), r=R_, w=["W12"])
                        V(lambda e, sub=sub: e.tensor_tensor(out=W12[:, sub, 1:2], in0=rsm[:, 3:4], in1=W12[:, sub, 0:1], op=ALU.subtract),
                          r=R_ + ["W12"], w=["W12"])
                        for g in range(4):
                            V(lambda e, g=g, sub=sub: e.tensor_scalar(out=M1all[:, sub, g * 8:(g + 1) * 8], in0=rsm[:, 24:32],
                                                                      scalar1=rsm[:, 4 + g:5 + g], scalar2=None, op0=ALU.mult), r=R_, w=["M1all"])
                            V(lambda e, g=g, sub=sub: e.tensor_scalar(out=M2all[:, sub, g * 8:(g + 1) * 8], in0=rsm[:, 32:40],
                                                                      scalar1=rsm[:, 4 + g:5 + g], scalar2=None, op0=ALU.mult), r=R_, w=["M2all"])
                P.barrier_all()
                sm.close(); open_stacks.remove(sm)

                stop("p2")
                se = newstack()
                pre = sb("pre", [128, NE], st=se)
                cnt_i = sb("cnt_i", [128, NE], I32, st=se)
                padf = sb("padf", [128, NE], st=se)
                incl = sb("incl", [128, NE], st=se)
                offs = sb("offs", [128, NE], st=se)
                cmp3 = sb("cmp3", [128, NB, NE], st=se)
                bef = sb("bef", [128, NB], st=se)
                tmpr = sb("tmpr", [128, NE], st=se)
                tmpr2 = sb("tmpr2", [128, NE], st=se)
                hrow = [sb("hrow%d" % i, [128, D], BF16, st=se) for i in range(2)]
                NW = 3
                wgb = [sb("wgb%d" % i, [128, 4096], BF16, st=se) for i in range(NW)]
                wub = [sb("wub%d" % i, [128, 4096], BF16, st=se) for i in range(NW)]
                wdb = [sb("wdb%d" % i, [128, 4096], BF16, st=se) for i in range(NW)]
                xbT = [sb("xbT%d" % i, [128, 8, 128], BF16, st=se) for i in range(2)]
                sgate = [sb("sgate%d" % i, [128, 512], st=se) for i in range(2)]
                actb = [sb("actb%d" % i, [128, 512], BF16, st=se) for i in range(2)]
                actT = [sb("actT%d" % i, [128, 4, 128], BF16, st=se) for i in range(2)]
                ybuf = [sb("ybuf%d" % i, [128, D], st=se) for i in range(2)]
                y1 = [sb("y1_%d" % i, [128, D], st=se) for i in range(2)]
                y2 = [sb("y2_%d" % i, [128, D], st=se) for i in range(2)]
                xm = [sb("xm%d" % i, [128, D], st=se) for i in range(2)]
                junk2 = sb("junk2", [128, D], st=se)
                st2 = sb("st2", [128, 4], st=se)
                g2_bc = sb("g2_bc", [128, 2, D], st=se)
                Rall = sb("Rall", [128, NSUB, NE], st=se)
                Mall = sb("Mall", [128, NSUB, NE], BF16, st=se)
                dest_f = sb("dest_f", [128, NSUB, 2], st=se)
                dest_i = sb("dest_i", [128, NSUB, 2], I32, st=se)
                widx = sb("widx", [128, NB, 2], I32, st=se)
                for s in range(2):
                    LD(lambda e, s=s: e.dma_start(out=g2_bc[:, s, :], in_=MODROW[l, s:s + 1, 5 * D:6 * D].to_broadcast([128, D])), w=["g2_bc"])

                zrow = sb("zrow", [128, D], BF16, st=se)
                V(lambda e: e.memset(zrow[:], 0.0), w=["zrow"])
                XBz = XB.rearrange("(n p) f -> p n f", p=128)
                ZS = 10 if NSB % 10 == 0 else 12
                assert NSB % ZS == 0
                for n0 in range(0, NSB, ZS):
                    LD(lambda e, n0=n0: e.dma_start(out=XBz[:, n0:n0 + ZS, :], in_=zrow[:].unsqueeze(1).to_broadcast([128, ZS, D])),
                       r=["zrow"], w=["XBz"])
                V(lambda e: e.tensor_tensor(out=Mall[:], in0=M1all[:], in1=M2all[:], op=ALU.add), r=["M1all", "M2all"], w=["Mall"])
                V(lambda e: e.memset(pre[:], 0.0), w=["pre"])
                for i in range(NSUB):
                    T(lambda e, i=i: e.matmul(ps[0][:, 0:NE], lhsT=trib[:], rhs=Mall[:, i, :], start=True, stop=True), r=["Mall", "trib"], w=["ps0"])
                    T(lambda e, i=i: e.matmul(ps[1][:, 0:NE], lhsT=onesb[:], rhs=Mall[:, i, :], start=True, stop=True), r=["Mall", "onesb"], w=["ps1"])
                    V(lambda e, i=i: e.tensor_tensor(out=Rall[:, i, :], in0=ps[0][:, 0:NE], in1=pre[:], op=ALU.add), r=["ps0", "pre"], w=["Rall"])
                    V(lambda e: e.tensor_tensor(out=pre[:], in0=ps[1][:, 0:NE], in1=pre[:], op=ALU.add), r=["ps1", "pre"], w=["pre"])
                cmpT = cmp3[:].rearrange("p n e -> p (n e)").rearrange("p (e n) -> p e n", n=NB)
                V(lambda e: e.tensor_tensor(out=cmpT, in0=pre[:].unsqueeze(2).to_broadcast([128, NE, NB]),
                                            in1=nblk.unsqueeze(1).to_broadcast([128, NE, NB]), op=ALU.is_gt), r=["pre", "cs"], w=["cmp3"])
                V(lambda e: e.tensor_reduce(out=padf[:], in_=cmpT, axis=AX.X, op=ALU.add), r=["cmp3"], w=["padf"])
                V(lambda e: e.tensor_scalar(out=padf[:], in0=padf[:], scalar1=float(BLK), scalar2=None, op0=ALU.mult), r=["padf"], w=["padf"])
                V(lambda e: e.tensor_tensor_scan(out=incl[:], data0=ones32[:, 0:NE], data1=padf[:], initial=0.0, op0=ALU.mult, op1=ALU.add),
                  r=["padf", "cs"], w=["incl"])
                V(lambda e: e.tensor_tensor(out=offs[:], in0=incl[:], in1=padf[:], op=ALU.subtract), r=["incl", "padf"], w=["offs"])
                V(lambda e: e.tensor_tensor(out=cmp3[:], in0=nblk.unsqueeze(2).to_broadcast([128, NB, NE]),
                                            in1=incl[:].unsqueeze(1).to_broadcast([128, NB, NE]), op=ALU.is_ge), r=["incl", "cs"], w=["cmp3"])
                V(lambda e: e.tensor_reduce(out=bef[:], in_=cmp3[:], axis=AX.X, op=ALU.add), r=["cmp3"], w=["bef"])
                V(lambda e: e.tensor_scalar(out=bef[:], in0=bef[:], scalar1=31.0, scalar2=128.0, op0=ALU.min, op1=ALU.mult), r=["bef"], w=["bef"])
                V(lambda e: e.tensor_scalar(out=bef[:], in0=bef[:], scalar1=pcol, scalar2=None, op0=ALU.add), r=["bef", "cs"], w=["bef"])
                V(lambda e: e.tensor_copy(widx[:, :, 0], bef[:]), r=["bef"], w=["widx"])
                V(lambda e: e.tensor_scalar(out=bef[:], in0=bef[:], scalar1=4096.0, scalar2=None, op0=ALU.add), r=["bef"], w=["bef"])
                V(lambda e: e.tensor_copy(widx[:, :, 1], bef[:]), r=["bef"], w=["widx"])
                for i in range(NSUB):
                    V(lambda e, i=i: e.tensor_tensor(out=tmpr[:], in0=Rall[:, i, :], in1=offs[:], op=ALU.add), r=["Rall", "offs"], w=["tmpr"])
                    V(lambda e, i=i: e.scalar_tensor_tensor(out=tmpr2[:], in0=tmpr[:], scalar=1.0, in1=M1all[:, i, :], op0=ALU.mult, op1=ALU.mult,
                                                            accum_out=dest_f[:, i, 0:1]), r=["tmpr", "M1all"], w=["tmpr2", "dest_f"])
                    V(lambda e, i=i: e.scalar_tensor_tensor(out=tmpr2[:], in0=tmpr[:], scalar=1.0, in1=M2all[:, i, :], op0=ALU.mult, op1=ALU.mult,
                                                            accum_out=dest_f[:, i, 1:2]), r=["tmpr", "M2all"], w=["tmpr2", "dest_f"])
                V(lambda e: e.tensor_copy(dest_i[:], dest_f[:]), r=["dest_f"], w=["dest_i"])
                for i in range(NSUB):
                    hb = hrow[i % 2]
                    hn = "hrow%d" % (i % 2)
                    LD(lambda e, i=i, hb=hb: e.dma_start(out=hb[:], in_=H2v[:, i, :]), r=["H2_%d" % i], w=[hn])
                    for k in range(2):
                        GD(lambda e, i=i, k=k, hb=hb: e.indirect_dma_start(
                            out=XB, out_offset=bass.IndirectOffsetOnAxis(ap=dest_i[:, i, k:k + 1], axis=0), in_=hb[:], in_offset=None),
                           r=[hn, "dest_i", "XBz"], w=["XBs%d_%d" % (i, k)])
                P.barrier_all()

                stop("p3")
                XBv = XB.rearrange("(n p) f -> p n f", p=128)
                YBv = YB.rearrange("(n p) f -> p n f", p=128)

                def wload(n):
                    k = n % NW
                    for hf in range(2):
                        GD(lambda e, n=n, hf=hf, k=k: e.indirect_dma_start(
                            out=wgb[k][:, hf * 2048:(hf + 1) * 2048], out_offset=None, in_=wg2[l],
                            in_offset=bass.IndirectOffsetOnAxis(ap=widx[:, n, hf:hf + 1], axis=0)), r=["widx"], w=["wgb%d" % k])
                        GD(lambda e, n=n, hf=hf, k=k: e.indirect_dma_start(
                            out=wub[k][:, hf * 2048:(hf + 1) * 2048], out_offset=None, in_=wu2[l],
                            in_offset=bass.IndirectOffsetOnAxis(ap=widx[:, n, hf:hf + 1], axis=0)), r=["widx"], w=["wub%d" % k])
                        GD(lambda e, n=n, hf=hf, k=k: e.indirect_dma_start(
                            out=wdb[k][:, hf * 2048:(hf + 1) * 2048], out_offset=None, in_=wd2[l],
                            in_offset=bass.IndirectOffsetOnAxis(ap=widx[:, n, hf:hf + 1], axis=0)), r=["widx"], w=["wdb%d" % k])

                def stA1(sbi):
                    p = sbi % 2
                    hb, hn = hrow[p], "hrow%d" % p
                    LD(lambda e: e.dma_start(out=hb[:], in_=XBv[:, sbi, :]), r=[], w=[hn])
                    for c in range(8):
                        T(lambda e, c=c: e.matmul(ps[4 + c // 4][:, (c % 4) * 128:(c % 4 + 1) * 128], lhsT=hb[:, c * 128:(c + 1) * 128],
                                                  rhs=identb[:], start=True, stop=True), r=[hn, "identb"], w=["ps%d" % (4 + c // 4)])
                    A(lambda e: e.activation(out=xbT[p][:, 0:4, :].rearrange("p c t -> p (c t)"), in_=ps[4][:, :], func=AF.Copy),
                      r=["ps4"], w=["xbT%d" % p])
                    V(lambda e: e.tensor_copy(xbT[p][:, 4:8, :].rearrange("p c t -> p (c t)"), ps[5][:, :]), r=["ps5"], w=["xbT%d" % p])

                def stA2(sbi):
                    p = sbi % 2
                    kw = (sbi // (BLK // 128)) % NW
                    wg3 = wgb[kw][:].rearrange("p (c f) -> p c f", f=512)
                    wu3 = wub[kw][:].rearrange("p (c f) -> p c f", f=512)
                    for c in range(8):
                        T(lambda e, c=c: e.matmul(ps[2 * p][:, :], lhsT=xbT[p][:, c, :], rhs=wg3[:, c, :], start=(c == 0), stop=(c == 7)),
                          r=["xbT%d" % p, "wgb%d" % kw], w=["ps%d" % (2 * p)])
                    for c in range(8):
                        T(lambda e, c=c: e.matmul(ps[2 * p + 1][:, :], lhsT=xbT[p][:, c, :], rhs=wu3[:, c, :], start=(c == 0), stop=(c == 7)),
                          r=["xbT%d" % p, "wub%d" % kw], w=["ps%d" % (2 * p + 1)])

                def stA3(sbi):
                    p = sbi % 2
                    A(lambda e: e.activation(out=sgate[p][:], in_=ps[2 * p][:, :], func=AF.Silu), r=["ps%d" % (2 * p)], w=["sgate%d" % p])
                    V(lambda e: e.tensor_tensor(out=actb[p][:], in0=ps[2 * p + 1][:, :], in1=sgate[p][:], op=ALU.mult),
                      r=["ps%d" % (2 * p + 1), "sgate%d" % p], w=["actb%d" % p])

                def stB1(sbi):
                    p = sbi % 2
                    for c in range(4):
                        T(lambda e, c=c: e.matmul(ps[6][:, c * 128:(c + 1) * 128], lhsT=actb[p][:, c * 128:(c + 1) * 128], rhs=identb[:],
                                                  start=True, stop=True), r=["actb%d" % p, "identb"], w=["ps6"])
                    V(lambda e: e.tensor_copy(actT[p][:].rearrange("p c t -> p (c t)"), ps[6][:, :]), r=["ps6"], w=["actT%d" % p])

                def stB2(sbi):
                    p = sbi % 2
                    kw = (sbi // (BLK // 128)) % NW
                    wd3 = wdb[kw][:].rearrange("p (c f) -> p c f", f=1024)
                    for hf in range(2):
                        for c in range(4):
                            T(lambda e, c=c, hf=hf: e.matmul(ps[6 + hf][:, :], lhsT=actT[p][:, c, :], rhs=wd3[:, c, hf * 512:(hf + 1) * 512],
                                                             start=(c == 0), stop=(c == 3)), r=["actT%d" % p, "wdb%d" % kw], w=["ps%d" % (6 + hf)])
                    yb, yn = ybuf[p], "ybuf%d" % p
                    A(lambda e: e.activation(out=yb[:, 0:512], in_=ps[6][:, :], func=AF.Copy), r=["ps6"], w=[yn])
                    V(lambda e: e.tensor_copy(yb[:, 512:1024], ps[7][:, :]), r=["ps7"], w=[yn])
                    LD(lambda e: e.dma_start(out=YBv[:, sbi, :], in_=yb[:]), r=[yn], w=["YB%d" % sbi])

                SPB = BLK // 128
                wload(0)
                wload(1)
                stA1(0)
                stA2(0)
                stA3(0)
                for sbi in range(NSB):
                    n = sbi // SPB
                    if sbi % SPB == 0 and n + 2 < NB:
                        wload(n + 2)
                    if sbi + 1 < NSB:
                        stA1(sbi + 1)
                    stB1(sbi)
                    if sbi + 1 < NSB:
                        stA2(sbi + 1)
                        stA3(sbi + 1)
                    stB2(sbi)
                P.barrier_all()

                stop("p4")
                XOv = XS1.rearrange("(n p) f -> p n f", p=128)
                OUTv = out.rearrange("(n p) f -> p n f", p=128)
                for i in range(NSUB):
                    if last and i < 2:
                        continue
                    k = i % 2
                    s = 1 if i < 2 else 0
                    GD(lambda e, i=i, k=k: e.indirect_dma_start(out=y1[k][:], out_offset=None, in_=YB,
                                                                in_offset=bass.IndirectOffsetOnAxis(ap=dest_i[:, i, 0:1], axis=0)),
                       r=["dest_i"], w=["y1_%d" % k])
                    GD(lambda e, i=i, k=k: e.indirect_dma_start(out=y2[k][:], out_offset=None, in_=YB,
                                                                in_offset=bass.IndirectOffsetOnAxis(ap=dest_i[:, i, 1:2], axis=0)),
                       r=["dest_i"], w=["y2_%d" % k])
                    LD(lambda e, i=i, k=k: e.dma_start(out=xm[k][:], in_=XMv[:, i, :]), r=["XM%d" % (i // 2)], w=["xm%d" % k])
                    V(lambda e, i=i, k=k: e.tensor_scalar(out=y1[k][:], in0=y1[k][:], scalar1=W12[:, i, 0:1], scalar2=None, op0=ALU.mult),
                      r=["y1_%d" % k, "W12"], w=["y1_%d" % k])
                    V(lambda e, i=i, k=k: e.scalar_tensor_tensor(out=y1[k][:], in0=y2[k][:], scalar=W12[:, i, 1:2], in1=y1[k][:],
                                                                 op0=ALU.mult, op1=ALU.add), r=["y1_%d" % k, "y2_%d" % k, "W12"], w=["y1_%d" % k])
                    V(lambda e, k=k, s=s: e.tensor_tensor(out=y1[k][:], in0=y1[k][:], in1=g2_bc[:, s, :], op=ALU.mult),
                      r=["y1_%d" % k, "g2_bc"], w=["y1_%d" % k])
                    V(lambda e, k=k: e.tensor_tensor(out=xm[k][:], in0=xm[k][:], in1=y1[k][:], op=ALU.add), r=["xm%d" % k, "y1_%d" % k], w=["xm%d" % k])
                    if not last:
                        LD(lambda e, i=i, k=k: e.dma_start(out=XOv[:, i, :], in_=xm[k][:]), r=["xm%d" % k], w=["XIN%d" % (i // 2)])
                    else:
                        if dbg:
                            LD(lambda e, i=i, k=k: e.dma_start(out=XOv[:, i, :], in_=xm[k][:]), r=["xm%d" % k], w=["XIN%d" % (i // 2)])
                        A(lambda e, k=k: e.activation(out=junk2[:], in_=xm[k][:], func=AF.Square, accum_out=st2[:, 0:1]), r=["xm%d" % k], w=["junk2", "st2"])
                        V(lambda e: e.tensor_scalar(out=st2[:, 1:2], in0=st2[:, 0:1], scalar1=1.0 / D, scalar2=EPS, op0=ALU.mult, op1=ALU.add),
                          r=["st2"], w=["st2"])
                        A(lambda e: e.activation(out=st2[:, 1:2], in_=st2[:, 1:2], func=AF.Sqrt), r=["st2"], w=["st2"])
                        V(lambda e: e.reciprocal(out=st2[:, 2:3], in_=st2[:, 1:2]), r=["st2"], w=["st2"])
                        V(lambda e, k=k: e.scalar_tensor_tensor(out=xm[k][:], in0=xm[k][:], scalar=st2[:, 2:3], in1=fn_bc[:], op0=ALU.mult, op1=ALU.mult),
                          r=["xm%d" % k, "st2", "fn_bc"], w=["xm%d" % k])
                        LD(lambda e, i=i, k=k: e.dma_start(out=OUTv[:, i - 2, :], in_=xm[k][:]), r=["xm%d" % k], w=["OUT"], is_out=True)
                if dbg and last:
                    LD(lambda e: e.dma_start(out=YBv[:, NSB - 1, :], in_=fn_bc[:]), r=["fn_bc"], w=["YBdbg"])
                    LD(lambda e: e.dma_start(out=YBv[:, NSB - 2, 0:4], in_=st2[:]), r=["st2"], w=["YBdbg2"])
                P.barrier_all()
                se.close(); open_stacks.remove(se)
        except _Stop:
            for stx in reversed(open_stacks):
                stx.close()
        P.finish()
        block = E(nc.Block())
        P.replay(block)
    return nc


def _consts():
    c = np.zeros((128, CW), np.float32)
    c[:, 0:128] = np.eye(128, dtype=np.float32)
    tp = np.arange(128)[:, None]
    tt = np.arange(128)[None, :]
    c[:, 128:256] = (tp < tt).astype(np.float32)
    m = np.ones(256, np.float32)
    m[::64] = 0.0
    c[:, 256:512] = m[None, :]
    c[:, 512:640] = 1.0
    c[:, 640:640 + NB] = (np.arange(NB) * float(BLK))[None, :]
    c[:, 760] = np.arange(128)
    same = (tp // 64) == (tt // 64)
    m8 = np.zeros((128, 256), np.uint8)
    m8[:, 0:128] = (same & (tt >= tp)).astype(np.uint8)
    m8[:, 128:256] = (same & (tt <= tp)).astype(np.uint8)
    return c, m8


def _relay_gate(w):
    a = w.reshape(NE, 8, 128, 512).transpose(0, 2, 1, 3).reshape(NE, 128, 2, 2048)
    return np.ascontiguousarray(a.transpose(2, 0, 1, 3).reshape(2 * NE * 128, 2048))


def _relay_down(w):
    a = w.reshape(NE, 4, 128, 1024).transpose(0, 2, 1, 3).reshape(NE, 128, 2, 2048)
    return np.ascontiguousarray(a.transpose(2, 0, 1, 3).reshape(2 * NE * 128, 2048))


def make_in_maps(inputs, cores, small=False):
    f = lambda a: np.ascontiguousarray(np.asarray(a, dtype=np.float32))
    x, c, ctx, c_ctx = f(inputs['x']), f(inputs['c']), f(inputs['ctx']), f(inputs['c_ctx'])
    norm1, norm2 = f(inputs['norm1']), f(inputs['norm2'])
    cst, cst8 = _consts()
    shared = dict(
        w_mod=f(inputs['w_mod']), b_mod=f(inputs['b_mod']), w_in=f(inputs['w_in']), n2row=norm2,
        sgn=f(inputs['sgu_norm']), sgw=f(inputs['sgu_w']), sgb=f(inputs['sgu_b']).reshape(2, 512),
        w_out=f(inputs['w_out']),
        wrt=np.ascontiguousarray(np.concatenate([f(inputs['w_group']), f(inputs['w_router'])], axis=2)),
        brt=np.ascontiguousarray(np.concatenate([f(inputs['b_group']), f(inputs['b_router'])], axis=1)),
        wg2_0=_relay_gate(f(inputs['w_gate'][0])), wg2_1=_relay_gate(f(inputs['w_gate'][1])),
        wu2_0=_relay_gate(f(inputs['w_up'][0])), wu2_1=_relay_gate(f(inputs['w_up'][1])),
        wd2_0=_relay_down(f(inputs['w_down'][0])), wd2_1=_relay_down(f(inputs['w_down'][1])),
        fnorm=f(inputs['final_norm']).reshape(1, D), cst=cst, cst8=cst8,
    )
    if small:
        for k in list(shared):
            if k[:3] in ("wg2", "wu2", "wd2"):
                shared[k] = np.zeros((8, 8), np.float32)
    maps = []
    for b in cores:
        rows = np.zeros((72, 128), np.float32)
        for l in range(2):
            rows[l * 16:l * 16 + 8] = norm1[l].reshape(8, 128)
            rows[l * 16 + 8:l * 16 + 16] = norm2[l].reshape(8, 128)
        rows[32:48] = f(inputs['lb_logits']).reshape(16, 128)
        rows[48:56] = f(inputs['hgrn_norm']).reshape(8, 128)
        rows[56:64] = c[b].reshape(8, 128)
        rows[64:72] = c_ctx.reshape(8, 128)
        m = dict(shared)
        m['xs'] = np.ascontiguousarray(np.concatenate([ctx[b], x[b]], axis=0))
        m['rows_in'] = rows
        maps.append(m)
    return maps


def kernel(**inputs):
    n = 8
    nc = build_nc()
    in_maps = make_in_maps(inputs, list(range(n)))
    res = run_bass_kernel_spmd(nc, in_maps, core_ids=list(range(n)))
    return np.stack([np.asarray(r["out"], dtype=np.float32) for r in res.results], axis=0)
```
